# Optimizing a Trainium2 kernel written in Bass

```python
import math
import jax
import jax.numpy as jnp
from jax import lax
import numpy as np

D_MODEL = 4096
BATCH = 8
SEQ = 2048
DEPTH = 4

GRID_W = 64
CTX_LEN = 256
N_MIXERS = 4
ROPE_BASE = 10000.0
NORM_EPS = 1e-6
MOD_RANK = 512

SWA_HEADS = 32
SWA_KV_HEADS = 8
SWA_HEAD_DIM = 128
SWA_WINDOW = 128
SWA_BLOCK = 128

SSD_D_INNER = 2 * D_MODEL
SSD_HEAD_DIM = 64
SSD_HEADS = SSD_D_INNER // SSD_HEAD_DIM
SSD_GROUPS = 8
SSD_STATE = 128
SSD_CONV = 3
SSD_CHUNK = 128

S5_WIDTH = D_MODEL
S5_GROUP = 16
S5_NGROUPS = S5_WIDTH // S5_GROUP
S5_STATE = 64
S5_CHUNK = 128

MLA_HEADS = 32
MLA_Q_RANK = 1024
MLA_KV_RANK = 512
MLA_NOPE = 128
MLA_ROPE = 64
MLA_V = 128
MLA_BLOCK = 128

MOE_GROUPS = 4
MOE_PER_GROUP = 8
MOE_EXPERTS = MOE_GROUPS * MOE_PER_GROUP
MOE_TOPK = 2
MOE_HIDDEN = 256
MOE_BLOCK = 128

kernel_name = 'hybrid_interleaved_diffusion_trunk'


def rmsnorm(x, g):
    xf = x.astype(jnp.float32)
    xf = xf * lax.rsqrt(jnp.mean(xf * xf, axis=-1, keepdims=True) + NORM_EPS)
    return (xf * g.astype(jnp.float32)).astype(x.dtype)


def adaln(cvec, w_down, w_up, b):
    m = (jax.nn.silu(cvec) @ w_down) @ w_up + b
    return m.reshape(cvec.shape[:-1] + (6, D_MODEL))


def modulate(t, g, shift, scale):
    return rmsnorm(t, g) * (1.0 + scale) + shift


def rope_tables(n_tokens, rot_dim):
    rows = n_tokens // GRID_W
    row = jnp.repeat(jnp.arange(rows, dtype=jnp.float32), GRID_W)
    col = jnp.tile(jnp.arange(GRID_W, dtype=jnp.float32), rows)
    axis_dim = rot_dim // 2
    inv_freq = ROPE_BASE ** (-jnp.arange(0, axis_dim, 2, dtype=jnp.float32) / axis_dim)
    ang = jnp.stack([row[:, None] * inv_freq, col[:, None] * inv_freq], 0)
    return jnp.cos(ang), jnp.sin(ang)


def apply_rope_2d(x, tables):
    cos, sin = tables
    extra = x.ndim - 3
    axis_dim = x.shape[-1] // 2
    half = axis_dim // 2
    outs = []
    for a in range(2):
        xa = x[..., a * axis_dim:(a + 1) * axis_dim]
        x1, x2 = xa[..., :half], xa[..., half:]
        cs = cos[a].reshape((cos.shape[1],) + (1,) * extra + (half,)).astype(x.dtype)
        sn = sin[a].reshape((sin.shape[1],) + (1,) * extra + (half,)).astype(x.dtype)
        outs += [x1 * cs - x2 * sn, x2 * cs + x1 * sn]
    return jnp.concatenate(outs, -1)


def sink_attend(s, vals, sink):
    sk = sink[None, :, :, None]
    m = jnp.maximum(jnp.max(s, axis=-1), sk)
    p = jnp.exp(s - m[..., None])
    p = p / (jnp.sum(p, axis=-1, keepdims=True) + jnp.exp(sk - m)[..., None])
    return jnp.einsum('bkgqs,bskd->bqkgd', p.astype(vals.dtype), vals)


def mixer_swa(h, hc, w_in, q_g, k_g, sinks, w_out, ctx_out):
    n_b, n_s, _ = h.shape
    grp = SWA_HEADS // SWA_KV_HEADS
    nq = SWA_HEADS * SWA_HEAD_DIM
    nkv = SWA_KV_HEADS * SWA_HEAD_DIM
    scale = SWA_HEAD_DIM ** -0.5

    def project(t):
        n_l = t.shape[1]
        p = t @ w_in
        q = rmsnorm(p[..., :nq].reshape(n_b, n_l, SWA_KV_HEADS, grp, SWA_HEAD_DIM), q_g)
        k = rmsnorm(p[..., nq:nq + nkv].reshape(n_b, n_l, SWA_KV_HEADS, SWA_HEAD_DIM), k_g)
        v = p[..., nq + nkv:].reshape(n_b, n_l, SWA_KV_HEADS, SWA_HEAD_DIM)
        return q * scale, k, v

    q, k, v = project(h)
    qc, kc, vc = project(hc)
    rope = rope_tables(n_s, SWA_HEAD_DIM)
    q = apply_rope_2d(q, rope)
    k = apply_rope_2d(k, rope)
    sink = sinks.reshape(SWA_KV_HEADS, grp).astype(jnp.float32)

    n_blk = n_s // SWA_BLOCK
    pad = ((0, 0), (SWA_BLOCK, SWA_BLOCK), (0, 0), (0, 0))
    kpad, vpad = jnp.pad(k, pad), jnp.pad(v, pad)
    qb = q.reshape(n_b, n_blk, SWA_BLOCK, SWA_KV_HEADS, grp, SWA_HEAD_DIM).swapaxes(0, 1)

    def block(args):
        j, qj = args
        start = j * SWA_BLOCK
        kb = lax.dynamic_slice_in_dim(kpad, start, 3 * SWA_BLOCK, axis=1)
        vb = lax.dynamic_slice_in_dim(vpad, start, 3 * SWA_BLOCK, axis=1)
        qpos = start + jnp.arange(SWA_BLOCK)
        kpos = start - SWA_BLOCK + jnp.arange(3 * SWA_BLOCK)
        valid = (jnp.abs(kpos[None, :] - qpos[:, None]) <= SWA_WINDOW) & ((kpos >= 0) & (kpos < n_s))[None, :]
        s_lat = jnp.einsum('bqkgd,bskd->bkgqs', qj, kb, preferred_element_type=jnp.float32)
        s_lat = jnp.where(valid, s_lat, -jnp.inf)
        s_ctx = jnp.einsum('bqkgd,bskd->bkgqs', qj, kc, preferred_element_type=jnp.float32)
        return sink_attend(jnp.concatenate([s_lat, s_ctx], -1), jnp.concatenate([vb, vc], 1), sink)

    o = lax.map(block, (jnp.arange(n_blk), qb))
    out = o.swapaxes(0, 1).reshape(n_b, n_s, nq) @ w_out
    if not ctx_out:
        return out, None
    s_cc = jnp.einsum('bqkgd,bskd->bkgqs', qc, kc, preferred_element_type=jnp.float32)
    oc = sink_attend(s_cc, vc, sink).reshape(n_b, hc.shape[1], nq) @ w_out
    return out, oc


def dwconv_centred(t, w, b):
    width = w.shape[0]
    y = lax.conv_general_dilated(t, w[:, None, :].astype(t.dtype), (1,), [(width // 2, width // 2)],
                                 dimension_numbers=('NWC', 'WIO', 'NWC'), feature_group_count=t.shape[-1])
    return y + b


def ssd_scan(xs, dt, a, bm, cm, state0):
    f32 = jnp.float32
    n_b, n_l = xs.shape[:2]
    n_c = n_l // SSD_CHUNK

    def chunks(t):
        t = t.astype(f32)
        return t.reshape((n_b, n_c, SSD_CHUNK) + t.shape[2:]).swapaxes(0, 1)

    lower_tri = jnp.tril(jnp.ones((SSD_CHUNK, SSD_CHUNK), bool))[None, :, :, None, None]

    def step(state, inp):
        xq, dq, bq, cq = inp
        cum = jnp.cumsum(dq * a, axis=1)
        seg = cum[:, :, None] - cum[:, None, :]
        decay = jnp.exp(jnp.where(lower_tri, seg, -jnp.inf))
        xdt = xq * dq[..., None]
        cb = jnp.einsum('btgn,bsgn->btsg', cq, bq)
        y = jnp.einsum('btsg,btsgj,bsgjp->btgjp', cb, decay, xdt)
        y = y + jnp.einsum('btgn,bgjpn->btgjp', cq, state) * jnp.exp(cum)[..., None]
        to_end = jnp.exp(cum[:, -1:] - cum)
        state = state * jnp.exp(cum[:, -1])[..., None, None] + jnp.einsum('bsgj,bsgjp,bsgn->bgjpn', to_end, xdt, bq)
        return state, y

    state, ys = lax.scan(step, state0.astype(f32), (chunks(xs), chunks(dt), chunks(bm), chunks(cm)))
    return ys.swapaxes(0, 1).reshape(xs.shape), state


def mixer_ssd(h, hc, w_in, conv_w, conv_b, dt_bias, a_log, d_skip, norm_g, w_out, ctx_out):
    f32 = jnp.float32
    n_b = h.shape[0]
    hg = SSD_HEADS // SSD_GROUPS
    gn = SSD_GROUPS * SSD_STATE

    def project(t):
        n_l = t.shape[1]
        p = t @ w_in
        z = p[..., :SSD_D_INNER]
        xbc = jax.nn.silu(dwconv_centred(p[..., SSD_D_INNER:2 * SSD_D_INNER + 2 * gn], conv_w, conv_b))
        xs = xbc[..., :SSD_D_INNER].reshape(n_b, n_l, SSD_GROUPS, hg, SSD_HEAD_DIM)
        bm = xbc[..., SSD_D_INNER:SSD_D_INNER + gn].reshape(n_b, n_l, SSD_GROUPS, SSD_STATE)
        cm = xbc[..., SSD_D_INNER + gn:].reshape(n_b, n_l, SSD_GROUPS, SSD_STATE)
        dt = p[..., 2 * SSD_D_INNER + 2 * gn:].reshape(n_b, n_l, 2, SSD_GROUPS, hg)
        return z, xs, bm, cm, dt

    z, xs, bm, cm, dt = project(h)
    zc, xsc, bmc, cmc, dtc = project(hc)
    dsk = d_skip.astype(f32).reshape(SSD_GROUPS, hg)[..., None]
    y = xs.astype(f32) * dsk
    yc = xsc.astype(f32) * dsk
    zero = jnp.zeros((n_b, SSD_GROUPS, hg, SSD_HEAD_DIM, SSD_STATE), f32)

    def flip(t, d):
        return jnp.flip(t, axis=1) if d else t

    for d in range(2):
        a = -jnp.exp(a_log[d].astype(f32)).reshape(SSD_GROUPS, hg)
        bias = dt_bias[d].astype(f32).reshape(SSD_GROUPS, hg)
        dt_l = jax.nn.softplus(dt[:, :, d].astype(f32) + bias)
        dt_c = jax.nn.softplus(dtc[:, :, d].astype(f32) + bias)
        yc_d, state_c = ssd_scan(flip(xsc, d), flip(dt_c, d), a, flip(bmc, d), flip(cmc, d), zero)
        y_d, _ = ssd_scan(flip(xs, d), flip(dt_l, d), a, flip(bm, d), flip(cm, d), state_c)
        y = y + flip(y_d, d)
        if ctx_out:
            yc = yc + flip(yc_d, d)

    def finish(yy, zz):
        n_l = yy.shape[1]
        g = yy.reshape(n_b, n_l, SSD_D_INNER).astype(h.dtype) * jax.nn.silu(zz)
        g = rmsnorm(g.reshape(n_b, n_l, SSD_GROUPS, -1), norm_g.reshape(SSD_GROUPS, -1))
        return g.reshape(n_b, n_l, SSD_D_INNER) @ w_out

    return finish(y, z), (finish(yc, zc) if ctx_out else None)


def complex_affine_combine(e1, e2):
    a1r, a1i, x1r, x1i = e1
    a2r, a2i, x2r, x2i = e2
    return (a2r * a1r - a2i * a1i, a2r * a1i + a2i * a1r,
            a2r * x1r - a2i * x1i + x2r, a2r * x1i + a2i * x1r + x2i)


def s5_scan(u, ar, ai, bbr, bbi, cr, ci, s0r, s0i):
    n_b, n_l = u.shape[:2]
    n_c = n_l // S5_CHUNK
    uch = u.reshape(n_b, n_c, S5_CHUNK, S5_NGROUPS, S5_GROUP).swapaxes(0, 1)
    a_r = jnp.broadcast_to(ar, (n_b, S5_CHUNK, S5_NGROUPS, S5_STATE))
    a_i = jnp.broadcast_to(ai, (n_b, S5_CHUNK, S5_NGROUPS, S5_STATE))

    def step(carry, uq):
        sr, si = carry
        xr = jnp.einsum('bqgc,gpc->bqgp', uq, bbr)
        xi = jnp.einsum('bqgc,gpc->bqgp', uq, bbi)
        pr, pi, lr, li = lax.associative_scan(complex_affine_combine, (a_r, a_i, xr, xi), axis=1)
        st_r = pr * sr[:, None] - pi * si[:, None] + lr
        st_i = pr * si[:, None] + pi * sr[:, None] + li
        yq = jnp.einsum('gcp,bqgp->bqgc', cr, st_r) - jnp.einsum('gcp,bqgp->bqgc', ci, st_i)
        return (st_r[:, -1], st_i[:, -1]), yq

    (sr, si), ys = lax.scan(step, (s0r, s0i), uch)
    return ys.swapaxes(0, 1).reshape(u.shape), sr, si


def mixer_s5(h, hc, w_in, lam_re, lam_im, log_dt, b_re, b_im, c_re, c_im, d_skip, w_glu, ctx_out):
    f32 = jnp.float32
    n_b = h.shape[0]
    u = (h @ w_in).astype(f32)
    uc = (hc @ w_in).astype(f32)
    ug = u.reshape(u.shape[:2] + (S5_NGROUPS, S5_GROUP))
    ucg = uc.reshape(uc.shape[:2] + (S5_NGROUPS, S5_GROUP))
    br, bi, cr, ci = (t.astype(f32) for t in (b_re, b_im, c_re, c_im))
    dsk = d_skip.astype(f32)
    y = dsk * u
    yc = dsk * uc
    zero = jnp.zeros((n_b, S5_NGROUPS, S5_STATE), f32)

    def flip(t, d):
        return jnp.flip(t, axis=1) if d else t

    for d in range(2):
        lr = lam_re[d].astype(f32)
        li = lam_im[d].astype(f32)
        step = jnp.exp(log_dt[d].astype(f32))[:, None]
        mag = jnp.exp(lr * step)
        ar, ai = mag * jnp.cos(li * step), mag * jnp.sin(li * step)
        den = lr * lr + li * li
        fr = ((ar - 1.0) * lr + ai * li) / den
        fi = (ai * lr - (ar - 1.0) * li) / den
        bbr = fr[..., None] * br - fi[..., None] * bi
        bbi = fr[..., None] * bi + fi[..., None] * br
        yc_d, sr, si = s5_scan(flip(ucg, d), ar, ai, bbr, bbi, cr, ci, zero, zero)
        y_d, _, _ = s5_scan(flip(ug, d), ar, ai, bbr, bbi, cr, ci, sr, si)
        y = y + flip(y_d, d).reshape(u.shape)
        if ctx_out:
            yc = yc + flip(yc_d, d).reshape(uc.shape)

    def glu(t):
        g = jax.nn.gelu(t).astype(h.dtype) @ w_glu
        return g[..., :D_MODEL] * jax.nn.sigmoid(g[..., D_MODEL:])

    return glu(y), (glu(yc) if ctx_out else None)


def mixer_mla(h, hc, w_in, q_a_g, kv_a_g, w_uq, w_ukv, q_g, k_g, w_out, ctx_out):
    n_b, n_s, _ = h.shape
    dk = MLA_NOPE + MLA_ROPE
    scale = dk ** -0.5

    def project(t, rope):
        n_l = t.shape[1]
        p = t @ w_in
        cq = rmsnorm(p[..., :MLA_Q_RANK], q_a_g)
        ckv = rmsnorm(p[..., MLA_Q_RANK:MLA_Q_RANK + MLA_KV_RANK], kv_a_g)
        k_rope = jnp.broadcast_to(p[..., None, MLA_Q_RANK + MLA_KV_RANK:], (n_b, n_l, MLA_HEADS, MLA_ROPE))
        q = rmsnorm((cq @ w_uq).reshape(n_b, n_l, MLA_HEADS, dk), q_g)
        kv = (ckv @ w_ukv).reshape(n_b, n_l, MLA_HEADS, MLA_NOPE + MLA_V)
        k = rmsnorm(jnp.concatenate([kv[..., :MLA_NOPE], k_rope], -1), k_g)
        v = kv[..., MLA_NOPE:]
        if rope is not None:
            q = jnp.concatenate([q[..., :MLA_NOPE], apply_rope_2d(q[..., MLA_NOPE:], rope)], -1)
            k = jnp.concatenate([k[..., :MLA_NOPE], apply_rope_2d(k[..., MLA_NOPE:], rope)], -1)
        return q * scale, k, v

    q, k, v = project(h, rope_tables(n_s, MLA_ROPE))
    qc, kc, vc = project(hc, None)
    k_all = jnp.concatenate([k, kc], 1)
    v_all = jnp.concatenate([v, vc], 1)
    n_blk = n_s // MLA_BLOCK
    qb = q.reshape(n_b, n_blk, MLA_BLOCK, MLA_HEADS, dk).swapaxes(0, 1)

    def block(qj):
        s = jnp.einsum('bqhd,bkhd->bhqk', qj, k_all, preferred_element_type=jnp.float32)
        p = jax.nn.softmax(s, axis=-1)
        return jnp.einsum('bhqk,bkhd->bqhd', p.astype(v_all.dtype), v_all)

    o = lax.map(block, qb).swapaxes(0, 1).reshape(n_b, n_s, MLA_HEADS * MLA_V)
    out = o @ w_out
    if not ctx_out:
        return out, None
    s_cc = jnp.einsum('bqhd,bkhd->bhqk', qc, kc, preferred_element_type=jnp.float32)
    oc = jnp.einsum('bhqk,bkhd->bqhd', jax.nn.softmax(s_cc, axis=-1).astype(vc.dtype), vc)
    return out, oc.reshape(n_b, hc.shape[1], MLA_HEADS * MLA_V) @ w_out


def grouped_expert_ffn(h, expert, gate, w1, w3, w2):
    n_tok = h.shape[0]
    n_asg = n_tok * MOE_TOPK
    flat = expert.reshape(-1)
    order = jnp.argsort(flat)
    sorted_e = flat[order]
    counts = jnp.bincount(flat, length=MOE_EXPERTS)
    padded = (counts + MOE_BLOCK - 1) // MOE_BLOCK * MOE_BLOCK
    pad_end = jnp.cumsum(padded)
    pad_start = pad_end - padded
    start = jnp.cumsum(counts) - counts
    dest = pad_start[sorted_e] + jnp.arange(n_asg) - start[sorted_e]
    n_blk = -(-n_asg // MOE_BLOCK) + MOE_EXPERTS
    tok_sorted = (order // MOE_TOPK).astype(jnp.int32)
    src = jnp.full((n_blk * MOE_BLOCK,), n_tok, jnp.int32).at[dest].set(tok_sorted)
    blk_e = jnp.minimum(jnp.searchsorted(pad_end, jnp.arange(n_blk) * MOE_BLOCK, side='right'), MOE_EXPERTS - 1)
    hp = jnp.concatenate([h, jnp.zeros((1, h.shape[1]), h.dtype)], 0)
    xb = hp[src].reshape(n_blk, MOE_BLOCK, h.shape[1])

    def expert_block(args):
        xe, e = args
        return (jax.nn.silu(xe @ w1[e]) * (xe @ w3[e])) @ w2[e]

    yb = lax.map(expert_block, (xb, blk_e)).reshape(n_blk * MOE_BLOCK, h.shape[1])
    contrib = yb[dest].astype(jnp.float32) * gate.reshape(-1)[order][:, None]
    return jax.ops.segment_sum(contrib, tok_sorted, num_segments=n_tok).astype(h.dtype)


def hier_moe(h, w_group, b_group, w_expert, b_expert, w1, w3, w2):
    n_tok = h.shape[0]
    rows = jnp.arange(n_tok)
    lg = jnp.dot(h, w_group, preferred_element_type=jnp.float32) + b_group.astype(jnp.float32)
    grp = jnp.argmax(lg, axis=-1)
    p_grp = jax.nn.softmax(lg, axis=-1)[rows, grp][:, None]
    le = jnp.dot(h, w_expert, preferred_element_type=jnp.float32) + b_expert.astype(jnp.float32)
    le = le.reshape(n_tok, MOE_GROUPS, MOE_PER_GROUP)[rows, grp]
    top_p, top_i = lax.top_k(jax.nn.softmax(le, axis=-1), MOE_TOPK)
    gate = p_grp * top_p / jnp.sum(top_p, axis=-1, keepdims=True)
    expert = grp[:, None].astype(jnp.int32) * MOE_PER_GROUP + top_i.astype(jnp.int32)
    return grouped_expert_ffn(h, expert, gate, w1, w3, w2)


def setup_inputs(seed: int = 0) -> dict:
    key = jax.random.key(seed)
    keys = iter(jax.random.split(key, 96))

    def nrm(shape, std):
        return std * jax.random.normal(next(keys), shape, jnp.float32)

    def gain(shape):
        return 1.0 + nrm(shape, 0.02)

    def unif(shape, lo, hi):
        return jax.random.uniform(next(keys), shape, jnp.float32, lo, hi)

    n_a, n_b, n_c, n_d = [len(range(m, DEPTH, N_MIXERS)) for m in range(N_MIXERS)]
    dm = D_MODEL
    swa_cols = (SWA_HEADS + 2 * SWA_KV_HEADS) * SWA_HEAD_DIM
    gn = SSD_GROUPS * SSD_STATE
    ssd_cols = 2 * SSD_D_INNER + 2 * gn + 2 * SSD_HEADS
    dt0 = jnp.exp(unif((n_b, 2, SSD_HEADS), math.log(1e-3), math.log(1e-1)))
    mla_dk = MLA_NOPE + MLA_ROPE
    s5_shape = (n_c, 2, S5_NGROUPS, S5_STATE)
    return {
        'x': nrm((BATCH, SEQ, dm), 1.0),
        'c': nrm((BATCH, dm), 1.0),
        'ctx': nrm((BATCH, CTX_LEN, dm), 1.0),
        'c_ctx': nrm((dm,), 1.0),
        'mod_down': nrm((DEPTH, dm, MOD_RANK), dm ** -0.5),
        'mod_up': nrm((DEPTH, MOD_RANK, 6 * dm), 0.5 * MOD_RANK ** -0.5),
        'mod_b': nrm((DEPTH, 6 * dm), 0.02),
        'norm1_g': gain((DEPTH, dm)),
        'norm2_g': gain((DEPTH, dm)),
        'swa_w_in': nrm((n_a, dm, swa_cols), dm ** -0.5),
        'swa_q_g': gain((n_a, SWA_HEAD_DIM)),
        'swa_k_g': gain((n_a, SWA_HEAD_DIM)),
        'swa_sinks': nrm((n_a, SWA_HEADS), 1.0),
        'swa_w_out': nrm((n_a, SWA_HEADS * SWA_HEAD_DIM, dm), (SWA_HEADS * SWA_HEAD_DIM) ** -0.5),
        'ssd_w_in': nrm((n_b, dm, ssd_cols), dm ** -0.5),
        'ssd_conv_w': nrm((n_b, SSD_CONV, SSD_D_INNER + 2 * gn), SSD_CONV ** -0.5),
        'ssd_conv_b': nrm((n_b, SSD_D_INNER + 2 * gn), 0.02),
        'ssd_dt_bias': dt0 + jnp.log(-jnp.expm1(-dt0)),
        'ssd_a_log': jnp.log(unif((n_b, 2, SSD_HEADS), 1.0, 16.0)),
        'ssd_d': 1.0 + nrm((n_b, SSD_HEADS), 0.1),
        'ssd_norm_g': gain((n_b, SSD_D_INNER)),
        'ssd_w_out': nrm((n_b, SSD_D_INNER, dm), SSD_D_INNER ** -0.5),
        's5_w_in': nrm((n_c, dm, S5_WIDTH), dm ** -0.5),
        's5_lam_re': -0.5 + nrm(s5_shape, 0.01),
        's5_lam_im': math.pi * jnp.arange(S5_STATE, dtype=jnp.float32) + nrm(s5_shape, 0.01),
        's5_log_dt': unif((n_c, 2, S5_NGROUPS), math.log(1e-3), math.log(1e-1)),
        's5_b_re': nrm((n_c, S5_NGROUPS, S5_STATE, S5_GROUP), (2 * S5_GROUP) ** -0.5),
        's5_b_im': nrm((n_c, S5_NGROUPS, S5_STATE, S5_GROUP), (2 * S5_GROUP) ** -0.5),
        's5_c_re': nrm((n_c, S5_NGROUPS, S5_GROUP, S5_STATE), S5_STATE ** -0.5),
        's5_c_im': nrm((n_c, S5_NGROUPS, S5_GROUP, S5_STATE), S5_STATE ** -0.5),
        's5_d': nrm((n_c, S5_WIDTH), 1.0),
        's5_w_glu': nrm((n_c, S5_WIDTH, 2 * dm), S5_WIDTH ** -0.5),
        'mla_w_in': nrm((n_d, dm, MLA_Q_RANK + MLA_KV_RANK + MLA_ROPE), dm ** -0.5),
        'mla_q_a_g': gain((n_d, MLA_Q_RANK)),
        'mla_kv_a_g': gain((n_d, MLA_KV_RANK)),
        'mla_w_uq': nrm((n_d, MLA_Q_RANK, MLA_HEADS * mla_dk), MLA_Q_RANK ** -0.5),
        'mla_w_ukv': nrm((n_d, MLA_KV_RANK, MLA_HEADS * (MLA_NOPE + MLA_V)), MLA_KV_RANK ** -0.5),
        'mla_q_g': gain((n_d, mla_dk)),
        'mla_k_g': gain((n_d, mla_dk)),
        'mla_w_out': nrm((n_d, MLA_HEADS * MLA_V, dm), (MLA_HEADS * MLA_V) ** -0.5),
        'moe_w_group': nrm((DEPTH, dm, MOE_GROUPS), dm ** -0.5),
        'moe_b_group': nrm((DEPTH, MOE_GROUPS), 0.01),
        'moe_w_expert': nrm((DEPTH, dm, MOE_EXPERTS), dm ** -0.5),
        'moe_b_expert': nrm((DEPTH, MOE_EXPERTS), 0.01),
        'moe_w1': nrm((DEPTH, MOE_EXPERTS, dm, MOE_HIDDEN), dm ** -0.5),
        'moe_w3': nrm((DEPTH, MOE_EXPERTS, dm, MOE_HIDDEN), dm ** -0.5),
        'moe_w2': nrm((DEPTH, MOE_EXPERTS, MOE_HIDDEN, dm), MOE_HIDDEN ** -0.5),
    }


def reference(x, c, ctx, c_ctx, mod_down, mod_up, mod_b, norm1_g, norm2_g,
              swa_w_in, swa_q_g, swa_k_g, swa_sinks, swa_w_out,
              ssd_w_in, ssd_conv_w, ssd_conv_b, ssd_dt_bias, ssd_a_log, ssd_d, ssd_norm_g, ssd_w_out,
              s5_w_in, s5_lam_re, s5_lam_im, s5_log_dt, s5_b_re, s5_b_im, s5_c_re, s5_c_im, s5_d, s5_w_glu,
              mla_w_in, mla_q_a_g, mla_kv_a_g, mla_w_uq, mla_w_ukv, mla_q_g, mla_k_g, mla_w_out,
              moe_w_group, moe_b_group, moe_w_expert, moe_b_expert, moe_w1, moe_w3, moe_w2):
    n_s = x.shape[1]
    xc = ctx
    for i in range(DEPTH):
        kind, slot = i % N_MIXERS, i // N_MIXERS
        last = i == DEPTH - 1
        ml = adaln(c, mod_down[i], mod_up[i], mod_b[i])
        mc = adaln(c_ctx, mod_down[i], mod_up[i], mod_b[i])
        h = modulate(x, norm1_g[i], ml[:, 0, None], ml[:, 1, None])
        hc = modulate(xc, norm1_g[i], mc[0], mc[1])
        if kind == 0:
            o, oc = mixer_swa(h, hc, swa_w_in[slot], swa_q_g[slot], swa_k_g[slot], swa_sinks[slot],
                              swa_w_out[slot], not last)
        elif kind == 1:
            o, oc = mixer_ssd(h, hc, ssd_w_in[slot], ssd_conv_w[slot], ssd_conv_b[slot], ssd_dt_bias[slot],
                              ssd_a_log[slot], ssd_d[slot], ssd_norm_g[slot], ssd_w_out[slot], not last)
        elif kind == 2:
            o, oc = mixer_s5(h, hc, s5_w_in[slot], s5_lam_re[slot], s5_lam_im[slot], s5_log_dt[slot],
                             s5_b_re[slot], s5_b_im[slot], s5_c_re[slot], s5_c_im[slot], s5_d[slot],
                             s5_w_glu[slot], not last)
        else:
            o, oc = mixer_mla(h, hc, mla_w_in[slot], mla_q_a_g[slot], mla_kv_a_g[slot], mla_w_uq[slot],
                              mla_w_ukv[slot], mla_q_g[slot], mla_k_g[slot], mla_w_out[slot], not last)
        x = x + ml[:, 2, None] * o
        h = modulate(x, norm2_g[i], ml[:, 3, None], ml[:, 4, None])
        moe_p = (moe_w_group[i], moe_b_group[i], moe_w_expert[i], moe_b_expert[i], moe_w1[i], moe_w3[i], moe_w2[i])
        if last:
            y = hier_moe(h.reshape(-1, D_MODEL), *moe_p).reshape(h.shape)
        else:
            xc = xc + mc[2] * oc
            hc = modulate(xc, norm2_g[i], mc[3], mc[4])
            tok = jnp.concatenate([h, hc], 1)
            yt = hier_moe(tok.reshape(-1, D_MODEL), *moe_p).reshape(tok.shape)
            y = yt[:, :n_s]
            xc = xc + mc[5] * yt[:, n_s:]
        x = x + ml[:, 5, None] * y
    return x
```

```python
import os
import numpy as np
from contextlib import ExitStack
import concourse.bass as bass
import concourse.mybir as mybir
from concourse.bass_utils import run_bass_kernel_spmd

F32 = mybir.dt.float32
BF16 = mybir.dt.bfloat16
I32 = mybir.dt.int32
ALU = mybir.AluOpType
AF = mybir.ActivationFunctionType
AX = mybir.AxisListType

D = 4096
NL = 2048
NCX = 256
NT = NL + NCX
NTT = NT // 128
KC = D // 128
EPS = 1e-6
N_DMA_SEMS = 24


class Prog:
    def __init__(self, nc):
        self.nc = nc
        self.ops = []
        self.st = {}
        self.bar = {}
        self.out_dmas = []
        self.psum = set()

    def _prune(self, lst):
        last = {}
        out = []
        for o in lst:
            op = self.ops[o]
            if op[3]:
                out.append(o)
            else:
                last[op[0]] = o
        return out + list(last.values())

    def add(self, eng, fn, reads=(), writes=(), dma=False, is_out=False):
        oid = len(self.ops)
        deps = set()
        reads = [k.name if hasattr(k, "name") else k for k in reads]
        writes = [k.name if hasattr(k, "name") else k for k in writes]
        writes = writes + [k for k in reads if k in self.psum and k not in writes]
        for k in reads:
            w, r, pv = self.st.setdefault(k, [[], [], []])
            deps.update(w)
        for k in writes:
            w, r, pv = self.st.setdefault(k, [[], [], []])
            if r:
                deps.update(w)
                deps.update(r)
            else:
                deps.update(pv)
                deps.update(o for o in w if not (eng == "pe" and self.ops[o][0] == "pe") and not (dma and self.ops[o][3]))
        if eng in self.bar:
            deps.update(self.bar.pop(eng))
        self.ops.append([eng, fn, deps, dma, False, (tuple(reads), tuple(writes))])
        for k in reads:
            s = self.st[k]
            s[1].append(oid)
            s[1] = self._prune(s[1])
        for k in writes:
            s = self.st[k]
            if s[1]:
                s[2] = self._prune(list(s[0]) + [x for x in s[1] if x != oid])
                s[0] = [oid]
                s[1] = []
            else:
                s[0].append(oid)
                s[0] = self._prune(s[0])
        deps.discard(oid)
        if is_out:
            self.out_dmas.append(oid)
        return oid

    def barrier(self, engs=None):
        last = {}
        dmas = []
        for i, op in enumerate(self.ops):
            if op[3]:
                dmas.append(i)
            else:
                last[op[0]] = i
        start = getattr(self, "_bar_start", 0)
        dmas = [d for d in dmas if d >= start]
        self._bar_start = len(self.ops)
        deps = set(dmas) | set(last.values())
        for e in (engs or ("pe", "act", "dve", "pool", "sp")):
            self.bar[e] = set(deps) | self.bar.get(e, set())
        if engs is None:
            self.st = {}

    def dma(self, out, in_, reads=(), writes=(), eng="sp", is_out=False, slow=False):
        kw = {"allow_slow_non_contiguous": True} if slow else {}
        return self.add(eng, lambda e: e.dma_start(out=out, in_=in_, **kw), reads, writes, dma=True, is_out=is_out)

    def emit(self):
        nc = self.nc
        ops = self.ops
        for op in ops:
            for d in op[2]:
                ops[d][4] = True
        for d in self.out_dmas:
            ops[d][4] = True
        engs = ["pe", "act", "dve", "pool", "sp"]
        with ExitStack() as es:
            csem = {e: es.enter_context(nc.semaphore("c_" + e)) for e in engs}
            dsem = [es.enter_context(nc.semaphore("d%d" % i)) for i in range(N_DMA_SEMS)]
            sig = {}
            ccount = {e: 0 for e in engs}
            duse = [0] * N_DMA_SEMS
            nd = 0
            dprev = {}
            nsw = 0
            nhw = 0
            for i, op in enumerate(ops):
                if op[3]:
                    if op[0] == "pool":
                        s = nsw % 8
                        nsw += 1
                    else:
                        s = 8 + nhw % (N_DMA_SEMS - 8)
                        nhw += 1
                    nd += 1
                    duse[s] += 1
                    sig[i] = (("d", s), 16 * duse[s])
                    op[4] = True
                elif op[4]:
                    ccount[op[0]] += 1
                    sig[i] = (("c", op[0]), ccount[op[0]])
            self.stats = (dict(ccount), nd)

            def semobj(k):
                return csem[k[1]] if k[0] == "c" else dsem[k[1]]

            final_waits = [sig[d] for d in self.out_dmas]

            def run(engname, e):
                waited = {}

                def wait(k, v):
                    if waited.get(k, 0) < v:
                        e.wait_ge(semobj(k), v)
                        waited[k] = v
                for i, op in enumerate(ops):
                    if op[0] != engname:
                        continue
                    for d in sorted(op[2]):
                        k, v = sig[d]
                        wait(k, v)
                    if op[3]:
                        k, v = sig[i]
                        if v > 16:
                            wait(k, v - 16)
                    ins = op[1](e)
                    if op[4]:
                        k, v = sig[i]
                        ins.then_inc(semobj(k), 16 if op[3] else 1)
                if engname == "sp":
                    for k, v in final_waits:
                        wait(k, v)

            with nc.Block() as block:
                @block.tensor
                def _(e):
                    run("pe", e)

                @block.scalar
                def _(e):
                    run("act", e)

                @block.vector
                def _(e):
                    run("dve", e)

                @block.gpsimd
                def _(e):
                    run("pool", e)

                @block.sync
                def _(e):
                    run("sp", e)


class Ctx:
    def __init__(self, nc):
        self.nc = nc
        self.P = Prog(nc)
        self.n = 0
        self.dram = {}

    def name(self, s):
        self.n += 1
        return "%s_%d" % (s, self.n)

    def sb(self, es, shape, dt, name="t"):
        return es.enter_context(self.nc.sbuf_tensor(self.name(name), list(shape), dt))

    def ps(self, es, shape, dt=F32, name="ps"):
        t = es.enter_context(self.nc.psum_tensor(self.name(name), list(shape), dt))
        self.P.psum.add(t.name)
        return t

    def dr(self, name, shape, dt, kind="Internal"):
        t = self.nc.dram_tensor(name, list(shape), dt, kind=kind)
        self.dram[name] = t
        return t


def pass_adaln(C, c_b, c_ctx, mod_down, mod_up, mod_b, modv, ident, layers=range(4)):
    P = C.P
    nc = C.nc
    with ExitStack() as es:
        cT = C.sb(es, [128, 2, KC], F32, "cT")
        sT = C.sb(es, [128, 2, KC], F32, "sT")
        L = C.sb(es, [128, KC, 128], F32, "L")
        idt = C.sb(es, [128, 128], F32, "idt")
        wd = [C.sb(es, [128, 8, 512], F32, "wd") for _ in range(2)]
        tsb = C.sb(es, [128, 512], F32, "tsb")
        tT = C.sb(es, [128, 4, 128], F32, "tT")
        wu = [C.sb(es, [128, 4, 512], F32, "wu") for _ in range(3)]
        bb = [C.sb(es, [128, 512], F32, "bb") for _ in range(3)]
        osb = [C.sb(es, [128, 512], F32, "osb") for _ in range(3)]
        pdn = C.ps(es, [128, 512], F32, "pdn")
        ptr = C.ps(es, [128, 512], F32, "ptr")
        pup = [C.ps(es, [128, 512], F32, "pup") for _ in range(2)]

        P.dma(idt[:, :], ident.ap(), writes=[idt])
        P.dma(cT[:, 0, :], c_b.ap().rearrange("(k p) -> p k", p=128), writes=[cT], slow=True)
        P.dma(cT[:, 1, :], c_ctx.ap().rearrange("(k p) -> p k", p=128), writes=[cT], slow=True)
        P.add("act", lambda e: e.activation(out=sT[:, :, :], in_=cT[:, :, :], func=AF.Silu), reads=[cT], writes=[sT])
        P.add("dve", lambda e: e.tensor_copy(out=L[:, :, 0:64], in_=sT[:, 0, :, None].to_broadcast([128, KC, 64])),
              reads=[sT], writes=[L])
        P.add("dve", lambda e: e.tensor_copy(out=L[:, :, 64:128], in_=sT[:, 1, :, None].to_broadcast([128, KC, 64])),
              reads=[sT], writes=[L])
        nw = 0
        nu = 0
        for l in layers:
            for q in range(4):
                w = wd[nw % 2]
                nw += 1
                P.dma(w[:, :, :], mod_down.ap()[l, q * 1024:(q + 1) * 1024, :].rearrange("(k p) r -> p k r", p=128),
                      writes=[w])
                for kk in range(8):
                    k = q * 8 + kk
                    P.add("pe", lambda e, w=w, kk=kk, k=k: e.matmul(pdn[:, :], lhsT=L[:, k, :], rhs=w[:, kk, :],
                                                                      start=(k == 0), stop=(k == KC - 1)),
                          reads=[L, w], writes=[pdn])
            P.add("dve", lambda e: e.tensor_copy(out=tsb[:, :], in_=pdn[:, :]), reads=[pdn], writes=[tsb])
            for j in range(4):
                P.add("pe", lambda e, j=j: e.transpose(out=ptr[:, j * 128:(j + 1) * 128], in_=tsb[:, j * 128:(j + 1) * 128],
                                                       identity=idt[:, :]),
                      reads=[tsb, idt], writes=[ptr])
            P.add("dve", lambda e: e.tensor_copy(out=tT[:, :, :], in_=ptr[:, :].rearrange("p (j m) -> p j m", j=4)),
                  reads=[ptr], writes=[tT])
            for n in range(48):
                w = wu[nu % 3]
                b = bb[nu % 3]
                o = osb[nu % 3]
                pp = pup[nu % 2]
                nu += 1
                P.dma(w[:, :, :], mod_up.ap()[l, :, n * 512:(n + 1) * 512].rearrange("(k p) c -> p k c", p=128),
                      writes=[w])
                P.dma(b[:, :], mod_b.ap()[l, n * 512:(n + 1) * 512].partition_broadcast(128), writes=[b])
                for k in range(4):
                    P.add("pe", lambda e, w=w, k=k, pp=pp: e.matmul(pp[:, :], lhsT=tT[:, k, :], rhs=w[:, k, :],
                                                                    start=(k == 0), stop=(k == 3)),
                          reads=[tT, w], writes=[pp])
                P.add("dve", lambda e, o=o, pp=pp, b=b: e.tensor_tensor(out=o[:, :], in0=pp[:, :], in1=b[:, :], op=ALU.add),
                      reads=[pp, b], writes=[o])
                P.dma(modv.ap()[l, 0, n * 512:(n + 1) * 512], o[0:1, :], reads=[o], writes=[("modv", l)])
                P.dma(modv.ap()[l, 1, n * 512:(n + 1) * 512], o[64:65, :], reads=[o], writes=[("modv", l)])
    P.barrier()


def pass_norm(C, xres, modv, l, gvec, i_shift, i_scale, hT, ident_bf, tiles=range(NTT)):
    P = C.P
    with ExitStack() as es:
        idb = C.sb(es, [128, 128], BF16, "idb")
        g = C.sb(es, [128, D], F32, "g")
        A = [C.sb(es, [128, D], F32, "A") for _ in range(2)]
        B = [C.sb(es, [128, D], F32, "B") for _ in range(2)]
        xt = [C.sb(es, [128, D], F32, "xt") for _ in range(2)]
        junk = C.sb(es, [128, D], BF16, "junk")
        hb = [C.sb(es, [128, D], BF16, "hb") for _ in range(2)]
        st = [C.sb(es, [128, 4], F32, "st") for _ in range(2)]
        hst = [C.sb(es, [128, KC, 128], BF16, "hst") for _ in range(2)]
        ptr = [C.ps(es, [128, 1024], BF16, "ptr") for _ in range(2)]
        P.dma(idb[:, :], ident_bf.ap(), writes=[idb])
        P.dma(g[:, :], gvec.partition_broadcast(128), writes=[g])
        for s in range(2):
            P.dma(A[s][:, :], modv.ap()[l, s, i_scale * D:(i_scale + 1) * D].partition_broadcast(128),
                  reads=[("modv", l)], writes=[A[s]])
            P.dma(B[s][:, :], modv.ap()[l, s, i_shift * D:(i_shift + 1) * D].partition_broadcast(128),
                  reads=[("modv", l)], writes=[B[s]])
            P.add("dve", lambda e, s=s: e.scalar_tensor_tensor(out=A[s][:, :], in0=A[s][:, :], scalar=1.0, in1=g[:, :],
                                                               op0=ALU.add, op1=ALU.mult),
                  reads=[A[s], g], writes=[A[s]])
        n = 0
        npt = 0
        for i in tiles:
            s = 0 if i < NL // 128 else 1
            x = xt[n % 2]
            h = hb[n % 2]
            sv = st[n % 2]
            hs = hst[n % 2]
            n += 1
            P.dma(x[:, :], xres.ap()[i * 128:(i + 1) * 128, :], reads=[("xres", i)], writes=[x])
            P.add("act", lambda e, x=x, sv=sv: e.activation(out=junk[:, :], in_=x[:, :], func=AF.Square,
                                                             accum_out=sv[:, 0:1]),
                  reads=[x], writes=[junk, sv])
            P.add("dve", lambda e, sv=sv: e.tensor_scalar(out=sv[:, 1:2], in0=sv[:, 0:1], scalar1=1.0 / D, scalar2=EPS,
                                                          op0=ALU.mult, op1=ALU.add), reads=[sv], writes=[sv])
            P.add("act", lambda e, sv=sv: e.activation(out=sv[:, 3:4], in_=sv[:, 1:2], func=AF.Sqrt), reads=[sv], writes=[sv])
            P.add("dve", lambda e, sv=sv: e.reciprocal(out=sv[:, 2:3], in_=sv[:, 3:4]), reads=[sv], writes=[sv])
            P.add("dve", lambda e, x=x, sv=sv, s=s: e.scalar_tensor_tensor(out=x[:, :], in0=x[:, :], scalar=sv[:, 2:3],
                                                                           in1=A[s][:, :], op0=ALU.mult, op1=ALU.mult),
                  reads=[x, sv, A[s]], writes=[x])
            P.add("pool", lambda e, x=x, h=h, s=s: e.tensor_tensor(out=h[:, :], in0=x[:, :], in1=B[s][:, :], op=ALU.add),
                  reads=[x, B[s]], writes=[h])
            for q in range(4):
                pt = ptr[npt % 2]
                npt += 1
                for kk in range(8):
                    k = q * 8 + kk
                    P.add("pe", lambda e, pt=pt, kk=kk, k=k, h=h: e.transpose(out=pt[:, kk * 128:(kk + 1) * 128],
                                                                             in_=h[:, k * 128:(k + 1) * 128],
                                                                             identity=idb[:, :]),
                          reads=[h, idb], writes=[pt])
                P.add("act", lambda e, pt=pt, hs=hs, q=q: e.copy(out=hs[:, q * 8:(q + 1) * 8, :],
                                                                  in_=pt[:, :].rearrange("p (k t) -> p k t", k=8)),
                      reads=[pt], writes=[hs])
            P.dma(hT.ap()[i], hs[:, :, :], reads=[hs], writes=[("hT", i)])
    P.barrier()


def load_act(C, dst, src, t0, nt, key):
    for i in range(nt):
        C.P.dma(dst[:, i, :, :], src.ap()[t0 + i], reads=[(key, t0 + i)], writes=[dst])


def gemm_ws(C, es, act, nt, kcx, W, col0, ncols, epi, wname, nsub=3, wbufs=3, psb=None):
    P = C.P
    wt = [C.sb(es, [128, kcx, 128], BF16, "wws") for _ in range(wbufs)]
    ps = psb if psb is not None else [C.ps(es, [128, 512], F32, "gps") for _ in range(2)]
    nps = 0
    for j in range((ncols + 127) // 128):
        w = wt[j % wbufs]
        c = col0 + j * 128
        cw = min(128, ncols - j * 128)
        P.dma(w[:, :, 0:cw], W[:, c:c + cw].rearrange("(k p) c -> p k c", p=128), writes=[w], eng="pool")
        for tt0 in range(0, nt, nsub):
            ntl = min(nsub, nt - tt0)
            pp = ps[nps % len(ps)]
            nps += 1
            for k in range(kcx):
                P.add("pe", lambda e, pp=pp, w=w, k=k, tt0=tt0, ntl=ntl, cw=cw: e.matmul(
                    pp[0:cw, 0:ntl * 128], lhsT=w[:, k, 0:cw], rhs=act[:, tt0:tt0 + ntl, k, :],
                    start=(k == 0), stop=(k == kcx - 1)), reads=[w, act], writes=[pp])
            epi(pp, j, tt0, ntl)


def gemm_ts(C, es, act, nt, kcx, W, col0, ncols, epi, cb=512, wbufs=2, psb=None):
    P = C.P
    wt = [C.sb(es, [128, kcx, cb], BF16, "wts") for _ in range(wbufs)]
    ps = psb if psb is not None else [C.ps(es, [128, 512], F32, "gps") for _ in range(2)]
    nps = 0
    for j in range(ncols // cb):
        w = wt[j % wbufs]
        c = col0 + j * cb
        P.dma(w[:, :, :], W[:, c:c + cb].rearrange("(k p) c -> p k c", p=128), writes=[w], eng="pool")
        for ti in range(nt):
            pp = ps[nps % len(ps)]
            nps += 1
            for k in range(kcx):
                P.add("pe", lambda e, pp=pp, w=w, k=k, ti=ti: e.matmul(
                    pp[:, 0:cb], lhsT=act[:, ti, k, :], rhs=w[:, k, :],
                    start=(k == 0), stop=(k == kcx - 1)), reads=[w, act], writes=[pp])
            epi(pp, ti, j * cb)


def rms_rstd(P, e_src_ps, ones_bf, sq, pss, rs, n, src_reads, eps=EPS):
    P.add("act", lambda e: e.activation(out=sq[:, 0:n], in_=e_src_ps, func=AF.Square), reads=src_reads, writes=[sq])
    P.add("pe", lambda e: e.matmul(pss[:, 0:n], lhsT=ones_bf, rhs=sq[:, 0:n], start=True, stop=True),
          reads=[sq], writes=[pss])
    P.add("dve", lambda e: e.tensor_scalar(out=rs[:, 0:n], in0=pss[:, 0:n], scalar1=eps, scalar2=None, op0=ALU.add),
          reads=[pss], writes=[rs])
    P.add("act", lambda e: e.activation(out=rs[:, 0:n], in_=rs[:, 0:n], func=AF.Sqrt), reads=[rs], writes=[rs])
    P.add("dve", lambda e: e.reciprocal(out=rs[:, 0:n], in_=rs[:, 0:n]), reads=[rs], writes=[rs])


def pass_swa_proj(C, hT, w_in, q_g, k_g, cosT, sinT, rotm, qT, Vd):
    P = C.P
    scale = 128 ** -0.5
    def do_block(blk):
        with ExitStack() as es:
            act = C.sb(es, [128, 9, KC, 128], BF16, "act")
            cs = C.sb(es, [128, 1152], F32, "cs")
            sn = C.sb(es, [128, 1152], F32, "sn")
            rm = C.sb(es, [128, 128], BF16, "rm")
            ones = C.sb(es, [128, 128], BF16, "ones")
            gq = C.sb(es, [128, 2], F32, "gq")
            sq = [C.sb(es, [128, 384], BF16, "sq") for _ in range(2)]
            rs = [C.sb(es, [128, 384], F32, "rs") for _ in range(2)]
            qn = [C.sb(es, [128, 384], BF16, "qn") for _ in range(2)]
            t1 = [C.sb(es, [128, 384], F32, "t1") for _ in range(2)]
            qo = [C.sb(es, [128, 384], BF16, "qo") for _ in range(2)]
            vo = [C.sb(es, [128, 512], BF16, "vo") for _ in range(2)]
            pss = [C.ps(es, [128, 512], F32, "pss") for _ in range(2)]
            prr = [C.ps(es, [128, 512], F32, "prr") for _ in range(2)]
            gps = [C.ps(es, [128, 512], F32, "gps") for _ in range(2)]
            load_act(C, act, hT, blk * 9, 9, "hT")
            P.dma(cs[:, :], cosT.ap()[:, blk * 1152:(blk + 1) * 1152], writes=[cs])
            P.dma(sn[:, :], sinT.ap()[:, blk * 1152:(blk + 1) * 1152], writes=[sn])
            P.dma(rm[:, :], rotm.ap(), writes=[rm])
            P.dma(gq[:, 0:1], q_g.rearrange("(p o) -> p o", o=1), writes=[gq])
            P.dma(gq[:, 1:2], k_g.rearrange("(p o) -> p o", o=1), writes=[gq])
            P.add("dve", lambda e: e.tensor_scalar(out=gq[:, 0:1], in0=gq[:, 0:1], scalar1=scale, scalar2=None, op0=ALU.mult),
                  reads=[gq], writes=[gq])
            P.add("pool", lambda e: e.memset(ones[:, :], 1.0 / 128), writes=[ones])
            cnt = [0]

            def epi(pp, j, tt0, ntl):
                n = ntl * 128
                c = cnt[0] % 2
                cnt[0] += 1
                gi = 0 if j < 32 else 1
                rms_rstd(P, pp[:, 0:n], ones[:, :], sq[c], pss[c], rs[c], n, [pp])
                P.add("dve", lambda e: e.scalar_tensor_tensor(out=qn[c][:, 0:n], in0=pp[:, 0:n], scalar=gq[:, gi:gi + 1],
                                                              in1=rs[c][:, 0:n], op0=ALU.mult, op1=ALU.mult),
                      reads=[pp, gq, rs[c]], writes=[qn[c]])
                P.add("pe", lambda e: e.matmul(prr[c][:, 0:n], lhsT=rm[:, :], rhs=qn[c][:, 0:n], start=True, stop=True),
                      reads=[rm, qn[c]], writes=[prr[c]])
                P.add("pool", lambda e: e.tensor_tensor(out=t1[c][:, 0:n], in0=qn[c][:, 0:n], in1=cs[:, tt0 * 128:tt0 * 128 + n],
                                                       op=ALU.mult), reads=[qn[c], cs], writes=[t1[c]])
                P.add("dve", lambda e: e.tensor_tensor(out=rs[c][:, 0:n], in0=prr[c][:, 0:n], in1=sn[:, tt0 * 128:tt0 * 128 + n],
                                                      op=ALU.mult), reads=[prr[c], sn, rs[c]], writes=[rs[c]])
                P.add("dve", lambda e: e.tensor_tensor(out=qo[c][:, 0:n], in0=t1[c][:, 0:n], in1=rs[c][:, 0:n], op=ALU.add),
                      reads=[t1[c], rs[c]], writes=[qo[c]])
                tg = (blk * 9 + tt0) * 128
                P.dma(qT.ap()[j, :, tg:tg + n], qo[c][:, 0:n], reads=[qo[c]], writes=[("qT", j)])

            gemm_ws(C, es, act, 9, KC, w_in, 0, 40 * 128, epi, "w", psb=gps)
            vc = [0]

            def epv(pp, ti, c0):
                c = vc[0] % 2
                vc[0] += 1
                P.add("act", lambda e: e.copy(out=vo[c][:, :], in_=pp[:, 0:512]), reads=[pp], writes=[vo[c]])
                tg = (blk * 9 + ti) * 128
                P.dma(Vd.ap()[tg:tg + 128, c0:c0 + 512], vo[c][:, :], reads=[vo[c]], writes=[("Vd", c0)])

            gemm_ts(C, es, act, 9, KC, w_in, 40 * 128, 1024, epv, psb=gps)
        P.barrier()
    for blk_ in range(2):
        do_block(blk_)


def attn_step(P, terms, sc, pt, mask, vap, ones, ov, dn, first, last, rd, n=512):
    for ti, (lt, rh) in enumerate(terms):
        P.add("pe", lambda e, lt=lt, rh=rh, ti=ti: e.matmul(sc[:, 0:n], lhsT=lt, rhs=rh, start=(ti == 0),
                                                            stop=(ti == len(terms) - 1)), reads=rd, writes=[sc])
    P.add("act", lambda e: e.activation(out=pt[:, 0:n], in_=sc[:, 0:n], func=AF.Exp), reads=[sc], writes=[pt])
    if mask is not None:
        P.add("pool", lambda e: e.tensor_tensor(out=pt[:, 0:n], in0=pt[:, 0:n], in1=mask, op=ALU.mult),
              reads=[pt], writes=[pt])
    P.add("pe", lambda e: e.matmul(ov[:, 0:n], lhsT=vap, rhs=pt[:, 0:n], start=first, stop=last), reads=rd + [pt], writes=[ov])
    P.add("pe", lambda e: e.matmul(dn[:, 0:n], lhsT=ones, rhs=pt[:, 0:n], start=first, stop=last), reads=[pt], writes=[dn])


def pass_swa_attn(C, qT, Vd, sinks, maskP, maskN, oT):
    P = C.P
    with ExitStack() as es:
        qs = [C.sb(es, [128, 4, NT], BF16, "qs") for _ in range(2)]
        ks = [C.sb(es, [128, NT], BF16, "ks") for _ in range(2)]
        vs = [C.sb(es, [128, NTT, 128], BF16, "vs") for _ in range(2)]
        mP = C.sb(es, [128, 128], BF16, "mP")
        mN = C.sb(es, [128, 128], BF16, "mN")
        ones = C.sb(es, [128, 128], BF16, "ones")
        esk = C.sb(es, [128, 32], F32, "esk")
        pts = [C.sb(es, [128, 512], BF16, "pt") for _ in range(3)]
        den = [C.sb(es, [128, 512], F32, "den") for _ in range(2)]
        ob = [C.sb(es, [128, 512], BF16, "ob") for _ in range(2)]
        scp = [C.ps(es, [128, 512], F32, "sc") for _ in range(3)]
        ovp = [C.ps(es, [128, 512], F32, "ov") for _ in range(2)]
        dnp = [C.ps(es, [128, 512], F32, "dn") for _ in range(2)]
        P.dma(mP[:, :], maskP.ap(), writes=[mP])
        P.dma(mN[:, :], maskN.ap(), writes=[mN])
        P.add("pool", lambda e: e.memset(ones[:, :], 1.0), writes=[ones])
        P.dma(esk[:, :], sinks.partition_broadcast(128), writes=[esk])
        P.add("act", lambda e: e.activation(out=esk[:, :], in_=esk[:, :], func=AF.Exp), reads=[esk], writes=[esk])
        nstep = 0
        nblk = 0
        for g in range(8):
            q = qs[g % 2]
            k = ks[g % 2]
            v = vs[g % 2]
            P.dma(q[:, :, :], qT.ap()[4 * g:4 * g + 4].rearrange("h d t -> d h t"), reads=[("qT", 4 * g + i) for i in range(4)],
                  writes=[q])
            P.dma(k[:, :], qT.ap()[32 + g], reads=[("qT", 32 + g)], writes=[k])
            P.dma(v[:, :, :], Vd.ap()[:, g * 128:(g + 1) * 128].rearrange("(i p) d -> p i d", p=128),
                  reads=[("Vd", 0), ("Vd", 512)], writes=[v])
            for j in range(NTT):
                if j < 16:
                    chunks = [(c, m) for c, m in ((j - 1, mP), (j, None), (j + 1, mN)) if 0 <= c < 16] + [(16, None), (17, None)]
                else:
                    chunks = [(16, None), (17, None)]
                ov = ovp[nblk % 2]
                dn = dnp[nblk % 2]
                dsb = den[nblk % 2]
                o = ob[nblk % 2]
                nblk += 1
                for ci, (c, m) in enumerate(chunks):
                    sc = scp[nstep % 3]
                    pt = pts[nstep % 3]
                    nstep += 1
                    mask = None if m is None else m[:, None, :].to_broadcast([128, 4, 128])
                    ptv = pt[:, :].rearrange("p (h t) -> p h t", h=4) if m is not None else None
                    terms = [(k[:, c * 128:(c + 1) * 128], q[:, :, j * 128:(j + 1) * 128])]
                    P.add("pe", lambda e, sc=sc, terms=terms: e.matmul(sc[:, :], lhsT=terms[0][0], rhs=terms[0][1],
                                                                       start=True, stop=True), reads=[k, q], writes=[sc])
                    P.add("act", lambda e, sc=sc, pt=pt: e.activation(out=pt[:, :], in_=sc[:, :], func=AF.Exp),
                          reads=[sc], writes=[pt])
                    if m is not None:
                        P.add("pool", lambda e, ptv=ptv, mask=mask: e.tensor_tensor(out=ptv, in0=ptv, in1=mask, op=ALU.mult),
                              reads=[pt, m], writes=[pt])
                    fst = ci == 0
                    lst = ci == len(chunks) - 1
                    P.add("pe", lambda e, ov=ov, v=v, c=c, pt=pt, fst=fst, lst=lst: e.matmul(
                        ov[:, :], lhsT=v[:, c, :], rhs=pt[:, :], start=fst, stop=lst), reads=[v, pt], writes=[ov])
                    P.add("pe", lambda e, dn=dn, pt=pt, fst=fst, lst=lst: e.matmul(
                        dn[:, :], lhsT=ones[:, :], rhs=pt[:, :], start=fst, stop=lst), reads=[ones, pt], writes=[dn])
                P.add("dve", lambda e, dsb=dsb, dn=dn, g=g: e.tensor_tensor(
                    out=dsb[:, :].rearrange("p (h t) -> p h t", h=4), in0=dn[:, :].rearrange("p (h t) -> p h t", h=4),
                    in1=esk[:, 4 * g:4 * g + 4, None].to_broadcast([128, 4, 128]), op=ALU.add), reads=[dn, esk], writes=[dsb])
                P.add("dve", lambda e, dsb=dsb: e.reciprocal(out=dsb[:, :], in_=dsb[:, :]), reads=[dsb], writes=[dsb])
                P.add("dve", lambda e, o=o, ov=ov, dsb=dsb: e.tensor_tensor(out=o[:, :], in0=ov[:, :], in1=dsb[:, :], op=ALU.mult),
                      reads=[ov, dsb], writes=[o])
                P.dma(oT.ap()[j, :, 4 * g:4 * g + 4, :], o[:, :].rearrange("p (h t) -> p h t", h=4), reads=[o],
                      writes=[("oT", j)])
    P.barrier()


def pass_outproj(C, aT, kcx, W, ncols, modv, l, i_gate, xres, nblk_tiles, glu=False, cb=512):
    P = C.P
    def do_block(t0):
        nt = min(nblk_tiles, NTT - t0)
        with ExitStack() as es:
            act = C.sb(es, [128, nt, kcx, 128], BF16, "act")
            G = [C.sb(es, [128, D], F32, "G") for _ in range(2)]
            xs = [C.sb(es, [128, 512], F32, "xs") for _ in range(3)]
            ts = [C.sb(es, [128, 512], F32, "ts") for _ in range(3)]
            load_act(C, act, aT, t0, nt, "aT")
            for s in range(2):
                P.dma(G[s][:, :], modv.ap()[l, s, i_gate * D:(i_gate + 1) * D].partition_broadcast(128),
                      reads=[("modv", l)], writes=[G[s]])
            cnt = [0]
            if not glu:
                def epi(pp, ti, c0):
                    c = cnt[0] % 3
                    cnt[0] += 1
                    ig = t0 + ti
                    s = 0 if ig < 16 else 1
                    P.dma(xs[c][:, 0:cb], xres.ap()[ig * 128:(ig + 1) * 128, c0:c0 + cb], reads=[("xres", ig)], writes=[xs[c]])
                    P.add("dve", lambda e: e.tensor_tensor(out=ts[c][:, 0:cb], in0=pp[:, 0:cb], in1=G[s][:, c0:c0 + cb], op=ALU.mult),
                          reads=[pp, G[s]], writes=[ts[c]])
                    P.add("pool", lambda e: e.tensor_tensor(out=xs[c][:, 0:cb], in0=xs[c][:, 0:cb], in1=ts[c][:, 0:cb], op=ALU.add),
                          reads=[xs[c], ts[c]], writes=[xs[c]])
                    P.dma(xres.ap()[ig * 128:(ig + 1) * 128, c0:c0 + cb], xs[c][:, 0:cb], reads=[xs[c]], writes=[("xres", ig)],
                          eng="act")
                gemm_ts(C, es, act, nt, kcx, W, 0, ncols, epi, cb=cb)
            else:
                wt = [C.sb(es, [128, kcx, 2, cb], BF16, "wglu") for _ in range(2)]
                sg = [C.sb(es, [128, 512], F32, "sg") for _ in range(2)]
                pa = [C.ps(es, [128, 512], F32, "pa") for _ in range(2)]
                pb = [C.ps(es, [128, 512], F32, "pb") for _ in range(2)]
                np_ = [0]

                def do_col(j):
                    w = wt[j % 2]
                    c0 = j * cb
                    for hh in range(2):
                        P.dma(w[:, :, hh, :], W[:, hh * D + c0:hh * D + c0 + cb].rearrange("(k p) c -> p k c", p=128), writes=[w], eng="pool")
                    def do_ti(ti):
                        a_ = pa[np_[0] % 2]
                        b_ = pb[np_[0] % 2]
                        s_ = sg[np_[0] % 2]
                        c = np_[0] % 3
                        np_[0] += 1
                        for (pp, hh) in ((a_, 0), (b_, 1)):
                            for k in range(kcx):
                                P.add("pe", lambda e, pp=pp, hh=hh, k=k, ti=ti: e.matmul(pp[:, 0:cb], lhsT=act[:, ti, k, :], rhs=w[:, k, hh, :],
                                                                                         start=(k == 0), stop=(k == kcx - 1)), reads=[w, act], writes=[pp])
                        ig = t0 + ti
                        s = 0 if ig < 16 else 1
                        P.add("act", lambda e: e.activation(out=s_[:, 0:cb], in_=b_[:, 0:cb], func=AF.Sigmoid), reads=[b_], writes=[s_])
                        P.add("dve", lambda e: e.tensor_tensor(out=s_[:, 0:cb], in0=s_[:, 0:cb], in1=a_[:, 0:cb], op=ALU.mult), reads=[s_, a_], writes=[s_])
                        P.dma(xs[c][:, 0:cb], xres.ap()[ig * 128:(ig + 1) * 128, c0:c0 + cb], reads=[("xres", ig)], writes=[xs[c]])
                        P.add("pool", lambda e: e.tensor_tensor(out=ts[c][:, 0:cb], in0=s_[:, 0:cb], in1=G[s][:, c0:c0 + cb], op=ALU.mult),
                              reads=[s_, G[s]], writes=[ts[c]])
                        P.add("dve", lambda e: e.tensor_tensor(out=xs[c][:, 0:cb], in0=xs[c][:, 0:cb], in1=ts[c][:, 0:cb], op=ALU.add),
                              reads=[xs[c], ts[c]], writes=[xs[c]])
                        P.dma(xres.ap()[ig * 128:(ig + 1) * 128, c0:c0 + cb], xs[c][:, 0:cb], reads=[xs[c]], writes=[("xres", ig)], eng="act")

                    for ti_ in range(nt):
                        do_ti(ti_)

                for j_ in range(ncols // cb):
                    do_col(j_)
        P.barrier()
    for t0_ in range(0, NTT, nblk_tiles):
        do_block(t0_)


def _bf(a):
    import ml_dtypes
    return np.asarray(a, dtype=np.float32).astype(ml_dtypes.bfloat16)


def host_consts():
    c = {}
    eye = np.eye(128, dtype=np.float32)
    c["ident"] = eye
    c["ident_bf"] = _bf(eye)
    kk = np.arange(128)[:, None]
    qq = np.arange(128)[None, :]
    c["maskP"] = _bf((kk >= qq).astype(np.float32))
    c["maskN"] = _bf((kk <= qq).astype(np.float32))

    def rope(rot_dim):
        axis_dim = rot_dim // 2
        half = axis_dim // 2
        inv = (np.float32(10000.0) ** (-np.arange(0, axis_dim, 2, dtype=np.float32) / np.float32(axis_dim))).astype(np.float32)
        t = np.arange(NL)
        pos = [(t // 64).astype(np.float32), (t % 64).astype(np.float32)]
        cs = np.ones((rot_dim, NT), np.float32)
        sn = np.zeros((rot_dim, NT), np.float32)
        rot = np.zeros((rot_dim, rot_dim), np.float32)
        for a in range(2):
            ang = (pos[a][None, :] * inv[:, None]).astype(np.float32)
            for hh in range(2):
                r0 = a * axis_dim + hh * half
                cs[r0:r0 + half, :NL] = np.cos(ang)
                sn[r0:r0 + half, :NL] = np.sin(ang)
            for i in range(half):
                d1 = a * axis_dim + i
                d2 = d1 + half
                rot[d2, d1] = -1.0
                rot[d1, d2] = 1.0
        return cs, sn, rot
    ufb = np.stack([(kk <= qq), (kk >= qq)]).astype(np.float32)
    c["ssd_U"] = ufb
    c["ssd_negm"] = _bf((ufb - 1.0) * 30000.0)
    r = np.arange(128)
    c["s5_rowmask"] = np.stack([(r % 32 < 16), (r % 32 >= 16)], 1).astype(np.float32)
    c["s5_cmask"] = ((r[:, None] % 32 < 16) == (r[None, :] < 64)).astype(np.float32)
    cs, sn, rot = rope(128)
    c["swa_cos"], c["swa_sin"], c["swa_rot"] = cs, sn, _bf(rot)
    cs, sn, rot = rope(64)
    c["mla_cos"], c["mla_sin"], c["mla_rot"] = cs, sn, _bf(rot)
    return c


def pass_moe(C, hT, w_group, b_group, w_expert, b_expert, w1, w3, w2, modv, l, xres, ident, groups=None, stages=(1, 2, 3), nexp=32, dbg_gt=None, dbg2=None):
    P = C.P
    if groups is None:
        groups = [(0, 4), (4, 4), (8, 4), (12, 4), (16, 2)]
    BIG = 1.0e9
    with ExitStack() as es:
        act = C.sb(es, [128, 4, KC, 128], BF16, "act")
        gT = C.sb(es, [128, 32, 2, 512], BF16, "gT")
        w2u = [C.sb(es, [128, 8, 2, 512], BF16, "w2u") for _ in range(2)]
        wch = [C.sb(es, [128, KC, 128], BF16, "wch") for _ in range(3)]
        wr = C.sb(es, [128, KC, 36], BF16, "wr")
        bia = C.sb(es, [128, 36], F32, "bia")
        idt = C.sb(es, [128, 128], F32, "idt")
        Ssb = [C.sb(es, [128, 512], F32, "Ssb") for _ in range(2)]
        Tsb = [C.sb(es, [128, 512], F32, "Tsb") for _ in range(2)]
        GBs = [C.sb(es, [128, 512], F32, "GBs") for _ in range(2)]
        GT = C.sb(es, [32, 512], F32, "GT")
        L = [C.sb(es, [128, 36], F32, "L") for _ in range(2)]
        L2 = [C.sb(es, [128, 32], F32, "L2") for _ in range(2)]
        sm = [C.sb(es, [128, 16], F32, "sm") for _ in range(2)]
        oh = [C.sb(es, [128, 4 + 32 + 32], F32, "oh") for _ in range(2)]
        Gm = [C.sb(es, [128, 32], F32, "Gm") for _ in range(2)]
        xs = [C.sb(es, [128, 512], F32, "xs") for _ in range(3)]
        ts = [C.sb(es, [128, 512], F32, "ts") for _ in range(3)]
        G5 = [C.sb(es, [128, 512], F32, "G5") for _ in range(2)]
        ppr = C.ps(es, [128, 512], F32, "ppr")
        ptr = C.ps(es, [128, 512], F32, "ptr")
        H = [C.ps(es, [128, 512], F32, "H") for _ in range(4)]
        GBp = [C.ps(es, [128, 512], F32, "GBp") for _ in range(2)]

        P.dma(idt[:, :], ident.ap(), writes=[idt])
        P.dma(wr[:, :, 0:4], w_group.rearrange("(k p) c -> p k c", p=128), writes=[wr], eng="pool")
        P.dma(wr[:, :, 4:36], w_expert.rearrange("(k p) c -> p k c", p=128), writes=[wr], eng="pool")
        P.dma(bia[:, 0:4], b_group.partition_broadcast(128), writes=[bia])
        P.dma(bia[:, 4:36], b_expert.partition_broadcast(128), writes=[bia])
        nw = 0
        nu = 0
        nx = 0
        ng = 0
        nr = 0
        def do_group(t0, nt):
            nonlocal nw, nu, nx, ng, nr
            n = nt * 128
            s_idx = 0 if t0 < 16 else 1
            load_act(C, act, hT, t0, nt, "hT")
            for ti in (range(nt) if 1 in stages else []):
                c = nr % 2
                nr += 1
                Lc, L2c, smc, ohc, Gc = L[c], L2[c], sm[c], oh[c], Gm[c]
                for k in range(KC):
                    P.add("pe", lambda e, ti=ti, k=k: e.matmul(ppr[:, 0:36], lhsT=act[:, ti, k, :], rhs=wr[:, k, :],
                                                               start=(k == 0), stop=(k == KC - 1)), reads=[act, wr], writes=[ppr])
                P.add("dve", lambda e, Lc=Lc: e.tensor_tensor(out=Lc[:, :], in0=ppr[:, 0:36], in1=bia[:, :], op=ALU.add),
                      reads=[ppr, bia], writes=[Lc])
                P.add("dve", lambda e, Lc=Lc, smc=smc: e.tensor_reduce(out=smc[:, 0:1], in_=Lc[:, 0:4], axis=AX.X, op=ALU.max),
                      reads=[Lc], writes=[smc])
                P.add("dve", lambda e, Lc=Lc, smc=smc, ohc=ohc: e.tensor_scalar(out=ohc[:, 0:4], in0=Lc[:, 0:4], scalar1=smc[:, 0:1],
                                                                               scalar2=None, op0=ALU.is_equal),
                      reads=[Lc, smc], writes=[ohc])
                P.add("dve", lambda e, smc=smc: e.tensor_scalar(out=smc[:, 1:2], in0=smc[:, 0:1], scalar1=-1.0, scalar2=None,
                                                                op0=ALU.mult), reads=[smc], writes=[smc])
                P.add("act", lambda e, Lc=Lc, smc=smc, ohc=ohc: e.activation(out=ohc[:, 36:40], in_=Lc[:, 0:4], func=AF.Exp,
                                                                            bias=smc[:, 1:2], accum_out=smc[:, 2:3]),
                      reads=[Lc, smc], writes=[ohc, smc])
                P.add("dve", lambda e, smc=smc: e.reciprocal(out=smc[:, 3:4], in_=smc[:, 2:3]), reads=[smc], writes=[smc])
                P.add("dve", lambda e, ohc=ohc: e.tensor_scalar(out=ohc[:, 40:44], in0=ohc[:, 0:4], scalar1=-1.0, scalar2=BIG,
                                                                op0=ALU.add, op1=ALU.mult), reads=[ohc], writes=[ohc])
                P.add("dve", lambda e, Lc=Lc, L2c=L2c, ohc=ohc: e.tensor_tensor(
                    out=L2c[:, :].rearrange("p (g j) -> p g j", g=4), in0=Lc[:, 4:36].rearrange("p (g j) -> p g j", g=4),
                    in1=ohc[:, 40:44, None].to_broadcast([128, 4, 8]), op=ALU.add), reads=[Lc, ohc], writes=[L2c])
                P.add("dve", lambda e, L2c=L2c, smc=smc: e.tensor_reduce(out=smc[:, 4:5], in_=L2c[:, :], axis=AX.X, op=ALU.max),
                      reads=[L2c], writes=[smc])
                P.add("dve", lambda e, L2c=L2c, smc=smc, ohc=ohc: e.tensor_scalar(out=ohc[:, 4:36], in0=L2c[:, :], scalar1=smc[:, 4:5],
                                                                                 scalar2=None, op0=ALU.is_equal),
                      reads=[L2c, smc], writes=[ohc])
                P.add("dve", lambda e, L2c=L2c, ohc=ohc: e.scalar_tensor_tensor(out=L2c[:, :], in0=ohc[:, 4:36], scalar=-BIG,
                                                                               in1=L2c[:, :], op0=ALU.mult, op1=ALU.add),
                      reads=[L2c, ohc], writes=[L2c])
                P.add("dve", lambda e, L2c=L2c, smc=smc: e.tensor_reduce(out=smc[:, 5:6], in_=L2c[:, :], axis=AX.X, op=ALU.max),
                      reads=[L2c], writes=[smc])
                P.add("dve", lambda e, L2c=L2c, smc=smc, Gc=Gc: e.tensor_scalar(out=Gc[:, :], in0=L2c[:, :], scalar1=smc[:, 5:6],
                                                                               scalar2=None, op0=ALU.is_equal),
                      reads=[L2c, smc], writes=[Gc])
                P.add("dve", lambda e, smc=smc: e.tensor_tensor(out=smc[:, 6:7], in0=smc[:, 5:6], in1=smc[:, 4:5], op=ALU.subtract),
                      reads=[smc], writes=[smc])
                P.add("act", lambda e, smc=smc: e.activation(out=smc[:, 7:8], in_=smc[:, 6:7], func=AF.Exp), reads=[smc], writes=[smc])
                P.add("dve", lambda e, smc=smc: e.tensor_scalar(out=smc[:, 8:9], in0=smc[:, 7:8], scalar1=1.0, scalar2=None, op0=ALU.add),
                      reads=[smc], writes=[smc])
                P.add("dve", lambda e, smc=smc: e.reciprocal(out=smc[:, 9:10], in_=smc[:, 8:9]), reads=[smc], writes=[smc])
                P.add("dve", lambda e, smc=smc: e.tensor_tensor(out=smc[:, 10:11], in0=smc[:, 9:10], in1=smc[:, 3:4], op=ALU.mult),
                      reads=[smc], writes=[smc])
                P.add("dve", lambda e, smc=smc: e.tensor_tensor(out=smc[:, 11:12], in0=smc[:, 10:11], in1=smc[:, 7:8], op=ALU.mult),
                      reads=[smc], writes=[smc])
                P.add("dve", lambda e, Gc=Gc, smc=smc: e.tensor_scalar(out=Gc[:, :], in0=Gc[:, :], scalar1=smc[:, 11:12], scalar2=None,
                                                                      op0=ALU.mult), reads=[Gc, smc], writes=[Gc])
                P.add("dve", lambda e, Gc=Gc, smc=smc, ohc=ohc: e.scalar_tensor_tensor(out=Gc[:, :], in0=ohc[:, 4:36], scalar=smc[:, 10:11],
                                                                                      in1=Gc[:, :], op0=ALU.mult, op1=ALU.add),
                      reads=[Gc, smc, ohc], writes=[Gc])
                P.add("pe", lambda e, Gc=Gc: e.transpose(out=ptr[0:32, 0:128], in_=Gc[:, :], identity=idt[:, :]),
                      reads=[Gc, idt], writes=[ptr])
                P.add("act", lambda e, ti=ti: e.copy(out=GT[:, ti * 128:(ti + 1) * 128], in_=ptr[0:32, 0:128]),
                      reads=[ptr], writes=[GT])
            if dbg_gt is not None:
                P.dma(dbg_gt.ap()[:, t0 * 128:t0 * 128 + n], GT[:, 0:n], reads=[GT], writes=[("dbg_gt", t0)], is_out=True)
            for ex in (range(nexp) if 2 in stages else []):
                for (wsrc, c, hi) in ((w1, 0, 0), (w1, 1, 1), (w3, 0, 2), (w3, 1, 3)):
                    w = wch[nw % 3]
                    nw += 1
                    P.dma(w[:, :, :], wsrc[ex, :, c * 128:(c + 1) * 128].rearrange("(k p) c -> p k c", p=128), writes=[w], eng="pool")
                    for k in range(KC):
                        P.add("pe", lambda e, w=w, k=k, hi=hi: e.matmul(H[hi][:, 0:n], lhsT=w[:, k, :], rhs=act[:, 0:nt, k, :],
                                                                        start=(k == 0), stop=(k == KC - 1)),
                              reads=[w, act], writes=[H[hi]])
                gb = GBp[ng % 2]
                gbs = GBs[ng % 2]
                ng += 1
                dbg = int(os.environ.get("MOE_DBG", "9"))
                if dbg < 1:
                    continue
                P.add("pe", lambda e, gb=gb, ex=ex: e.matmul(gb[:, 0:n], lhsT=idt[0:32, ex:ex + 1].to_broadcast([32, 128]),
                                                             rhs=GT[:, 0:n], start=True, stop=True), reads=[idt, GT], writes=[gb])
                P.add("act", lambda e, gb=gb, gbs=gbs: e.copy(out=gbs[:, 0:n], in_=gb[:, 0:n]), reads=[gb], writes=[gbs])
                for c in (range(2) if dbg >= 2 else []):
                    S = Ssb[c]
                    T = Tsb[c]
                    P.add("act", lambda e, S=S, c=c: e.activation(out=S[:, 0:n], in_=H[c][:, 0:n], func=AF.Silu),
                          reads=[H[c]], writes=[S])
                    if dbg < 3:
                        continue
                    P.add("dve", lambda e, S=S, T=T, c=c: e.tensor_tensor(out=T[:, 0:n], in0=S[:, 0:n], in1=H[2 + c][:, 0:n], op=ALU.mult),
                          reads=[S, H[2 + c]], writes=[T])
                    if dbg < 4:
                        continue
                    P.add("pool", lambda e, T=T, gbs=gbs, ex=ex, c=c: e.tensor_tensor(out=gT[:, ex, c, 0:n], in0=T[:, 0:n], in1=gbs[:, 0:n],
                                                                                      op=ALU.mult), reads=[T, gbs], writes=[gT])
            if dbg2 is not None:
                for cc_ in range(2):
                    P.add("dve", lambda e, cc_=cc_: e.tensor_copy(out=Ssb[cc_][:, :], in_=gT[:, 0, cc_, :]), reads=[gT, Tsb[cc_]], writes=[Ssb[cc_]])
                for ii, tt_ in enumerate((Ssb[0], Tsb[0], GBs[0], Ssb[1], Tsb[1], GBs[1])):
                    P.dma(dbg2.ap()[ii], tt_[:, :], reads=[tt_], writes=[("dbg2", ii)], is_out=True)
            for cb in (range(8) if 3 in stages else []):
                g5 = G5[cb % 2]
                P.dma(g5[:, :], modv.ap()[l, s_idx, 5 * D + cb * 512:5 * D + (cb + 1) * 512].partition_broadcast(128),
                      reads=[("modv", l)], writes=[g5])
                for q in range(4):
                    u = w2u[nu % 2]
                    nu += 1
                    P.dma(u[:, :, :, :], w2[8 * q:8 * q + 8, :, cb * 512:(cb + 1) * 512].rearrange("e (c p) n -> p e c n", p=128),
                          writes=[u], eng="pool")
                    for ti in range(nt):
                        for el in range(8):
                            for c in range(2):
                                first = (q == 0 and el == 0 and c == 0)
                                last = (q == 3 and el == 7 and c == 1)
                                P.add("pe", lambda e, ti=ti, el=el, c=c, q=q, u=u, first=first, last=last: e.matmul(
                                    H[ti][:, :], lhsT=gT[:, 8 * q + el, c, ti * 128:(ti + 1) * 128], rhs=u[:, el, c, :],
                                    start=first, stop=last), reads=[gT, u], writes=[H[ti]])
                for ti in range(nt):
                    ig = t0 + ti
                    xc = xs[nx % 3]
                    tc = ts[nx % 3]
                    nx += 1
                    P.dma(xc[:, :], xres.ap()[ig * 128:(ig + 1) * 128, cb * 512:(cb + 1) * 512], reads=[("xres", ig)], writes=[xc])
                    P.add("dve", lambda e, tc=tc, ti=ti, g5=g5: e.tensor_tensor(out=tc[:, :], in0=H[ti][:, :], in1=g5[:, :], op=ALU.mult),
                          reads=[H[ti], g5], writes=[tc])
                    P.add("pool", lambda e, xc=xc, tc=tc: e.tensor_tensor(out=xc[:, :], in0=xc[:, :], in1=tc[:, :], op=ALU.add),
                          reads=[xc, tc], writes=[xc])
                    P.dma(xres.ap()[ig * 128:(ig + 1) * 128, cb * 512:(cb + 1) * 512], xc[:, :], reads=[xc], writes=[("xres", ig)],
                          eng="act")
        for (t0_, nt_) in groups:
            do_group(t0_, nt_)
    P.barrier()


def pass_mla_a(C, hT, w_in, q_a_g, kv_a_g, cqT, ckvT, krT):
    P = C.P
    def do_block(blk):
        with ExitStack() as es:
            act = C.sb(es, [128, 9, KC, 128], BF16, "act")
            raw = C.sb(es, [128, 12, 1152], F32, "raw")
            krs = [C.sb(es, [64, 384], F32, "krs") for _ in range(2)]
            ga = C.sb(es, [128, 12], F32, "ga")
            ones = C.sb(es, [128, 128], BF16, "ones")
            sq = [C.sb(es, [128, 384], BF16, "sq") for _ in range(3)]
            rs = [C.sb(es, [128, 384], F32, "rs") for _ in range(2)]
            stq = [C.sb(es, [128, 3, 8, 128], BF16, "stq") for _ in range(2)]
            stk = [C.sb(es, [128, 3, 4, 128], BF16, "stk") for _ in range(2)]
            pss = [C.ps(es, [128, 512], F32, "pss") for _ in range(2)]
            gps = [C.ps(es, [128, 512], F32, "gps") for _ in range(3)]
            load_act(C, act, hT, blk * 9, 9, "hT")
            P.dma(ga[:, 0:8], q_a_g.rearrange("(c p) -> p c", p=128), writes=[ga], slow=True)
            P.dma(ga[:, 8:12], kv_a_g.rearrange("(c p) -> p c", p=128), writes=[ga], slow=True)
            P.add("pool", lambda e: e.memset(ones[:, :], 1.0), writes=[ones])
            nk = [0]

            def epi(pp, j, tt0, ntl):
                n = ntl * 128
                if j < 12:
                    P.add("act", lambda e: e.copy(out=raw[:, j, tt0 * 128:tt0 * 128 + n], in_=pp[:, 0:n]), reads=[pp], writes=[raw])
                else:
                    kr = krs[nk[0] % 2]
                    nk[0] += 1
                    P.add("act", lambda e: e.copy(out=kr[:, 0:n], in_=pp[0:64, 0:n]), reads=[pp], writes=[kr])
                    tg = (blk * 9 + tt0) * 128
                    P.dma(krT.ap()[:, tg:tg + n], kr[:, 0:n], reads=[kr], writes=[("krT", 0)])

            gemm_ws(C, es, act, 9, KC, w_in, 0, 1600, epi, "w", psb=gps)
            nsq = 0
            def do_sub(sb_):
                nonlocal nsq
                c0 = sb_ * 384
                for (j0, nj, stg, dst, key) in ((0, 8, stq[sb_ % 2], cqT, "cqT"), (8, 4, stk[sb_ % 2], ckvT, "ckvT")):
                    pp = pss[(2 * sb_ + (j0 > 0)) % 2]
                    r = rs[(2 * sb_ + (j0 > 0)) % 2]
                    for jj in range(nj):
                        s_ = sq[nsq % 3]
                        nsq += 1
                        P.add("act", lambda e, s_=s_, jj=jj, j0=j0: e.activation(out=s_[:, :], in_=raw[:, j0 + jj, c0:c0 + 384], func=AF.Square),
                              reads=[raw], writes=[s_])
                        P.add("pe", lambda e, s_=s_, jj=jj, nj=nj, pp=pp: e.matmul(pp[:, 0:384], lhsT=ones[:, :], rhs=s_[:, :],
                                                                                 start=(jj == 0), stop=(jj == nj - 1)),
                              reads=[ones, s_], writes=[pp])
                    P.add("dve", lambda e, pp=pp, r=r, nj=nj: e.tensor_scalar(out=r[:, :], in0=pp[:, 0:384], scalar1=1.0 / (nj * 128),
                                                                              scalar2=EPS, op0=ALU.mult, op1=ALU.add), reads=[pp], writes=[r])
                    P.add("act", lambda e, r=r: e.activation(out=r[:, :], in_=r[:, :], func=AF.Sqrt), reads=[r], writes=[r])
                    P.add("dve", lambda e, r=r: e.reciprocal(out=r[:, :], in_=r[:, :]), reads=[r], writes=[r])
                    for jj in range(nj):
                        eng = "dve"
                        P.add(eng, lambda e, jj=jj, j0=j0, stg=stg, r=r: e.scalar_tensor_tensor(
                            out=stg[:, :, jj, :], in0=raw[:, j0 + jj, c0:c0 + 384].rearrange("p (i t) -> p i t", i=3),
                            scalar=ga[:, j0 + jj:j0 + jj + 1], in1=r[:, :].rearrange("p (i t) -> p i t", i=3),
                            op0=ALU.mult, op1=ALU.mult), reads=[raw, ga, r], writes=[stg])
                    ti0 = blk * 9 + sb_ * 3
                    P.dma(dst.ap()[ti0:ti0 + 3].rearrange("i p k t -> p i k t"), stg[:, :, :, :], reads=[stg], writes=[(key, ti0)])
            for sbi_ in range(3):
                do_sub(sbi_)
        P.barrier()
    for blk_ in range(2):
        do_block(blk_)


def pass_mla_b(C, cqT, ckvT, krT, w_uq, w_ukv, q_g, k_g, cos64, sin64, rot64, QN, QR, KN, KR, Vd):
    P = C.P
    scale = 192 ** -0.5
    with ExitStack() as es:
        actq = C.sb(es, [128, NTT, 8, 128], BF16, "actq")
        actk = C.sb(es, [128, NTT, 4, 128], BF16, "actk")
        kr = C.sb(es, [64, NT], F32, "kr")
        krsq = C.sb(es, [64, NT], BF16, "krsq")
        cs = C.sb(es, [64, NT], F32, "cs")
        sn = C.sb(es, [64, NT], F32, "sn")
        rm = C.sb(es, [64, 64], BF16, "rm")
        ones = C.sb(es, [128, 128], BF16, "ones")
        gq = C.sb(es, [128, 4], F32, "gq")
        wq = [C.sb(es, [128, 8, 192], BF16, "wq") for _ in range(2)]
        wk = [C.sb(es, [128, 4, 256], BF16, "wk") for _ in range(2)]
        sqn = [C.sb(es, [128, 384], BF16, "sqn") for _ in range(2)]
        sqr = [C.sb(es, [64, 384], BF16, "sqr") for _ in range(2)]
        rs = [C.sb(es, [128, 384], F32, "rs") for _ in range(2)]
        on_ = [C.sb(es, [128, 384], BF16, "on") for _ in range(2)]
        rn = [C.sb(es, [64, 384], BF16, "rn") for _ in range(2)]
        t1 = [C.sb(es, [64, 384], F32, "t1") for _ in range(2)]
        t2 = [C.sb(es, [64, 384], F32, "t2") for _ in range(2)]
        orr = [C.sb(es, [64, 384], BF16, "orr") for _ in range(2)]
        vo = [C.sb(es, [128, 3, 128], BF16, "vo") for _ in range(2)]
        pqn = C.ps(es, [128, 512], F32, "pqn")
        pqr = C.ps(es, [128, 512], F32, "pqr")
        pkn = C.ps(es, [128, 512], F32, "pkn")
        pss = [C.ps(es, [128, 512], F32, "pss") for _ in range(2)]
        prt = [C.ps(es, [128, 512], F32, "prt") for _ in range(2)]
        pv = C.ps(es, [128, 512], F32, "pv")
        load_act(C, actq, cqT, 0, NTT, "cqT")
        load_act(C, actk, ckvT, 0, NTT, "ckvT")
        P.dma(kr[:, :], krT.ap(), reads=[("krT", 0)], writes=[kr])
        P.dma(cs[:, :], cos64.ap(), writes=[cs])
        P.dma(sn[:, :], sin64.ap(), writes=[sn])
        P.dma(rm[:, :], rot64.ap(), writes=[rm])
        P.dma(gq[:, 0:1], q_g[0:128].rearrange("(p o) -> p o", o=1), writes=[gq])
        P.dma(gq[0:64, 1:2], q_g[128:192].rearrange("(p o) -> p o", o=1), writes=[gq])
        P.dma(gq[:, 2:3], k_g[0:128].rearrange("(p o) -> p o", o=1), writes=[gq])
        P.dma(gq[0:64, 3:4], k_g[128:192].rearrange("(p o) -> p o", o=1), writes=[gq])
        P.add("dve", lambda e: e.tensor_scalar(out=gq[:, 0:1], in0=gq[:, 0:1], scalar1=scale, scalar2=None, op0=ALU.mult),
              reads=[gq], writes=[gq])
        P.add("dve", lambda e: e.tensor_scalar(out=gq[0:64, 1:2], in0=gq[0:64, 1:2], scalar1=scale, scalar2=None, op0=ALU.mult),
              reads=[gq], writes=[gq])
        P.add("pool", lambda e: e.memset(ones[:, :], 1.0 / 192), writes=[ones])
        P.add("act", lambda e: e.activation(out=krsq[:, :], in_=kr[:, :], func=AF.Square), reads=[kr], writes=[krsq])
        cnt = 0

        def norm_rope(pn, pr_src, pr_reads, gi, dstN, dstR, h, tok0, n, c, extra_sq=None):
            ps_ = pss[c]
            r = rs[c]
            P.add("act", lambda e: e.activation(out=sqn[c][:, 0:n], in_=pn[:, 0:n], func=AF.Square), reads=[pn], writes=[sqn[c]])
            P.add("pe", lambda e: e.matmul(ps_[:, 0:n], lhsT=ones[:, :], rhs=sqn[c][:, 0:n], start=True, stop=False),
                  reads=[ones, sqn[c]], writes=[ps_])
            if extra_sq is None:
                P.add("act", lambda e: e.activation(out=sqr[c][:, 0:n], in_=pr_src, func=AF.Square), reads=pr_reads, writes=[sqr[c]])
                sq2 = sqr[c][:, 0:n]
                rd2 = [sqr[c]]
            else:
                sq2 = extra_sq
                rd2 = [krsq]
            P.add("pe", lambda e: e.matmul(ps_[:, 0:n], lhsT=ones[0:64, :], rhs=sq2, start=False, stop=True),
                  reads=[ones] + rd2, writes=[ps_])
            P.add("dve", lambda e: e.tensor_scalar(out=r[:, 0:n], in0=ps_[:, 0:n], scalar1=EPS, scalar2=None, op0=ALU.add),
                  reads=[ps_], writes=[r])
            P.add("act", lambda e: e.activation(out=r[:, 0:n], in_=r[:, 0:n], func=AF.Sqrt), reads=[r], writes=[r])
            P.add("dve", lambda e: e.reciprocal(out=r[:, 0:n], in_=r[:, 0:n]), reads=[r], writes=[r])
            P.add("dve", lambda e: e.scalar_tensor_tensor(out=on_[c][:, 0:n], in0=pn[:, 0:n], scalar=gq[:, gi:gi + 1], in1=r[:, 0:n],
                                                          op0=ALU.mult, op1=ALU.mult), reads=[pn, gq, r], writes=[on_[c]])
            P.dma(dstN.ap()[h, :, tok0:tok0 + n], on_[c][:, 0:n], reads=[on_[c]], writes=[("N", gi, h)])
            P.add("dve", lambda e: e.scalar_tensor_tensor(out=rn[c][:, 0:n], in0=pr_src, scalar=gq[0:64, gi + 1:gi + 2], in1=r[0:64, 0:n],
                                                          op0=ALU.mult, op1=ALU.mult), reads=pr_reads + [gq, r], writes=[rn[c]])
            pt_ = prt[c]
            P.add("pe", lambda e: e.matmul(pt_[0:64, 0:n], lhsT=rm[:, :], rhs=rn[c][:, 0:n], start=True, stop=True),
                  reads=[rm, rn[c]], writes=[pt_])
            P.add("pool", lambda e: e.tensor_tensor(out=t1[c][:, 0:n], in0=rn[c][:, 0:n], in1=cs[:, tok0:tok0 + n], op=ALU.mult),
                  reads=[rn[c], cs], writes=[t1[c]])
            P.add("dve", lambda e: e.tensor_tensor(out=t2[c][:, 0:n], in0=pt_[0:64, 0:n], in1=sn[:, tok0:tok0 + n], op=ALU.mult),
                  reads=[pt_, sn], writes=[t2[c]])
            P.add("pool", lambda e: e.tensor_tensor(out=orr[c][:, 0:n], in0=t1[c][:, 0:n], in1=t2[c][:, 0:n], op=ALU.add),
                  reads=[t1[c], t2[c]], writes=[orr[c]])
            P.dma(dstR.ap()[h, :, tok0:tok0 + n], orr[c][:, 0:n], reads=[orr[c]], writes=[("R", gi, h)])

        nv = 0
        for h in range(32):
            wqh = wq[h % 2]
            wkh = wk[h % 2]
            P.dma(wqh[:, :, :], w_uq[:, h * 192:(h + 1) * 192].rearrange("(k p) c -> p k c", p=128), writes=[wqh], eng="pool")
            P.dma(wkh[:, :, :], w_ukv[:, h * 256:(h + 1) * 256].rearrange("(k p) c -> p k c", p=128), writes=[wkh], eng="pool")
            for sb_ in range(6):
                ti0 = sb_ * 3
                tok0 = ti0 * 128
                n = 384
                for k in range(8):
                    P.add("pe", lambda e, k=k, wqh=wqh, ti0=ti0: e.matmul(pqn[:, 0:n], lhsT=wqh[:, k, 0:128], rhs=actq[:, ti0:ti0 + 3, k, :],
                                                                          start=(k == 0), stop=(k == 7)), reads=[wqh, actq], writes=[pqn])
                for k in range(8):
                    P.add("pe", lambda e, k=k, wqh=wqh, ti0=ti0: e.matmul(pqr[0:64, 0:n], lhsT=wqh[:, k, 128:192], rhs=actq[:, ti0:ti0 + 3, k, :],
                                                                          start=(k == 0), stop=(k == 7)), reads=[wqh, actq], writes=[pqr])
                for k in range(4):
                    P.add("pe", lambda e, k=k, wkh=wkh, ti0=ti0: e.matmul(pkn[:, 0:n], lhsT=wkh[:, k, 0:128], rhs=actk[:, ti0:ti0 + 3, k, :],
                                                                          start=(k == 0), stop=(k == 3)), reads=[wkh, actk], writes=[pkn])
                norm_rope(pqn, pqr[0:64, 0:n], [pqr], 0, QN, QR, h, tok0, n, 0)
                norm_rope(pkn, kr[:, tok0:tok0 + n], [kr], 2, KN, KR, h, tok0, n, 1, extra_sq=krsq[:, tok0:tok0 + n])
                v_ = vo[nv % 2]
                nv += 1
                for i in range(3):
                    for k in range(4):
                        P.add("pe", lambda e, k=k, i=i, wkh=wkh, ti0=ti0: e.matmul(pv[:, i * 128:(i + 1) * 128], lhsT=actk[:, ti0 + i, k, :],
                                                                                   rhs=wkh[:, k, 128:256], start=(k == 0), stop=(k == 3)),
                              reads=[wkh, actk], writes=[pv])
                P.add("act", lambda e, v_=v_: e.copy(out=v_[:, :, :], in_=pv[:, 0:384].rearrange("p (i d) -> p i d", i=3)),
                      reads=[pv], writes=[v_])
                P.dma(Vd.ap()[tok0:tok0 + 384, h * 128:(h + 1) * 128].rearrange("(i p) d -> p i d", p=128), v_[:, :, :],
                      reads=[v_], writes=[("Vd", h)])
    P.barrier()


def pass_mla_attn(C, QN, QR, KN, KR, Vd, oT, ctx_out=True):
    P = C.P
    with ExitStack() as es:
        qn = [C.sb(es, [128, NT], BF16, "qn") for _ in range(2)]
        qr = [C.sb(es, [64, NT], BF16, "qr") for _ in range(2)]
        kn = [C.sb(es, [128, NT], BF16, "kn") for _ in range(2)]
        kr = [C.sb(es, [64, NT], BF16, "kr") for _ in range(2)]
        vs = [C.sb(es, [128, NTT, 128], BF16, "vs") for _ in range(2)]
        ones = C.sb(es, [128, 128], BF16, "ones")
        pts = [C.sb(es, [128, 512], BF16, "pt") for _ in range(3)]
        den = [C.sb(es, [128, 512], F32, "den") for _ in range(2)]
        ob = [C.sb(es, [128, 512], BF16, "ob") for _ in range(2)]
        scp = [C.ps(es, [128, 512], F32, "sc") for _ in range(3)]
        ovp = [C.ps(es, [128, 512], F32, "ov") for _ in range(2)]
        dnp = [C.ps(es, [128, 512], F32, "dn") for _ in range(2)]
        P.add("pool", lambda e: e.memset(ones[:, :], 1.0), writes=[ones])
        nstep = 0
        nblk = 0
        qgroups = [(0, 4, list(range(18))), (4, 4, list(range(18))), (8, 4, list(range(18))), (12, 4, list(range(18)))]
        if ctx_out:
            qgroups.append((16, 2, [16, 17]))
        for h in range(32):
            a, b, c_, d_, v = qn[h % 2], qr[h % 2], kn[h % 2], kr[h % 2], vs[h % 2]
            P.dma(a[:, :], QN.ap()[h], reads=[("N", 0, h)], writes=[a])
            P.dma(b[:, :], QR.ap()[h], reads=[("R", 0, h)], writes=[b])
            P.dma(c_[:, :], KN.ap()[h], reads=[("N", 2, h)], writes=[c_])
            P.dma(d_[:, :], KR.ap()[h], reads=[("R", 2, h)], writes=[d_])
            P.dma(v[:, :, :], Vd.ap()[:, h * 128:(h + 1) * 128].rearrange("(i p) d -> p i d", p=128), reads=[("Vd", h)], writes=[v])
            def do_qg(t0, nt, chunks, h=h, a=a, b=b, c_=c_, d_=d_, v=v):
                nonlocal nstep, nblk
                n = nt * 128
                q0 = t0 * 128
                ov = ovp[nblk % 2]
                dn = dnp[nblk % 2]
                dsb = den[nblk % 2]
                o = ob[nblk % 2]
                nblk += 1
                for ci, c in enumerate(chunks):
                    sc = scp[nstep % 3]
                    pt = pts[nstep % 3]
                    nstep += 1
                    fst = ci == 0
                    lst = ci == len(chunks) - 1
                    P.add("pe", lambda e, sc=sc, c=c, c_=c_, a=a: e.matmul(sc[:, 0:n], lhsT=c_[:, c * 128:(c + 1) * 128], rhs=a[:, q0:q0 + n],
                                                                         start=True, stop=False), reads=[c_, a], writes=[sc])
                    P.add("pe", lambda e, sc=sc, c=c, d_=d_, b=b: e.matmul(sc[:, 0:n], lhsT=d_[:, c * 128:(c + 1) * 128], rhs=b[:, q0:q0 + n],
                                                                         start=False, stop=True), reads=[d_, b], writes=[sc])
                    P.add("act", lambda e, sc=sc, pt=pt: e.activation(out=pt[:, 0:n], in_=sc[:, 0:n], func=AF.Exp), reads=[sc], writes=[pt])
                    P.add("pe", lambda e, ov=ov, v=v, c=c, pt=pt, fst=fst, lst=lst: e.matmul(ov[:, 0:n], lhsT=v[:, c, :], rhs=pt[:, 0:n],
                                                                                           start=fst, stop=lst), reads=[v, pt], writes=[ov])
                    P.add("pe", lambda e, dn=dn, pt=pt, fst=fst, lst=lst: e.matmul(dn[:, 0:n], lhsT=ones[:, :], rhs=pt[:, 0:n],
                                                                                 start=fst, stop=lst), reads=[ones, pt], writes=[dn])
                P.add("dve", lambda e, dsb=dsb, dn=dn: e.reciprocal(out=dsb[:, 0:n], in_=dn[:, 0:n]), reads=[dn], writes=[dsb])
                P.add("dve", lambda e, o=o, ov=ov, dsb=dsb: e.tensor_tensor(out=o[:, 0:n], in0=ov[:, 0:n], in1=dsb[:, 0:n], op=ALU.mult),
                      reads=[ov, dsb], writes=[o])
                P.dma(oT.ap()[t0:t0 + nt, :, h, :].rearrange("i p t -> p i t"), o[:, 0:n].rearrange("p (i t) -> p i t", i=nt),
                      reads=[o], writes=[("oT", t0)])
            for (t0_, nt_, ch_) in qgroups:
                do_qg(t0_, nt_, ch_)
    P.barrier()


SSD_DI = 8192
SSD_XBC = 10240


def pass_ssd_proj(C, hT, w_in, xbc_raw, szd, dtd):
    P = C.P

    def do_block(blk):
        with ExitStack() as es:
            act = C.sb(es, [128, 9, KC, 128], BF16, "act")
            rw = [C.sb(es, [128, 384], F32, "rw") for _ in range(3)]
            zo = [C.sb(es, [128, 512], BF16, "zo") for _ in range(3)]
            do_ = [C.sb(es, [128, 256], F32, "do") for _ in range(2)]
            gps = [C.ps(es, [128, 512], F32, "gps") for _ in range(4)]
            load_act(C, act, hT, blk * 9, 9, "hT")
            cnt = [0, 0, 0]

            def epi(pp, j, tt0, ntl):
                n = ntl * 128
                r = rw[cnt[0] % 3]
                cnt[0] += 1
                P.add("act", lambda e: e.copy(out=r[:, 0:n], in_=pp[:, 0:n]), reads=[pp], writes=[r])
                tg = (blk * 9 + tt0) * 128
                P.dma(xbc_raw.ap()[j, :, tg:tg + n], r[:, 0:n], reads=[r], writes=[("xbc", j)])

            gemm_ws(C, es, act, 9, KC, w_in, SSD_DI, SSD_XBC, epi, "w", psb=gps)

            def epz(pp, ti, c0):
                o = zo[cnt[1] % 3]
                cnt[1] += 1
                P.add("act", lambda e: e.activation(out=o[:, :], in_=pp[:, 0:512], func=AF.Silu), reads=[pp], writes=[o])
                tg = (blk * 9 + ti) * 128
                P.dma(szd.ap()[tg:tg + 128, c0:c0 + 512], o[:, :], reads=[o], writes=[("sz", c0)])

            gemm_ts(C, es, act, 9, KC, w_in, 0, SSD_DI, epz, psb=gps)

            def epd(pp, ti, c0):
                o = do_[cnt[2] % 2]
                cnt[2] += 1
                P.add("act", lambda e: e.copy(out=o[:, :], in_=pp[:, 0:256]), reads=[pp], writes=[o])
                tg = (blk * 9 + ti) * 128
                P.dma(dtd.ap()[tg:tg + 128, :], o[:, :], reads=[o], writes=[("dtd", 0)])

            gemm_ts(C, es, act, 9, KC, w_in, SSD_DI + SSD_XBC, 256, epd, cb=256, psb=gps)
        P.barrier()

    for b_ in range(2):
        do_block(b_)


def pass_ssd_conv(C, xbc_raw, conv_w, conv_b, ident_bf, xs_tm, B_tm, BCT):
    P = C.P
    with ExitStack() as es:
        idb = C.sb(es, [128, 128], BF16, "idb")
        cw = C.sb(es, [128, 80, 4], F32, "cw")
        xr = [C.sb(es, [128, NT], F32, "xr") for _ in range(2)]
        ya = [C.sb(es, [128, NT], F32, "ya") for _ in range(2)]
        yb = [C.sb(es, [128, NT], BF16, "yb") for _ in range(2)]
        tm = [C.sb(es, [128, NTT, 128], BF16, "tm") for _ in range(2)]
        ptr = [C.ps(es, [128, 1024], BF16, "ptr") for _ in range(3)]
        P.dma(idb[:, :], ident_bf.ap(), writes=[idb])
        for c0_ in range(0, 80, 16):
            for kk in range(3):
                P.dma(cw[:, c0_:c0_ + 16, kk], conv_w[kk, c0_ * 128:(c0_ + 16) * 128].rearrange("(c p) -> p c", p=128), writes=[cw], slow=True)
            P.dma(cw[:, c0_:c0_ + 16, 3], conv_b[c0_ * 128:(c0_ + 16) * 128].rearrange("(c p) -> p c", p=128), writes=[cw], slow=True)
        npt = [0]

        def do_chunk(j):
            x = xr[j % 2]
            y = ya[j % 2]
            yo = yb[j % 2]
            P.dma(x[:, :], xbc_raw.ap()[j], reads=[("xbc", j)], writes=[x])
            for (a, b) in ((0, NL), (NL, NT)):
                P.add("dve", lambda e: e.tensor_scalar(out=y[:, a:b], in0=x[:, a:b], scalar1=cw[:, j, 1:2], scalar2=None, op0=ALU.mult),
                      reads=[x, cw], writes=[y]) if False else None
            P.add("dve", lambda e: e.tensor_scalar(out=y[:, :], in0=x[:, :], scalar1=cw[:, j, 1:2], scalar2=None, op0=ALU.mult),
                  reads=[x, cw], writes=[y])
            for (a, b) in ((0, NL), (NL, NT)):
                P.add("dve", lambda e, a=a, b=b: e.scalar_tensor_tensor(out=y[:, a + 1:b], in0=x[:, a:b - 1], scalar=cw[:, j, 0:1],
                                                                        in1=y[:, a + 1:b], op0=ALU.mult, op1=ALU.add),
                      reads=[x, cw, y], writes=[y])
                P.add("dve", lambda e, a=a, b=b: e.scalar_tensor_tensor(out=y[:, a:b - 1], in0=x[:, a + 1:b], scalar=cw[:, j, 2:3],
                                                                        in1=y[:, a:b - 1], op0=ALU.mult, op1=ALU.add),
                      reads=[x, cw, y], writes=[y])
            P.add("act", lambda e: e.activation(out=yo[:, :], in_=y[:, :], func=AF.Silu, bias=cw[:, j, 3:4]), reads=[y, cw], writes=[yo])
            if j >= 64:
                P.dma(BCT.ap()[j - 64], yo[:, :], reads=[yo], writes=[("BCT", j - 64)])
            if j < 72:
                t = tm[j % 2]
                for q in range(0, NTT, 8):
                    nq = min(8, NTT - q)
                    pt = ptr[npt[0] % 3]
                    npt[0] += 1
                    for i in range(nq):
                        P.add("pe", lambda e, i=i, q=q, pt=pt: e.transpose(out=pt[:, i * 128:(i + 1) * 128], in_=yo[:, (q + i) * 128:(q + i + 1) * 128],
                                                                           identity=idb[:, :]), reads=[yo, idb], writes=[pt])
                    P.add("act" if (q // 8) % 2 == 0 else "dve",
                          (lambda e, q=q, nq=nq, pt=pt: e.copy(out=t[:, q:q + nq, :], in_=pt[:, 0:nq * 128].rearrange("p (i c) -> p i c", i=nq)))
                          if (q // 8) % 2 == 0 else
                          (lambda e, q=q, nq=nq, pt=pt: e.tensor_copy(out=t[:, q:q + nq, :], in_=pt[:, 0:nq * 128].rearrange("p (i c) -> p i c", i=nq))),
                          reads=[pt], writes=[t])
                if j < 64:
                    P.dma(xs_tm.ap()[:, j * 128:(j + 1) * 128].rearrange("(i p) c -> p i c", p=128), t[:, :, :], reads=[t], writes=[("xs_tm", 0)])
                else:
                    P.dma(B_tm.ap()[:, (j - 64) * 128:(j - 63) * 128].rearrange("(i p) c -> p i c", p=128), t[:, :, :], reads=[t],
                          writes=[("B_tm", 0)])

        for j_ in range(80):
            do_chunk(j_)
    P.barrier()


def pass_ssd_scan(C, xs_tm, B_tm, BCT, dtd, dt_bias, a_log, d_skip, ident, ident_bf, Ufb, negm, ydr):
    P = C.P
    with ExitStack() as es:
        idt = C.sb(es, [128, 128], F32, "idt")
        idb = C.sb(es, [128, 128], BF16, "idb")
        U = C.sb(es, [128, 2, 128], F32, "U")
        NM = C.sb(es, [128, 2, 128], BF16, "NM")
        onesf = C.sb(es, [128, 128], F32, "onesf")
        bia = C.sb(es, [128, 2, 128], F32, "bia")
        av = C.sb(es, [128, 2, 128], F32, "av")
        dsk = C.sb(es, [128, 128], F32, "dsk")
        xst = [C.sb(es, [128, SSD_DI], BF16, "xst") for _ in range(2)]
        bt = [C.sb(es, [128, 1024], BF16, "bt") for _ in range(2)]
        bcT = [C.sb(es, [128, 16, 128], BF16, "bcT") for _ in range(2)]
        dtr = [C.sb(es, [128, 256], F32, "dtr") for _ in range(2)]
        dt = C.sb(es, [128, 128], F32, "dt")
        dta = C.sb(es, [128, 128], F32, "dta")
        ncum = C.sb(es, [128, 128], F32, "ncum")
        ecum = C.sb(es, [128, 128], F32, "ecum")
        clast = C.sb(es, [128, 128], F32, "clast")
        eclast = C.sb(es, [128, 128], F32, "eclast")
        te = C.sb(es, [128, 128], F32, "te")
        cumF = C.sb(es, [128, 128], F32, "cumF")
        stF = C.sb(es, [128, 8, 1024], F32, "stF")
        stB = C.sb(es, [128, 8, 1024], BF16, "stB")
        xdt = [C.sb(es, [128, 1024], BF16, "xdt") for _ in range(2)]
        xte = [C.sb(es, [128, 1024], BF16, "xte") for _ in range(2)]
        cb = [C.sb(es, [128, 128], F32, "cb") for _ in range(2)]
        E = [C.sb(es, [128, 128], F32, "E") for _ in range(3)]
        M = [C.sb(es, [128, 128], BF16, "M") for _ in range(3)]
        tmp = [C.sb(es, [128, 1024], F32, "tmp") for _ in range(2)]
        yo = [C.sb(es, [128, 1024], F32, "yo") for _ in range(2)]
        yp = [C.sb(es, [128, 1024], F32, "yp") for _ in range(2)]
        pc = C.ps(es, [128, 512], F32, "pc")
        pR = [C.ps(es, [128, 512], F32, "pR") for _ in range(2)]
        pY = C.ps(es, [128, 1024], F32, "pY")
        pI = C.ps(es, [128, 1024], F32, "pI")
        P.dma(idt[:, :], ident.ap(), writes=[idt])
        P.dma(idb[:, :], ident_bf.ap(), writes=[idb])
        P.dma(U[:, :, :], Ufb.ap().rearrange("d s t -> s d t"), writes=[U])
        P.dma(NM[:, :, :], negm.ap().rearrange("d s t -> s d t"), writes=[NM])
        P.add("pool", lambda e: e.memset(onesf[:, :], 1.0), writes=[onesf])
        for d in range(2):
            P.dma(bia[:, d, :], dt_bias[d].partition_broadcast(128), writes=[bia])
            P.dma(av[:, d, :], a_log[d].partition_broadcast(128), writes=[av])
        P.dma(dsk[:, :], d_skip.partition_broadcast(128), writes=[dsk])
        P.add("act", lambda e: e.activation(out=av[:, :, :], in_=av[:, :, :], func=AF.Exp), reads=[av], writes=[av])
        P.add("dve", lambda e: e.tensor_scalar(out=av[:, :, :], in0=av[:, :, :], scalar1=-1.0, scalar2=None, op0=ALU.mult),
              reads=[av], writes=[av])
        cnt = {"c": 0, "e": 0, "g": 0}

        def do_chunk(d, i, first):
            c = cnt["c"] % 2
            cnt["c"] += 1
            xs, b_, bc, dr = xst[c], bt[c], bcT[c], dtr[c]
            r0 = i * 128
            P.dma(xs[:, :], xs_tm.ap()[r0:r0 + 128, :], reads=[("xs_tm", 0)], writes=[xs])
            P.dma(b_[:, :], B_tm.ap()[r0:r0 + 128, :], reads=[("B_tm", 0)], writes=[b_])
            P.dma(bc[:, :, :], BCT.ap()[:, :, r0:r0 + 128].rearrange("g n t -> n g t"), reads=[("BCT", q) for q in range(16)], writes=[bc])
            P.dma(dr[:, :], dtd.ap()[r0:r0 + 128, :], reads=[("dtd", 0)], writes=[dr])
            P.add("dve", lambda e: e.tensor_tensor(out=dt[:, :], in0=dr[:, d * 128:(d + 1) * 128], in1=bia[:, d, :], op=ALU.add),
                  reads=[dr, bia], writes=[dt])
            P.add("act", lambda e: e.activation(out=dt[:, :], in_=dt[:, :], func=AF.Exp), reads=[dt], writes=[dt])
            P.add("dve", lambda e: e.tensor_scalar(out=dt[:, :], in0=dt[:, :], scalar1=1.0, scalar2=None, op0=ALU.add), reads=[dt], writes=[dt])
            P.add("act", lambda e: e.activation(out=dt[:, :], in_=dt[:, :], func=AF.Ln), reads=[dt], writes=[dt])
            P.add("dve", lambda e: e.tensor_tensor(out=dta[:, :], in0=dt[:, :], in1=av[:, d, :], op=ALU.mult), reads=[dt, av], writes=[dta])
            P.add("pe", lambda e: e.matmul(pc[:, 0:128], lhsT=U[:, d, :], rhs=dta[:, :], start=True, stop=True), reads=[U, dta], writes=[pc])
            P.add("pe", lambda e: e.matmul(pc[:, 128:256], lhsT=onesf[:, :], rhs=dta[:, :], start=True, stop=True), reads=[onesf, dta],
                  writes=[pc])
            P.add("pe", lambda e: e.matmul(pc[:, 256:384], lhsT=dta[:, :], rhs=U[:, d, :], start=True, stop=True), reads=[U, dta],
                  writes=[pc])
            P.add("dve", lambda e: e.tensor_scalar(out=ncum[:, :], in0=pc[:, 0:128], scalar1=-1.0, scalar2=None, op0=ALU.mult),
                  reads=[pc], writes=[ncum])
            P.add("act", lambda e: e.activation(out=ecum[:, :], in_=pc[:, 0:128], func=AF.Exp), reads=[pc], writes=[ecum])
            P.add("act", lambda e: e.copy(out=clast[:, :], in_=pc[:, 128:256]), reads=[pc], writes=[clast])
            P.add("act", lambda e: e.activation(out=eclast[:, :], in_=pc[:, 128:256], func=AF.Exp), reads=[pc], writes=[eclast])
            P.add("dve", lambda e: e.tensor_copy(out=cumF[:, :], in_=pc[:, 256:384]), reads=[pc], writes=[cumF])
            P.add("dve", lambda e: e.tensor_tensor(out=te[:, :], in0=clast[:, :], in1=ncum[:, :], op=ALU.add), reads=[clast, ncum], writes=[te])
            P.add("act", lambda e: e.activation(out=te[:, :], in_=te[:, :], func=AF.Exp), reads=[te], writes=[te])

            STOP = int(os.environ.get("SSD_STOP", "9"))
            if STOP <= 1:
                return

            def do_group(g):
                gc = cnt["g"] % 2
                cnt["g"] += 1
                xd, xt, cbs, tp, yy, ypv = xdt[gc], xte[gc], cb[gc], tmp[gc], yo[gc], yp[gc]
                hs = slice(g * 16, (g + 1) * 16)
                xg = xs[:, g * 1024:(g + 1) * 1024].rearrange("p (j q) -> p j q", j=16)
                P.add("dve", lambda e: e.tensor_tensor(out=xd[:, :].rearrange("p (j q) -> p j q", j=16), in0=xg,
                                                       in1=dt[:, hs, None].to_broadcast([128, 16, 64]), op=ALU.mult), reads=[xs, dt], writes=[xd])
                P.add("dve", lambda e: e.tensor_tensor(out=xt[:, :].rearrange("p (j q) -> p j q", j=16),
                                                        in0=xd[:, :].rearrange("p (j q) -> p j q", j=16),
                                                        in1=te[:, hs, None].to_broadcast([128, 16, 64]), op=ALU.mult), reads=[xd, te], writes=[xt])
                P.add("pe", lambda e: e.matmul(pc[:, 384:512], lhsT=bc[:, g, :], rhs=bc[:, 8 + g, :], start=True, stop=True), reads=[bc],
                      writes=[pc])
                P.add("act", lambda e: e.copy(out=cbs[:, :], in_=pc[:, 384:512]), reads=[pc], writes=[cbs])

                if STOP <= 2:
                    return

                def do_head(jj):
                    j = g * 16 + jj
                    ec = cnt["e"]
                    cnt["e"] += 1
                    pr = pR[ec % 2]
                    Ej = E[ec % 3]
                    Mj = M[ec % 3]
                    P.add("pe", lambda e: e.matmul(pr[:, 0:128], lhsT=idt[:, j:j + 1].to_broadcast([128, 128]), rhs=cumF[:, :], start=True, stop=False),
                          reads=[idt, cumF], writes=[pr])
                    P.add("pe", lambda e: e.matmul(pr[:, 0:128], lhsT=idb[:, :], rhs=NM[:, d, :], start=False, stop=True), reads=[idb, NM], writes=[pr])
                    P.add("act", lambda e: e.activation(out=Ej[:, :], in_=pr[:, 0:128], func=AF.Exp, bias=ncum[:, j:j + 1]),
                          reads=[pr, ncum], writes=[Ej])
                    P.add("dve", lambda e: e.tensor_tensor(out=Mj[:, :], in0=Ej[:, :], in1=cbs[:, :], op=ALU.mult), reads=[Ej, cbs], writes=[Mj])
                    P.add("pe", lambda e: e.matmul(pY[:, jj * 64:(jj + 1) * 64], lhsT=Mj[:, :], rhs=xd[:, jj * 64:(jj + 1) * 64], start=True, stop=True),
                          reads=[Mj, xd], writes=[pY])

                for jj_ in range(16):
                    do_head(jj_)
                if STOP <= 3:
                    return
                if d == 0:
                    P.add("dve", lambda e: e.tensor_tensor(out=ypv[:, :].rearrange("p (j q) -> p j q", j=16), in0=xg,
                                                            in1=dsk[:, hs, None].to_broadcast([128, 16, 64]), op=ALU.mult), reads=[xs, dsk], writes=[ypv])
                else:
                    P.dma(ypv[:, :], ydr.ap()[r0:r0 + 128, g * 1024:(g + 1) * 1024], reads=[("ydr", i, g)], writes=[ypv])
                if not first:
                    for hh in range(2):
                        P.add("pe", lambda e, hh=hh: e.matmul(pI[:, hh * 512:(hh + 1) * 512], lhsT=bc[:, 8 + g, :], rhs=stB[:, g, hh * 512:(hh + 1) * 512],
                                                              start=True, stop=True), reads=[bc, (stB.name, g)], writes=[pI])
                    P.add("dve", lambda e: e.tensor_tensor(out=tp[:, :].rearrange("p (j q) -> p j q", j=16),
                                                           in0=pI[:, :].rearrange("p (j q) -> p j q", j=16),
                                                           in1=ecum[:, hs, None].to_broadcast([128, 16, 64]), op=ALU.mult), reads=[pI, ecum], writes=[tp])
                    P.add("pool", lambda e: e.tensor_tensor(out=ypv[:, :], in0=ypv[:, :], in1=tp[:, :], op=ALU.add), reads=[ypv, tp], writes=[ypv])
                P.add("dve", lambda e: e.tensor_tensor(out=yy[:, :], in0=pY[:, :], in1=ypv[:, :], op=ALU.add), reads=[pY, ypv], writes=[yy])
                P.dma(ydr.ap()[r0:r0 + 128, g * 1024:(g + 1) * 1024], yy[:, :], reads=[yy], writes=[("ydr", i, g)], eng="act")
                for hh in range(2):
                    P.add("pe", lambda e, hh=hh: e.matmul(pI[:, hh * 512:(hh + 1) * 512], lhsT=b_[:, g * 128:(g + 1) * 128], rhs=xt[:, hh * 512:(hh + 1) * 512],
                                                          start=True, stop=True), reads=[b_, xt], writes=[pI])
                if first:
                    P.add("dve", lambda e: e.tensor_copy(out=stF[:, g, :], in_=pI[:, :]), reads=[pI], writes=[(stF.name, g)])
                else:
                    P.add("dve", lambda e: e.tensor_tensor(out=stF[:, g, :].rearrange("p (j q) -> p j q", j=16),
                                                            in0=stF[:, g, :].rearrange("p (j q) -> p j q", j=16),
                                                            in1=eclast[:, hs, None].to_broadcast([128, 16, 64]), op=ALU.mult),
                          reads=[(stF.name, g), eclast], writes=[(stF.name, g)])
                    P.add("dve", lambda e: e.tensor_tensor(out=stF[:, g, :], in0=stF[:, g, :], in1=pI[:, :], op=ALU.add),
                          reads=[(stF.name, g), pI], writes=[(stF.name, g)])
                P.add("act", lambda e: e.copy(out=stB[:, g, :], in_=stF[:, g, :]), reads=[(stF.name, g)], writes=[(stB.name, g)])

            for g_ in range(8):
                do_group(g_)

        for d_ in range(int(os.environ.get("SSD_NDIR", "2"))):
            order = [16, 17] + list(range(16)) if d_ == 0 else [17, 16] + list(range(15, -1, -1))
            for n_, i_ in enumerate(order[:int(os.environ.get("SSD_NCH", "18"))]):
                do_chunk(d_, i_, n_ == 0)
    P.barrier()


def pass_ssd_finish(C, ydr, szd, norm_g, ident_bf, gT):
    P = C.P
    with ExitStack() as es:
        idb = C.sb(es, [128, 128], BF16, "idb")
        ng = C.sb(es, [128, 64], F32, "ng")
        yt = [C.sb(es, [128, SSD_DI], F32, "yt") for _ in range(2)]
        zt = [C.sb(es, [128, SSD_DI], BF16, "zt") for _ in range(2)]
        junk = C.sb(es, [128, 1024], BF16, "junk")
        gb = [C.sb(es, [128, SSD_DI], BF16, "gb") for _ in range(2)]
        st = [C.sb(es, [128, 32], F32, "st") for _ in range(2)]
        hst = [C.sb(es, [128, 64, 128], BF16, "hst") for _ in range(2)]
        ptr = [C.ps(es, [128, 1024], BF16, "ptr") for _ in range(3)]
        P.dma(idb[:, :], ident_bf.ap(), writes=[idb])
        for c0_ in range(0, 64, 16):
            P.dma(ng[:, c0_:c0_ + 16], norm_g[c0_ * 128:(c0_ + 16) * 128].rearrange("(c p) -> p c", p=128), writes=[ng], slow=True)
        npt = [0]

        def do_tile(i):
            y, z_, g_, s_, hs = yt[i % 2], zt[i % 2], gb[i % 2], st[i % 2], hst[i % 2]
            P.dma(y[:, :], ydr.ap()[i * 128:(i + 1) * 128, :], reads=[("ydr", i, q) for q in range(8)], writes=[y])
            P.dma(z_[:, :], szd.ap()[i * 128:(i + 1) * 128, :], reads=[("sz", c0) for c0 in range(0, SSD_DI, 512)], writes=[z_])
            P.add("dve", lambda e: e.tensor_tensor(out=y[:, :], in0=y[:, :], in1=z_[:, :], op=ALU.mult), reads=[y, z_], writes=[y])
            for q in range(8):
                P.add("act", lambda e, q=q: e.activation(out=junk[:, :], in_=y[:, q * 1024:(q + 1) * 1024], func=AF.Square, accum_out=s_[:, q:q + 1]),
                      reads=[y], writes=[junk, s_])
            P.add("dve", lambda e: e.tensor_scalar(out=s_[:, 8:16], in0=s_[:, 0:8], scalar1=1.0 / 1024, scalar2=EPS, op0=ALU.mult, op1=ALU.add),
                  reads=[s_], writes=[s_])
            P.add("act", lambda e: e.activation(out=s_[:, 8:16], in_=s_[:, 8:16], func=AF.Sqrt), reads=[s_], writes=[s_])
            P.add("dve", lambda e: e.reciprocal(out=s_[:, 16:24], in_=s_[:, 8:16]), reads=[s_], writes=[s_])
            for q in range(8):
                P.add("dve" if q % 2 == 0 else "pool", lambda e, q=q: e.tensor_scalar(out=g_[:, q * 1024:(q + 1) * 1024], in0=y[:, q * 1024:(q + 1) * 1024],
                                                                                      scalar1=s_[:, 16 + q:17 + q], scalar2=None, op0=ALU.mult),
                      reads=[y, s_], writes=[g_])
            for q in range(8):
                pt = ptr[npt[0] % 3]
                npt[0] += 1
                for kk in range(8):
                    k = q * 8 + kk
                    P.add("pe", lambda e, kk=kk, k=k, pt=pt: e.transpose(out=pt[:, kk * 128:(kk + 1) * 128], in_=g_[:, k * 128:(k + 1) * 128],
                                                                         identity=idb[:, :]), reads=[g_, idb], writes=[pt])
                for kk in range(8):
                    k = q * 8 + kk
                    P.add("act", lambda e, kk=kk, k=k, pt=pt: e.activation(out=hs[:, k, :], in_=pt[:, kk * 128:(kk + 1) * 128], func=AF.Identity,
                                                                           scale=ng[:, k:k + 1]), reads=[pt, ng], writes=[hs])
            P.dma(gT.ap()[i], hs[:, :, :], reads=[hs], writes=[("aT", i)])

        for i_ in range(NTT):
            do_tile(i_)
    P.barrier()


TWO_PI = 2.0 * np.pi


def pass_s5_proj(C, hT, w_in, uT_d, u_tm):
    P = C.P

    def do_block(blk):
        with ExitStack() as es:
            act = C.sb(es, [128, 9, KC, 128], BF16, "act")
            rw = [C.sb(es, [128, 384], BF16, "rw") for _ in range(3)]
            uo = [C.sb(es, [128, 512], F32, "uo") for _ in range(3)]
            gps = [C.ps(es, [128, 512], F32, "gps") for _ in range(4)]
            load_act(C, act, hT, blk * 9, 9, "hT")
            cnt = [0, 0]

            def epi(pp, j, tt0, ntl):
                n = ntl * 128
                r = rw[cnt[0] % 3]
                cnt[0] += 1
                P.add("act", lambda e: e.copy(out=r[:, 0:n], in_=pp[:, 0:n]), reads=[pp], writes=[r])
                tg = (blk * 9 + tt0) * 128
                P.dma(uT_d.ap()[j, :, tg:tg + n], r[:, 0:n], reads=[r], writes=[("uT", 0)])

            gemm_ws(C, es, act, 9, KC, w_in, 0, D, epi, "w", psb=gps)

            def epu(pp, ti, c0):
                o = uo[cnt[1] % 3]
                cnt[1] += 1
                P.add("dve", lambda e: e.tensor_copy(out=o[:, :], in_=pp[:, 0:512]), reads=[pp], writes=[o])
                tg = (blk * 9 + ti) * 128
                P.dma(u_tm.ap()[tg:tg + 128, c0:c0 + 512], o[:, :], reads=[o], writes=[("u_tm", 0)])

            gemm_ts(C, es, act, 9, KC, w_in, 0, D, epu, psb=gps)
        P.barrier()

    for b_ in range(2):
        do_block(b_)


def s5_lambda_bar(P, lr, li, stp, ar, ai, t1, t2, ti):
    def K(a):
        return a.tensor
    P.add("act", lambda e: e.activation(out=stp, in_=stp, func=AF.Exp), reads=[K(stp)], writes=[K(stp)])
    P.add("dve", lambda e: e.tensor_tensor(out=t1, in0=lr, in1=stp, op=ALU.mult), reads=[K(lr), K(stp)], writes=[K(t1)])
    P.add("act", lambda e: e.activation(out=t1, in_=t1, func=AF.Exp), reads=[K(t1)], writes=[K(t1)])
    P.add("dve", lambda e: e.tensor_tensor(out=t2, in0=li, in1=stp, op=ALU.mult), reads=[K(li), K(stp)], writes=[K(t2)])

    def reduced_sin(dst, shift):
        P.add("dve", lambda e: e.tensor_scalar(out=dst, in0=t2, scalar1=shift, scalar2=1.0 / TWO_PI, op0=ALU.add, op1=ALU.mult),
              reads=[K(t2)], writes=[K(dst)])
        P.add("dve", lambda e: e.tensor_copy(out=ti, in_=dst), reads=[K(dst)], writes=[K(ti)])
        P.add("dve", lambda e: e.tensor_copy(out=dst, in_=ti), reads=[K(ti)], writes=[K(dst)])
        P.add("dve", lambda e: e.tensor_scalar(out=dst, in0=dst, scalar1=-TWO_PI, scalar2=shift, op0=ALU.mult, op1=ALU.add),
              reads=[K(dst)], writes=[K(dst)])
        P.add("dve", lambda e: e.tensor_tensor(out=dst, in0=dst, in1=t2, op=ALU.add), reads=[K(dst), K(t2)], writes=[K(dst)])
        for (thr, cmp_, adj) in ((np.pi, ALU.is_gt, -TWO_PI), (-np.pi, ALU.is_lt, TWO_PI)):
            P.add("dve", lambda e, thr=thr, cmp_=cmp_, adj=adj: e.tensor_scalar(out=stp, in0=dst, scalar1=thr, scalar2=adj, op0=cmp_, op1=ALU.mult),
                  reads=[K(dst)], writes=[K(stp)])
            P.add("dve", lambda e: e.tensor_tensor(out=dst, in0=dst, in1=stp, op=ALU.add), reads=[K(dst), K(stp)], writes=[K(dst)])
        P.add("act", lambda e: e.activation(out=dst, in_=dst, func=AF.Sin), reads=[K(dst)], writes=[K(dst)])

    reduced_sin(ai, 0.0)
    reduced_sin(ar, 0.5 * np.pi)
    P.add("dve", lambda e: e.tensor_tensor(out=ar, in0=ar, in1=t1, op=ALU.mult), reads=[K(ar), K(t1)], writes=[K(ar)])
    P.add("dve", lambda e: e.tensor_tensor(out=ai, in0=ai, in1=t1, op=ALU.mult), reads=[K(ai), K(t1)], writes=[K(ai)])


def pass_s5_scan(C, uT_d, lam_re, lam_im, log_dt, b_re, b_im, c_re, c_im, ident, rowmask, cmask, y5, dbg=None):
    P = C.P
    with ExitStack() as es:
        idt = C.sb(es, [128, 128], F32, "idt")
        rmk = C.sb(es, [128, 2], F32, "rmk")
        cmk = C.sb(es, [128, 128], F32, "cmk")
        CP = C.sb(es, [128, 128, 2, 32], BF16, "CP")
        BP = C.sb(es, [128, 2, KC, 2, 128], BF16, "BP")
        arS = C.sb(es, [128, 2, 128], F32, "arS")
        aiS = C.sb(es, [128, 2, 128], F32, "aiS")
        tt = [C.sb(es, [128, 128], F32, "tt") for _ in range(4)]
        px = [C.ps(es, [128, 512], F32, "px") for _ in range(2)]
        py = C.ps(es, [128, 2048], F32, "py")
        P.dma(idt[:, :], ident.ap(), writes=[idt])
        P.dma(rmk[:, :], rowmask.ap(), writes=[rmk])
        P.dma(cmk[:, :], cmask.ap(), writes=[cmk])
        with ExitStack() as es2:
            cin_ = C.sb(es2, [128, KC, 2, 64], F32, "cin")
            for ri, csrc in enumerate((c_re, c_im)):
                for dup in range(2):
                    for k0_ in range(0, KC, 8):
                        P.dma(cin_[:, k0_:k0_ + 8, dup, :],
                              csrc.rearrange("g c p -> (g c) p")[k0_ * 128:(k0_ + 8) * 128, :].rearrange("(k r) p -> r k p", r=128), writes=[cin_])
                P.add("dve", lambda e: e.tensor_tensor(out=cin_[:, :, :, :].rearrange("r k d p -> r k (d p)"),
                                                       in0=cin_[:, :, :, :].rearrange("r k d p -> r k (d p)"),
                                                       in1=cmk[:, None, :].to_broadcast([128, KC, 128]), op=ALU.mult), reads=[cin_, cmk], writes=[cin_])
                for kc in range(KC):
                    pp = px[kc % 2]
                    P.add("pe", lambda e, kc=kc, pp=pp: e.transpose(out=pp[:, 0:128], in_=cin_[:, kc, :, :].rearrange("r d p -> r (d p)"),
                                                                    identity=idt[:, :]), reads=[cin_, idt], writes=[pp])
                    P.add("act", lambda e, kc=kc, pp=pp, ri=ri: e.activation(out=CP[:, 4 * kc:4 * kc + 4, ri, :],
                                                                             in_=pp[:, 0:128].rearrange("p (q c) -> p q c", q=4),
                                                                             func=AF.Copy if ri == 0 else AF.Identity,
                                                                             scale=1.0 if ri == 0 else -1.0), reads=[pp], writes=[CP])
        P.barrier()
        chunks64 = {0: [2048 + 64 * i for i in range(4)] + [64 * i for i in range(32)],
                    1: [2048 + 64 * i for i in range(3, -1, -1)] + [64 * i for i in range(31, -1, -1)]}
        nst = [0]

        def prep_dir(d):
            with ExitStack() as es2:
                lr = C.sb(es2, [64, 256], F32, "lr")
                li = C.sb(es2, [64, 256], F32, "li")
                sp_ = C.sb(es2, [64, 256], F32, "sp")
                ar = C.sb(es2, [64, 256], F32, "ar")
                ai = C.sb(es2, [64, 256], F32, "ai")
                w1_ = C.sb(es2, [64, 256], F32, "w1")
                w2_ = C.sb(es2, [64, 256], F32, "w2")
                fr = C.sb(es2, [64, 256], F32, "fr")
                fi = C.sb(es2, [64, 256], F32, "fi")
                br = C.sb(es2, [64, 256, 16], F32, "br")
                bi = C.sb(es2, [64, 256, 16], F32, "bi")
                bbr = C.sb(es2, [64, 256, 16], F32, "bbr")
                bbi = C.sb(es2, [64, 256, 16], F32, "bbi")
                lrS = C.sb(es2, [128, 128], F32, "lrS")
                liS = C.sb(es2, [128, 128], F32, "liS")
                spS = C.sb(es2, [128, 128], F32, "spS")
                tiS = C.sb(es2, [128, 128], I32, "tiS")
                tiL = C.sb(es2, [64, 256], I32, "tiL")
                for q0 in range(0, 256, 64):
                    P.dma(lr[:, q0:q0 + 64], lam_re[d, q0:q0 + 64, :].rearrange("g p -> p g"), writes=[lr], slow=True)
                    P.dma(li[:, q0:q0 + 64], lam_im[d, q0:q0 + 64, :].rearrange("g p -> p g"), writes=[li], slow=True)
                P.dma(sp_[:, :], log_dt[d].partition_broadcast(64), writes=[sp_])
                for g0_ in range(0, 256, 32):
                    P.dma(br[:, g0_:g0_ + 32, :], b_re[g0_:g0_ + 32].rearrange("g p c -> p g c"), writes=[br])
                    P.dma(bi[:, g0_:g0_ + 32, :], b_im[g0_:g0_ + 32].rearrange("g p c -> p g c"), writes=[bi])
                for g2 in range(2):
                    for q0 in range(0, 128, 64):
                        P.dma(lrS[g2 * 64:(g2 + 1) * 64, q0:q0 + 64],
                              lam_re[d].rearrange("(q t) p -> t p q", t=2)[g2, :, q0:q0 + 64], writes=[lrS], slow=True)
                        P.dma(liS[g2 * 64:(g2 + 1) * 64, q0:q0 + 64],
                              lam_im[d].rearrange("(q t) p -> t p q", t=2)[g2, :, q0:q0 + 64], writes=[liS], slow=True)
                    P.dma(spS[g2 * 64:(g2 + 1) * 64, :], log_dt[d].rearrange("(q t) -> t q", t=2)[g2].partition_broadcast(64),
                          writes=[spS], slow=True)
                s5_lambda_bar(P, lrS[:, :], liS[:, :], spS[:, :], arS[:, d, :], aiS[:, d, :], tt[0][:, :], tt[1][:, :], tiS[:, :])
                s5_lambda_bar(P, lr[:, :], li[:, :], sp_[:, :], ar[:, :], ai[:, :], w1_[:, :], w2_[:, :], tiL[:, :])
                P.add("dve", lambda e: e.tensor_tensor(out=w1_[:, :], in0=lr[:, :], in1=lr[:, :], op=ALU.mult), reads=[lr], writes=[w1_])
                P.add("dve", lambda e: e.tensor_tensor(out=w2_[:, :], in0=li[:, :], in1=li[:, :], op=ALU.mult), reads=[li], writes=[w2_])
                P.add("dve", lambda e: e.tensor_tensor(out=w1_[:, :], in0=w1_[:, :], in1=w2_[:, :], op=ALU.add), reads=[w1_, w2_], writes=[w1_])
                P.add("dve", lambda e: e.reciprocal(out=w1_[:, :], in_=w1_[:, :]), reads=[w1_], writes=[w1_])
                P.add("dve", lambda e: e.tensor_scalar(out=ar[:, :], in0=ar[:, :], scalar1=-1.0, scalar2=None, op0=ALU.add), reads=[ar], writes=[ar])
                P.add("dve", lambda e: e.tensor_tensor(out=fr[:, :], in0=ar[:, :], in1=lr[:, :], op=ALU.mult), reads=[ar, lr], writes=[fr])
                P.add("dve", lambda e: e.tensor_tensor(out=w2_[:, :], in0=ai[:, :], in1=li[:, :], op=ALU.mult), reads=[ai, li], writes=[w2_])
                P.add("dve", lambda e: e.tensor_tensor(out=fr[:, :], in0=fr[:, :], in1=w2_[:, :], op=ALU.add), reads=[fr, w2_], writes=[fr])
                P.add("dve", lambda e: e.tensor_tensor(out=fr[:, :], in0=fr[:, :], in1=w1_[:, :], op=ALU.mult), reads=[fr, w1_], writes=[fr])
                P.add("dve", lambda e: e.tensor_tensor(out=fi[:, :], in0=ai[:, :], in1=lr[:, :], op=ALU.mult), reads=[ai, lr], writes=[fi])
                P.add("dve", lambda e: e.tensor_tensor(out=w2_[:, :], in0=ar[:, :], in1=li[:, :], op=ALU.mult), reads=[ar, li], writes=[w2_])
                P.add("dve", lambda e: e.tensor_tensor(out=fi[:, :], in0=fi[:, :], in1=w2_[:, :], op=ALU.subtract), reads=[fi, w2_], writes=[fi])
                P.add("dve", lambda e: e.tensor_tensor(out=fi[:, :], in0=fi[:, :], in1=w1_[:, :], op=ALU.mult), reads=[fi, w1_], writes=[fi])
                frb = fr[:, :, None].to_broadcast([64, 256, 16])
                fib = fi[:, :, None].to_broadcast([64, 256, 16])
                P.add("dve", lambda e: e.tensor_tensor(out=bbr[:, :, :], in0=br[:, :, :], in1=frb, op=ALU.mult), reads=[br, fr], writes=[bbr])
                P.add("dve", lambda e: e.tensor_tensor(out=bbi[:, :, :], in0=bi[:, :, :], in1=frb, op=ALU.mult), reads=[bi, fr], writes=[bbi])
                P.add("dve", lambda e: e.tensor_tensor(out=bi[:, :, :], in0=bi[:, :, :], in1=fib, op=ALU.mult), reads=[bi, fi, bbi], writes=[bi])
                P.add("dve", lambda e: e.tensor_tensor(out=br[:, :, :], in0=br[:, :, :], in1=fib, op=ALU.mult), reads=[br, fi, bbr], writes=[br])
                P.add("dve", lambda e: e.tensor_tensor(out=bbr[:, :, :], in0=bbr[:, :, :], in1=bi[:, :, :], op=ALU.subtract), reads=[bbr, bi], writes=[bbr])
                P.add("pool", lambda e: e.tensor_tensor(out=bbi[:, :, :], in0=bbi[:, :, :], in1=br[:, :, :], op=ALU.add), reads=[bbi, br], writes=[bbi])
                for ri, bsrc in enumerate((bbr, bbi)):
                    for kc in range(KC):
                        pp = px[kc % 2]
                        P.add("pe", lambda e, kc=kc, pp=pp, bsrc=bsrc: e.transpose(out=pp[:, 0:64], in_=bsrc[:, 8 * kc:8 * kc + 8, :].rearrange("p g c -> p (g c)"),
                                                                                   identity=idt[0:64, 0:64]), reads=[bsrc, idt], writes=[pp])
                        P.add("dve", lambda e, kc=kc, pp=pp, ri=ri: e.tensor_scalar(out=BP[:, d, kc, ri, 0:64], in0=pp[:, 0:64], scalar1=rmk[:, 0:1], scalar2=None,
                                                                                    op0=ALU.mult), reads=[pp, rmk], writes=[BP])
                        P.add("dve", lambda e, kc=kc, pp=pp, ri=ri: e.tensor_scalar(out=BP[:, d, kc, ri, 64:128], in0=pp[:, 0:64], scalar1=rmk[:, 1:2], scalar2=None,
                                                                                    op0=ALU.mult), reads=[pp, rmk], writes=[BP])
        for d_ in range(2):
            prep_dir(d_)
            P.barrier()
        if dbg is not None:
            P.dma(dbg["arS"].ap(), arS[:, :, :], reads=[arS], writes=["dbg_arS"], is_out=True)
            P.dma(dbg["aiS"].ap(), aiS[:, :, :], reads=[aiS], writes=["dbg_aiS"], is_out=True)
            P.dma(dbg["CP"].ap(), CP[:, :, :, :], reads=[CP], writes=["dbg_CP"], is_out=True)
            P.dma(dbg["BP"].ap(), BP[:, :, :, :, :], reads=[BP], writes=["dbg_BP"], is_out=True)
            P.barrier()
        Sr = C.sb(es, [128, 128], F32, "Sr")
        Si = C.sb(es, [128, 128], F32, "Si")
        X = C.sb(es, [128, 2, 128, 64], BF16, "X")
        Hr = C.sb(es, [128, 128, 64], F32, "Hr")
        Hi = C.sb(es, [128, 128, 64], F32, "Hi")
        uq = [C.sb(es, [128, KC, 64], BF16, "uq") for _ in range(4)]
        yo = [C.sb(es, [64, 2048], F32, "yo") for _ in range(2)]
        for uq_ in uq:
            P.add("pool", lambda e, uq_=uq_: e.memset(uq_[:, :, :], 0.0), writes=[uq_])

        def do_dir(d):
            P.add("dve", lambda e: e.memset(Sr[:, :], 0.0), writes=[Sr])
            P.add("pool", lambda e: e.memset(Si[:, :], 0.0), writes=[Si])

            def do_chunk(ci, t0):
                for qq in range(4):
                    P.dma(uq[qq][qq * 32:(qq + 1) * 32, :, :], uT_d.ap()[:, qq * 32:(qq + 1) * 32, t0:t0 + 64].rearrange("k r t -> r k t"),
                          reads=[("uT", 0)], writes=[uq[qq]])
                for b4 in range(32):
                    pp = px[b4 % 2]
                    for qq in range(4):
                        for ri in range(2):
                            sl = (qq * 2 + ri) * 64
                            P.add("pe", lambda e, pp=pp, qq=qq, ri=ri, sl=sl, b4=b4: e.matmul(pp[:, sl:sl + 64], lhsT=BP[:, d, b4, ri, :],
                                                                                              rhs=uq[qq][:, b4, :], start=True, stop=True),
                                  reads=[BP, uq[qq]], writes=[pp])
                    P.add("act", lambda e, pp=pp, b4=b4: e.copy(out=X[:, :, 4 * b4:4 * b4 + 4, :].rearrange("p r q t -> p q r t"),
                                                                in_=pp[:, :].rearrange("p (q r t) -> p q r t", q=4, r=2)), reads=[pp], writes=[X])
                if int(os.environ.get("S5_STOP", "9")) <= 1:
                    return
                order = range(64) if d == 0 else range(63, -1, -1)
                prev = None
                for t in order:
                    sr_p = Sr[:, :] if prev is None else Hr[:, :, prev]
                    si_p = Si[:, :] if prev is None else Hi[:, :, prev]
                    a_, b_, c_, d__ = tt
                    rd = [Sr, Si] if prev is None else [Hr, Hi]
                    P.add("dve", lambda e, sr_p=sr_p: e.tensor_tensor(out=a_[:, :], in0=arS[:, d, :], in1=sr_p, op=ALU.mult), reads=[arS] + rd, writes=[a_])
                    P.add("pool", lambda e, si_p=si_p: e.tensor_tensor(out=b_[:, :], in0=aiS[:, d, :], in1=si_p, op=ALU.mult), reads=[aiS] + rd, writes=[b_])
                    P.add("pool", lambda e, si_p=si_p: e.tensor_tensor(out=c_[:, :], in0=arS[:, d, :], in1=si_p, op=ALU.mult), reads=[arS] + rd, writes=[c_])
                    P.add("dve", lambda e, sr_p=sr_p: e.tensor_tensor(out=d__[:, :], in0=aiS[:, d, :], in1=sr_p, op=ALU.mult), reads=[aiS] + rd, writes=[d__])
                    P.add("dve", lambda e: e.tensor_tensor(out=a_[:, :], in0=a_[:, :], in1=b_[:, :], op=ALU.subtract), reads=[a_, b_], writes=[a_])
                    P.add("pool", lambda e: e.tensor_tensor(out=c_[:, :], in0=c_[:, :], in1=d__[:, :], op=ALU.add), reads=[c_, d__], writes=[c_])
                    P.add("dve", lambda e, t=t: e.tensor_tensor(out=Hr[:, :, t], in0=a_[:, :], in1=X[:, 0, :, t], op=ALU.add), reads=[a_, X], writes=[Hr])
                    P.add("pool", lambda e, t=t: e.tensor_tensor(out=Hi[:, :, t], in0=c_[:, :], in1=X[:, 1, :, t], op=ALU.add), reads=[c_, X], writes=[Hi])
                    prev = t
                P.add("dve", lambda e, prev=prev: e.tensor_copy(out=Sr[:, :], in_=Hr[:, :, prev]), reads=[Hr], writes=[Sr])
                P.add("pool", lambda e, prev=prev: e.tensor_copy(out=Si[:, :], in_=Hi[:, :, prev]), reads=[Hi], writes=[Si])
                if int(os.environ.get("S5_STOP", "9")) <= 2:
                    return
                P.add("dve", lambda e: e.tensor_copy(out=X[:, 0, :, :], in_=Hr[:, :, :]), reads=[Hr, X], writes=[X])
                P.add("pool", lambda e: e.tensor_copy(out=X[:, 1, :, :], in_=Hi[:, :, :]), reads=[Hi, X], writes=[X])
                for half in range(2):
                    o = yo[nst[0] % 2]
                    nst[0] += 1
                    for qh in range(64):
                        q = half * 64 + qh
                        P.add("pe", lambda e, q=q, qh=qh: e.matmul(py[0:64, qh * 32:(qh + 1) * 32], lhsT=X[:, 0, q, :], rhs=CP[:, q, 0, :], start=True, stop=False),
                              reads=[X, CP], writes=[py])
                        P.add("pe", lambda e, q=q, qh=qh: e.matmul(py[0:64, qh * 32:(qh + 1) * 32], lhsT=X[:, 1, q, :], rhs=CP[:, q, 1, :], start=False, stop=True),
                              reads=[X, CP], writes=[py])
                    dst = y5.ap()[t0:t0 + 64, half * 2048:(half + 1) * 2048]
                    if d == 0:
                        P.add("act", lambda e, o=o: e.copy(out=o[:, :], in_=py[0:64, :]), reads=[py], writes=[o])
                    else:
                        P.dma(o[:, :], dst, reads=[("y5", t0, half)], writes=[o])
                        P.add("dve", lambda e, o=o: e.tensor_tensor(out=o[:, :], in0=o[:, :], in1=py[0:64, :], op=ALU.add), reads=[o, py], writes=[o])
                    P.dma(dst, o[:, :], reads=[o], writes=[("y5", t0, half)], eng="act")

            for ci_, t0_ in enumerate(chunks64[d][:int(os.environ.get("S5_NCH", "36"))]):
                do_chunk(ci_, t0_)

        for d_ in range(int(os.environ.get("S5_NDIR", "2"))):
            do_dir(d_)
    P.barrier()


def pass_s5_glu_in(C, y5, u_tm, d_skip, ident_bf, gT5):
    P = C.P
    with ExitStack() as es:
        idb = C.sb(es, [128, 128], BF16, "idb")
        dsk = C.sb(es, [128, D], F32, "dsk")
        yt = [C.sb(es, [128, D], F32, "yt") for _ in range(2)]
        ut = [C.sb(es, [128, D], F32, "ut") for _ in range(2)]
        w_ = [C.sb(es, [128, D], F32, "w") for _ in range(2)]
        gb = [C.sb(es, [128, D], BF16, "gb") for _ in range(2)]
        hst = [C.sb(es, [128, KC, 128], BF16, "hst") for _ in range(2)]
        ptr = [C.ps(es, [128, 1024], BF16, "ptr") for _ in range(2)]
        P.dma(idb[:, :], ident_bf.ap(), writes=[idb])
        P.dma(dsk[:, :], d_skip.partition_broadcast(128), writes=[dsk])
        npt = [0]

        def do_tile(i):
            y, u, w, g, hs = yt[i % 2], ut[i % 2], w_[i % 2], gb[i % 2], hst[i % 2]
            rows = slice(i * 128, (i + 1) * 128)
            t64 = [(i * 128 + 64 * a_, h_) for a_ in range(2) for h_ in range(2)]
            P.dma(y[:, :], y5.ap()[rows, :], reads=[("y5", t, h) for (t, h) in t64], writes=[y])
            P.dma(u[:, :], u_tm.ap()[rows, :], reads=[("u_tm", 0)], writes=[u])
            P.add("dve", lambda e: e.tensor_tensor(out=u[:, :], in0=u[:, :], in1=dsk[:, :], op=ALU.mult), reads=[u, dsk], writes=[u])
            P.add("pool", lambda e: e.tensor_tensor(out=y[:, :], in0=y[:, :], in1=u[:, :], op=ALU.add), reads=[y, u], writes=[y])
            P.add("act", lambda e: e.activation(out=w[:, :], in_=y[:, :], func=AF.Square), reads=[y], writes=[w])
            P.add("dve", lambda e: e.tensor_scalar(out=w[:, :], in0=w[:, :], scalar1=0.044715, scalar2=1.0, op0=ALU.mult, op1=ALU.add), reads=[w], writes=[w])
            P.add("pool", lambda e: e.tensor_tensor(out=w[:, :], in0=w[:, :], in1=y[:, :], op=ALU.mult), reads=[w, y], writes=[w])
            P.add("act", lambda e: e.activation(out=w[:, :], in_=w[:, :], func=AF.Sigmoid, scale=1.5957691216057308), reads=[w], writes=[w])
            P.add("dve", lambda e: e.tensor_tensor(out=g[:, :], in0=w[:, :], in1=y[:, :], op=ALU.mult), reads=[w, y], writes=[g])
            for q in range(4):
                pt = ptr[npt[0] % 2]
                npt[0] += 1
                for kk in range(8):
                    k = q * 8 + kk
                    P.add("pe", lambda e, kk=kk, k=k, pt=pt: e.transpose(out=pt[:, kk * 128:(kk + 1) * 128], in_=g[:, k * 128:(k + 1) * 128],
                                                                         identity=idb[:, :]), reads=[g, idb], writes=[pt])
                P.add("act", lambda e, q=q, pt=pt: e.copy(out=hs[:, q * 8:(q + 1) * 8, :], in_=pt[:, :].rearrange("p (k t) -> p k t", k=8)),
                      reads=[pt], writes=[hs])
            P.dma(gT5.ap()[i], hs[:, :, :], reads=[hs], writes=[("aT", i)])

        for i_ in range(NTT):
            do_tile(i_)
    P.barrier()


_W_SHAPES = {
    "mod_down": [4, D, 512], "mod_up": [4, 512, 6 * D], "mod_b": [4, 6 * D], "norm1_g": [4, D], "norm2_g": [4, D], "c_ctx": [D],
    "swa_w_in": [1, D, 6144], "swa_q_g": [1, 128], "swa_k_g": [1, 128], "swa_sinks": [1, 32], "swa_w_out": [1, D, D],
    "ssd_w_in": [1, D, 18688], "ssd_conv_w": [1, 3, 10240], "ssd_conv_b": [1, 10240], "ssd_dt_bias": [1, 2, 128], "ssd_a_log": [1, 2, 128],
    "ssd_d": [1, 128], "ssd_norm_g": [1, 8192], "ssd_w_out": [1, 8192, D],
    "s5_w_in": [1, D, D], "s5_lam_re": [1, 2, 256, 64], "s5_lam_im": [1, 2, 256, 64], "s5_log_dt": [1, 2, 256],
    "s5_b_re": [1, 256, 64, 16], "s5_b_im": [1, 256, 64, 16], "s5_c_re": [1, 256, 16, 64], "s5_c_im": [1, 256, 16, 64], "s5_d": [1, D],
    "s5_w_glu": [1, D, 2 * D],
    "mla_w_in": [1, D, 1600], "mla_q_a_g": [1, 1024], "mla_kv_a_g": [1, 512], "mla_w_uq": [1, 1024, 6144], "mla_w_ukv": [1, 512, 8192],
    "mla_q_g": [1, 192], "mla_k_g": [1, 192], "mla_w_out": [1, D, D],
    "moe_w_group": [4, D, 4], "moe_b_group": [4, 4], "moe_w_expert": [4, D, 32], "moe_b_expert": [4, 32],
    "moe_w1": [4, 32, D, 256], "moe_w3": [4, 32, D, 256], "moe_w2": [4, 32, 256, D],
}


def build_program(layers=(0, 1, 2, 3)):
    nc = bass.Bass("TRN2", target_bir_lowering=False)
    C = Ctx(nc)
    P = C.P
    hc = host_consts()

    def inp(name, shape, dt=F32):
        return nc.dram_tensor(name, list(shape), dt, kind="ExternalInput")

    x = inp("x", [NL, D])
    ctx = inp("ctx", [NCX, D])
    c_b = inp("c_b", [D])
    W = {k: inp(k, v) for k, v in _W_SHAPES.items()}
    cin = {k: inp(k, v.shape, F32 if v.dtype == np.float32 else BF16) for k, v in hc.items()}
    out = nc.dram_tensor("out", [NL, D], F32, kind="ExternalOutput")
    xres = nc.dram_tensor("xres", [NT, D], F32)
    modv = nc.dram_tensor("modv", [4, 2, 6 * D], F32)
    hT = nc.dram_tensor("hT", [NTT, 128, KC, 128], BF16)
    aT = nc.dram_tensor("aT", [NTT, 128, 64, 128], BF16)
    aT32 = nc.dram_tensor("aT32", [NTT, 128, KC, 128], BF16)
    for i in range(NTT):
        src = x.ap()[i * 128:(i + 1) * 128, :] if i < 16 else ctx.ap()[(i - 16) * 128:(i - 15) * 128, :]
        P.dma(xres.ap()[i * 128:(i + 1) * 128, :], src, writes=[("xres", i)])
    pass_adaln(C, c_b, W["c_ctx"], W["mod_down"], W["mod_up"], W["mod_b"], modv, cin["ident"], layers=layers)
    for l in layers:
        last = l == 3
        pass_norm(C, xres, modv, l, W["norm1_g"].ap()[l], 0, 1, hT, cin["ident_bf"])
        if l == 0:
            qT = nc.dram_tensor("qT", [40, 128, NT], BF16)
            Vd = nc.dram_tensor("Vd_swa", [NT, 1024], BF16)
            pass_swa_proj(C, hT, W["swa_w_in"].ap()[0], W["swa_q_g"].ap()[0], W["swa_k_g"].ap()[0], cin["swa_cos"], cin["swa_sin"],
                          cin["swa_rot"], qT, Vd)
            pass_swa_attn(C, qT, Vd, W["swa_sinks"].ap()[0], cin["maskP"], cin["maskN"], aT32)
            pass_outproj(C, aT32, KC, W["swa_w_out"].ap()[0], D, modv, l, 2, xres, 9)
        elif l == 1:
            xbc_raw = nc.dram_tensor("xbc_raw", [80, 128, NT], F32)
            szd = nc.dram_tensor("szd", [NT, 8192], BF16)
            dtd = nc.dram_tensor("dtd", [NT, 256], F32)
            xs_tm = nc.dram_tensor("xs_tm", [NT, 8192], BF16)
            B_tm = nc.dram_tensor("B_tm", [NT, 1024], BF16)
            BCT = nc.dram_tensor("BCT", [16, 128, NT], BF16)
            ydr = nc.dram_tensor("ydr", [NT, 8192], F32)
            pass_ssd_proj(C, hT, W["ssd_w_in"].ap()[0], xbc_raw, szd, dtd)
            pass_ssd_conv(C, xbc_raw, W["ssd_conv_w"].ap()[0], W["ssd_conv_b"].ap()[0], cin["ident_bf"], xs_tm, B_tm, BCT)
            pass_ssd_scan(C, xs_tm, B_tm, BCT, dtd, W["ssd_dt_bias"].ap()[0], W["ssd_a_log"].ap()[0], W["ssd_d"].ap()[0],
                          cin["ident"], cin["ident_bf"], cin["ssd_U"], cin["ssd_negm"], ydr)
            pass_ssd_finish(C, ydr, szd, W["ssd_norm_g"].ap()[0], cin["ident_bf"], aT)
            pass_outproj(C, aT, 64, W["ssd_w_out"].ap()[0], D, modv, l, 2, xres, 4, cb=256)
        elif l == 2:
            uT_d = nc.dram_tensor("uT_d", [32, 128, NT], BF16)
            u_tm = nc.dram_tensor("u_tm", [NT, D], F32)
            y5 = nc.dram_tensor("y5", [NT, D], F32)
            pass_s5_proj(C, hT, W["s5_w_in"].ap()[0], uT_d, u_tm)
            pass_s5_scan(C, uT_d, W["s5_lam_re"].ap()[0], W["s5_lam_im"].ap()[0], W["s5_log_dt"].ap()[0], W["s5_b_re"].ap()[0],
                         W["s5_b_im"].ap()[0], W["s5_c_re"].ap()[0], W["s5_c_im"].ap()[0], cin["ident"], cin["s5_rowmask"], cin["s5_cmask"], y5)
            pass_s5_glu_in(C, y5, u_tm, W["s5_d"].ap()[0], cin["ident_bf"], aT32)
            pass_outproj(C, aT32, KC, W["s5_w_glu"].ap()[0], D, modv, l, 2, xres, 9, glu=True, cb=256)
        else:
            cqT = nc.dram_tensor("cqT", [NTT, 128, 8, 128], BF16)
            ckvT = nc.dram_tensor("ckvT", [NTT, 128, 4, 128], BF16)
            krT = nc.dram_tensor("krT", [64, NT], F32)
            QN = nc.dram_tensor("QN", [32, 128, NT], BF16)
            QR = nc.dram_tensor("QR", [32, 64, NT], BF16)
            KN = nc.dram_tensor("KN", [32, 128, NT], BF16)
            KR = nc.dram_tensor("KR", [32, 64, NT], BF16)
            Vm = nc.dram_tensor("Vd_mla", [NT, 4096], BF16)
            pass_mla_a(C, hT, W["mla_w_in"].ap()[0], W["mla_q_a_g"].ap()[0], W["mla_kv_a_g"].ap()[0], cqT, ckvT, krT)
            pass_mla_b(C, cqT, ckvT, krT, W["mla_w_uq"].ap()[0], W["mla_w_ukv"].ap()[0], W["mla_q_g"].ap()[0], W["mla_k_g"].ap()[0],
                       cin["mla_cos"], cin["mla_sin"], cin["mla_rot"], QN, QR, KN, KR, Vm)
            pass_mla_attn(C, QN, QR, KN, KR, Vm, aT32, ctx_out=False)
            pass_outproj(C, aT32, KC, W["mla_w_out"].ap()[0], D, modv, l, 2, xres, 9)
        pass_norm(C, xres, modv, l, W["norm2_g"].ap()[l], 3, 4, hT, cin["ident_bf"])
        pass_moe(C, hT, W["moe_w_group"].ap()[l], W["moe_b_group"].ap()[l], W["moe_w_expert"].ap()[l], W["moe_b_expert"].ap()[l],
                 W["moe_w1"].ap()[l], W["moe_w3"].ap()[l], W["moe_w2"].ap()[l], modv, l, xres, cin["ident"],
                 groups=[(0, 4), (4, 4), (8, 4), (12, 4)] if last else None)
    for i in range(16):
        P.dma(out.ap()[i * 128:(i + 1) * 128, :], xres.ap()[i * 128:(i + 1) * 128, :], reads=[("xres", i)], writes=[("out", i)], is_out=True)
    P.emit()
    return nc, hc


def kernel(**inputs):
    nc, hc = build_program()
    shared = {k: np.ascontiguousarray(np.asarray(inputs[k], dtype=np.float32)) for k in _W_SHAPES}
    shared.update(hc)
    x = np.asarray(inputs["x"], dtype=np.float32)
    c = np.asarray(inputs["c"], dtype=np.float32)
    ctx = np.asarray(inputs["ctx"], dtype=np.float32)
    in_maps = []
    for b in range(8):
        m = dict(shared)
        m["x"] = np.ascontiguousarray(x[b])
        m["ctx"] = np.ascontiguousarray(ctx[b])
        m["c_b"] = np.ascontiguousarray(c[b])
        in_maps.append(m)
    res = run_bass_kernel_spmd(nc, in_maps, core_ids=list(range(8)))
    return np.stack([np.asarray(r["out"], dtype=np.float32) for r in res.results], 0)
```

```python
import os
import numpy as np
from contextlib import ExitStack
import concourse.bass as bass
import concourse.mybir as mybir
from concourse.bass_utils import run_bass_kernel_spmd

F32 = mybir.dt.float32
BF16 = mybir.dt.bfloat16
I32 = mybir.dt.int32
ALU = mybir.AluOpType
AF = mybir.ActivationFunctionType
AX = mybir.AxisListType

D = 4096
NL = 2048
NCX = 256
NT = NL + NCX
NTT = NT // 128
KC = D // 128
EPS = 1e-6
N_DMA_SEMS = 24


class Prog:
    def __init__(self, nc):
        self.nc = nc
        self.ops = []
        self.st = {}
        self.bar = {}
        self.out_dmas = []
        self.psum = set()

    def _prune(self, lst):
        last = {}
        out = []
        for o in lst:
            op = self.ops[o]
            if op[3]:
                out.append(o)
            else:
                last[op[0]] = o
        return out + list(last.values())

    def add(self, eng, fn, reads=(), writes=(), dma=False, is_out=False):
        oid = len(self.ops)
        deps = set()
        reads = [k.name if hasattr(k, "name") else k for k in reads]
        writes = [k.name if hasattr(k, "name") else k for k in writes]
        writes = writes + [k for k in reads if k in self.psum and k not in writes]
        for k in reads:
            w, r, pv = self.st.setdefault(k, [[], [], []])
            deps.update(w)
        for k in writes:
            w, r, pv = self.st.setdefault(k, [[], [], []])
            if r:
                deps.update(w)
                deps.update(r)
            else:
                deps.update(pv)
                deps.update(o for o in w if not (eng == "pe" and self.ops[o][0] == "pe") and not (dma and self.ops[o][3]))
        if eng in self.bar:
            deps.update(self.bar.pop(eng))
        self.ops.append([eng, fn, deps, dma, False, (tuple(reads), tuple(writes))])
        for k in reads:
            s = self.st[k]
            s[1].append(oid)
            s[1] = self._prune(s[1])
        for k in writes:
            s = self.st[k]
            if s[1]:
                s[2] = self._prune(list(s[0]) + [x for x in s[1] if x != oid])
                s[0] = [oid]
                s[1] = []
            else:
                s[0].append(oid)
                s[0] = self._prune(s[0])
        deps.discard(oid)
        if is_out:
            self.out_dmas.append(oid)
        return oid

    def barrier(self, engs=None):
        last = {}
        dmas = []
        for i, op in enumerate(self.ops):
            if op[3]:
                dmas.append(i)
            else:
                last[op[0]] = i
        start = getattr(self, "_bar_start", 0)
        dmas = [d for d in dmas if d >= start]
        self._bar_start = len(self.ops)
        deps = set(dmas) | set(last.values())
        for e in (engs or ("pe", "act", "dve", "pool", "sp")):
            self.bar[e] = set(deps) | self.bar.get(e, set())
        if engs is None:
            self.st = {}

    def dma(self, out, in_, reads=(), writes=(), eng="sp", is_out=False, slow=False):
        kw = {"allow_slow_non_contiguous": True} if slow else {}
        return self.add(eng, lambda e: e.dma_start(out=out, in_=in_, **kw), reads, writes, dma=True, is_out=is_out)

    def emit(self):
        nc = self.nc
        ops = self.ops
        for op in ops:
            for d in op[2]:
                ops[d][4] = True
        for d in self.out_dmas:
            ops[d][4] = True
        engs = ["pe", "act", "dve", "pool", "sp"]
        with ExitStack() as es:
            csem = {e: es.enter_context(nc.semaphore("c_" + e)) for e in engs}
            dsem = [es.enter_context(nc.semaphore("d%d" % i)) for i in range(N_DMA_SEMS)]
            sig = {}
            ccount = {e: 0 for e in engs}
            duse = [0] * N_DMA_SEMS
            nd = 0
            dprev = {}
            nsw = 0
            nhw = 0
            for i, op in enumerate(ops):
                if op[3]:
                    if op[0] == "pool":
                        s = nsw % 8
                        nsw += 1
                    else:
                        s = 8 + nhw % (N_DMA_SEMS - 8)
                        nhw += 1
                    nd += 1
                    duse[s] += 1
                    sig[i] = (("d", s), 16 * duse[s])
                    op[4] = True
                elif op[4]:
                    ccount[op[0]] += 1
                    sig[i] = (("c", op[0]), ccount[op[0]])
            self.stats = (dict(ccount), nd)

            def semobj(k):
                return csem[k[1]] if k[0] == "c" else dsem[k[1]]

            final_waits = [sig[d] for d in self.out_dmas]

            def run(engname, e):
                waited = {}

                def wait(k, v):
                    if waited.get(k, 0) < v:
                        e.wait_ge(semobj(k), v)
                        waited[k] = v
                for i, op in enumerate(ops):
                    if op[0] != engname:
                        continue
                    for d in sorted(op[2]):
                        k, v = sig[d]
                        wait(k, v)
                    if op[3]:
                        k, v = sig[i]
                        if v > 16:
                            wait(k, v - 16)
                    ins = op[1](e)
                    if op[4]:
                        k, v = sig[i]
                        ins.then_inc(semobj(k), 16 if op[3] else 1)
                if engname == "sp":
                    for k, v in final_waits:
                        wait(k, v)

            with nc.Block() as block:
                @block.tensor
                def _(e):
                    run("pe", e)

                @block.scalar
                def _(e):
                    run("act", e)

                @block.vector
                def _(e):
                    run("dve", e)

                @block.gpsimd
                def _(e):
                    run("pool", e)

                @block.sync
                def _(e):
                    run("sp", e)


class Ctx:
    def __init__(self, nc):
        self.nc = nc
        self.P = Prog(nc)
        self.n = 0
        self.dram = {}

    def name(self, s):
        self.n += 1
        return "%s_%d" % (s, self.n)

    def sb(self, es, shape, dt, name="t"):
        return es.enter_context(self.nc.sbuf_tensor(self.name(name), list(shape), dt))

    def ps(self, es, shape, dt=F32, name="ps"):
        t = es.enter_context(self.nc.psum_tensor(self.name(name), list(shape), dt))
        self.P.psum.add(t.name)
        return t

    def dr(self, name, shape, dt, kind="Internal"):
        t = self.nc.dram_tensor(name, list(shape), dt, kind=kind)
        self.dram[name] = t
        return t


def pass_adaln(C, c_b, c_ctx, mod_down, mod_up, mod_b, modv, ident, layers=range(4)):
    P = C.P
    nc = C.nc
    with ExitStack() as es:
        cT = C.sb(es, [128, 2, KC], F32, "cT")
        sT = C.sb(es, [128, 2, KC], F32, "sT")
        L = C.sb(es, [128, KC, 128], F32, "L")
        idt = C.sb(es, [128, 128], F32, "idt")
        wd = [C.sb(es, [128, 8, 512], F32, "wd") for _ in range(2)]
        tsb = C.sb(es, [128, 512], F32, "tsb")
        tT = C.sb(es, [128, 4, 128], F32, "tT")
        wu = [C.sb(es, [128, 4, 512], F32, "wu") for _ in range(3)]
        bb = [C.sb(es, [128, 512], F32, "bb") for _ in range(3)]
        osb = [C.sb(es, [128, 512], F32, "osb") for _ in range(3)]
        pdn = C.ps(es, [128, 512], F32, "pdn")
        ptr = C.ps(es, [128, 512], F32, "ptr")
        pup = [C.ps(es, [128, 512], F32, "pup") for _ in range(2)]

        P.dma(idt[:, :], ident.ap(), writes=[idt])
        P.dma(cT[:, 0, :], c_b.ap().rearrange("(k p) -> p k", p=128), writes=[cT], slow=True)
        P.dma(cT[:, 1, :], c_ctx.ap().rearrange("(k p) -> p k", p=128), writes=[cT], slow=True)
        P.add("act", lambda e: e.activation(out=sT[:, :, :], in_=cT[:, :, :], func=AF.Silu), reads=[cT], writes=[sT])
        P.add("dve", lambda e: e.tensor_copy(out=L[:, :, 0:64], in_=sT[:, 0, :, None].to_broadcast([128, KC, 64])),
              reads=[sT], writes=[L])
        P.add("dve", lambda e: e.tensor_copy(out=L[:, :, 64:128], in_=sT[:, 1, :, None].to_broadcast([128, KC, 64])),
              reads=[sT], writes=[L])
        nw = 0
        nu = 0
        for l in layers:
            for q in range(4):
                w = wd[nw % 2]
                nw += 1
                P.dma(w[:, :, :], mod_down.ap()[l, q * 1024:(q + 1) * 1024, :].rearrange("(k p) r -> p k r", p=128),
                      writes=[w])
                for kk in range(8):
                    k = q * 8 + kk
                    P.add("pe", lambda e, w=w, kk=kk, k=k: e.matmul(pdn[:, :], lhsT=L[:, k, :], rhs=w[:, kk, :],
                                                                      start=(k == 0), stop=(k == KC - 1)),
                          reads=[L, w], writes=[pdn])
            P.add("dve", lambda e: e.tensor_copy(out=tsb[:, :], in_=pdn[:, :]), reads=[pdn], writes=[tsb])
            for j in range(4):
                P.add("pe", lambda e, j=j: e.transpose(out=ptr[:, j * 128:(j + 1) * 128], in_=tsb[:, j * 128:(j + 1) * 128],
                                                       identity=idt[:, :]),
                      reads=[tsb, idt], writes=[ptr])
            P.add("dve", lambda e: e.tensor_copy(out=tT[:, :, :], in_=ptr[:, :].rearrange("p (j m) -> p j m", j=4)),
                  reads=[ptr], writes=[tT])
            for n in range(48):
                w = wu[nu % 3]
                b = bb[nu % 3]
                o = osb[nu % 3]
                pp = pup[nu % 2]
                nu += 1
                P.dma(w[:, :, :], mod_up.ap()[l, :, n * 512:(n + 1) * 512].rearrange("(k p) c -> p k c", p=128),
                      writes=[w])
                P.dma(b[:, :], mod_b.ap()[l, n * 512:(n + 1) * 512].partition_broadcast(128), writes=[b])
                for k in range(4):
                    P.add("pe", lambda e, w=w, k=k, pp=pp: e.matmul(pp[:, :], lhsT=tT[:, k, :], rhs=w[:, k, :],
                                                                    start=(k == 0), stop=(k == 3)),
                          reads=[tT, w], writes=[pp])
                P.add("dve", lambda e, o=o, pp=pp, b=b: e.tensor_tensor(out=o[:, :], in0=pp[:, :], in1=b[:, :], op=ALU.add),
                      reads=[pp, b], writes=[o])
                P.dma(modv.ap()[l, 0, n * 512:(n + 1) * 512], o[0:1, :], reads=[o], writes=[("modv", l)])
                P.dma(modv.ap()[l, 1, n * 512:(n + 1) * 512], o[64:65, :], reads=[o], writes=[("modv", l)])
    P.barrier()


def pass_norm(C, xres, modv, l, gvec, i_shift, i_scale, hT, ident_bf, tiles=range(NTT)):
    P = C.P
    with ExitStack() as es:
        idb = C.sb(es, [128, 128], BF16, "idb")
        g = C.sb(es, [128, D], F32, "g")
        A = [C.sb(es, [128, D], F32, "A") for _ in range(2)]
        B = [C.sb(es, [128, D], F32, "B") for _ in range(2)]
        xt = [C.sb(es, [128, D], F32, "xt") for _ in range(2)]
        junk = C.sb(es, [128, D], BF16, "junk")
        hb = [C.sb(es, [128, D], BF16, "hb") for _ in range(2)]
        st = [C.sb(es, [128, 4], F32, "st") for _ in range(2)]
        hst = [C.sb(es, [128, KC, 128], BF16, "hst") for _ in range(2)]
        ptr = [C.ps(es, [128, 1024], BF16, "ptr") for _ in range(2)]
        P.dma(idb[:, :], ident_bf.ap(), writes=[idb])
        P.dma(g[:, :], gvec.partition_broadcast(128), writes=[g])
        for s in range(2):
            P.dma(A[s][:, :], modv.ap()[l, s, i_scale * D:(i_scale + 1) * D].partition_broadcast(128),
                  reads=[("modv", l)], writes=[A[s]])
            P.dma(B[s][:, :], modv.ap()[l, s, i_shift * D:(i_shift + 1) * D].partition_broadcast(128),
                  reads=[("modv", l)], writes=[B[s]])
            P.add("dve", lambda e, s=s: e.scalar_tensor_tensor(out=A[s][:, :], in0=A[s][:, :], scalar=1.0, in1=g[:, :],
                                                               op0=ALU.add, op1=ALU.mult),
                  reads=[A[s], g], writes=[A[s]])
        n = 0
        npt = 0
        for i in tiles:
            s = 0 if i < NL // 128 else 1
            x = xt[n % 2]
            h = hb[n % 2]
            sv = st[n % 2]
            hs = hst[n % 2]
            n += 1
            P.dma(x[:, :], xres.ap()[i * 128:(i + 1) * 128, :], reads=[("xres", i)], writes=[x])
            P.add("act", lambda e, x=x, sv=sv: e.activation(out=junk[:, :], in_=x[:, :], func=AF.Square,
                                                             accum_out=sv[:, 0:1]),
                  reads=[x], writes=[junk, sv])
            P.add("dve", lambda e, sv=sv: e.tensor_scalar(out=sv[:, 1:2], in0=sv[:, 0:1], scalar1=1.0 / D, scalar2=EPS,
                                                          op0=ALU.mult, op1=ALU.add), reads=[sv], writes=[sv])
            P.add("act", lambda e, sv=sv: e.activation(out=sv[:, 3:4], in_=sv[:, 1:2], func=AF.Sqrt), reads=[sv], writes=[sv])
            P.add("dve", lambda e, sv=sv: e.reciprocal(out=sv[:, 2:3], in_=sv[:, 3:4]), reads=[sv], writes=[sv])
            P.add("dve", lambda e, x=x, sv=sv, s=s: e.scalar_tensor_tensor(out=x[:, :], in0=x[:, :], scalar=sv[:, 2:3],
                                                                           in1=A[s][:, :], op0=ALU.mult, op1=ALU.mult),
                  reads=[x, sv, A[s]], writes=[x])
            P.add("pool", lambda e, x=x, h=h, s=s: e.tensor_tensor(out=h[:, :], in0=x[:, :], in1=B[s][:, :], op=ALU.add),
                  reads=[x, B[s]], writes=[h])
            for q in range(4):
                pt = ptr[npt % 2]
                npt += 1
                for kk in range(8):
                    k = q * 8 + kk
                    P.add("pe", lambda e, pt=pt, kk=kk, k=k, h=h: e.transpose(out=pt[:, kk * 128:(kk + 1) * 128],
                                                                             in_=h[:, k * 128:(k + 1) * 128],
                                                                             identity=idb[:, :]),
                          reads=[h, idb], writes=[pt])
                P.add("act", lambda e, pt=pt, hs=hs, q=q: e.copy(out=hs[:, q * 8:(q + 1) * 8, :],
                                                                  in_=pt[:, :].rearrange("p (k t) -> p k t", k=8)),
                      reads=[pt], writes=[hs])
            P.dma(hT.ap()[i], hs[:, :, :], reads=[hs], writes=[("hT", i)])
    P.barrier()


def load_act(C, dst, src, t0, nt, key):
    for i in range(nt):
        C.P.dma(dst[:, i, :, :], src.ap()[t0 + i], reads=[(key, t0 + i)], writes=[dst])


def gemm_ws(C, es, act, nt, kcx, W, col0, ncols, epi, wname, nsub=3, wbufs=3, psb=None):
    P = C.P
    wt = [C.sb(es, [128, kcx, 128], BF16, "wws") for _ in range(wbufs)]
    ps = psb if psb is not None else [C.ps(es, [128, 512], F32, "gps") for _ in range(2)]
    nps = 0
    for j in range((ncols + 127) // 128):
        w = wt[j % wbufs]
        c = col0 + j * 128
        cw = min(128, ncols - j * 128)
        P.dma(w[:, :, 0:cw], W[:, c:c + cw].rearrange("(k p) c -> p k c", p=128), writes=[w], eng="pool")
        for tt0 in range(0, nt, nsub):
            ntl = min(nsub, nt - tt0)
            pp = ps[nps % len(ps)]
            nps += 1
            for k in range(kcx):
                P.add("pe", lambda e, pp=pp, w=w, k=k, tt0=tt0, ntl=ntl, cw=cw: e.matmul(
                    pp[0:cw, 0:ntl * 128], lhsT=w[:, k, 0:cw], rhs=act[:, tt0:tt0 + ntl, k, :],
                    start=(k == 0), stop=(k == kcx - 1)), reads=[w, act], writes=[pp])
            epi(pp, j, tt0, ntl)


def gemm_ts(C, es, act, nt, kcx, W, col0, ncols, epi, cb=512, wbufs=2, psb=None):
    P = C.P
    wt = [C.sb(es, [128, kcx, cb], BF16, "wts") for _ in range(wbufs)]
    ps = psb if psb is not None else [C.ps(es, [128, 512], F32, "gps") for _ in range(2)]
    nps = 0
    for j in range(ncols // cb):
        w = wt[j % wbufs]
        c = col0 + j * cb
        P.dma(w[:, :, :], W[:, c:c + cb].rearrange("(k p) c -> p k c", p=128), writes=[w], eng="pool")
        for ti in range(nt):
            pp = ps[nps % len(ps)]
            nps += 1
            for k in range(kcx):
                P.add("pe", lambda e, pp=pp, w=w, k=k, ti=ti: e.matmul(
                    pp[:, 0:cb], lhsT=act[:, ti, k, :], rhs=w[:, k, :],
                    start=(k == 0), stop=(k == kcx - 1)), reads=[w, act], writes=[pp])
            epi(pp, ti, j * cb)


def rms_rstd(P, e_src_ps, ones_bf, sq, pss, rs, n, src_reads, eps=EPS):
    P.add("act", lambda e: e.activation(out=sq[:, 0:n], in_=e_src_ps, func=AF.Square), reads=src_reads, writes=[sq])
    P.add("pe", lambda e: e.matmul(pss[:, 0:n], lhsT=ones_bf, rhs=sq[:, 0:n], start=True, stop=True),
          reads=[sq], writes=[pss])
    P.add("dve", lambda e: e.tensor_scalar(out=rs[:, 0:n], in0=pss[:, 0:n], scalar1=eps, scalar2=None, op0=ALU.add),
          reads=[pss], writes=[rs])
    P.add("act", lambda e: e.activation(out=rs[:, 0:n], in_=rs[:, 0:n], func=AF.Sqrt), reads=[rs], writes=[rs])
    P.add("dve", lambda e: e.reciprocal(out=rs[:, 0:n], in_=rs[:, 0:n]), reads=[rs], writes=[rs])


def pass_swa_proj(C, hT, w_in, q_g, k_g, cosT, sinT, rotm, qT, Vd):
    P = C.P
    scale = 128 ** -0.5
    def do_block(blk):
        with ExitStack() as es:
            act = C.sb(es, [128, 9, KC, 128], BF16, "act")
            cs = C.sb(es, [128, 1152], F32, "cs")
            sn = C.sb(es, [128, 1152], F32, "sn")
            rm = C.sb(es, [128, 128], BF16, "rm")
            ones = C.sb(es, [128, 128], BF16, "ones")
            gq = C.sb(es, [128, 2], F32, "gq")
            sq = [C.sb(es, [128, 384], BF16, "sq") for _ in range(2)]
            rs = [C.sb(es, [128, 384], F32, "rs") for _ in range(2)]
            qn = [C.sb(es, [128, 384], BF16, "qn") for _ in range(2)]
            t1 = [C.sb(es, [128, 384], F32, "t1") for _ in range(2)]
            qo = [C.sb(es, [128, 384], BF16, "qo") for _ in range(2)]
            vo = [C.sb(es, [128, 512], BF16, "vo") for _ in range(2)]
            pss = [C.ps(es, [128, 512], F32, "pss") for _ in range(2)]
            prr = [C.ps(es, [128, 512], F32, "prr") for _ in range(2)]
            gps = [C.ps(es, [128, 512], F32, "gps") for _ in range(2)]
            load_act(C, act, hT, blk * 9, 9, "hT")
            P.dma(cs[:, :], cosT.ap()[:, blk * 1152:(blk + 1) * 1152], writes=[cs])
            P.dma(sn[:, :], sinT.ap()[:, blk * 1152:(blk + 1) * 1152], writes=[sn])
            P.dma(rm[:, :], rotm.ap(), writes=[rm])
            P.dma(gq[:, 0:1], q_g.rearrange("(p o) -> p o", o=1), writes=[gq])
            P.dma(gq[:, 1:2], k_g.rearrange("(p o) -> p o", o=1), writes=[gq])
            P.add("dve", lambda e: e.tensor_scalar(out=gq[:, 0:1], in0=gq[:, 0:1], scalar1=scale, scalar2=None, op0=ALU.mult),
                  reads=[gq], writes=[gq])
            P.add("pool", lambda e: e.memset(ones[:, :], 1.0 / 128), writes=[ones])
            cnt = [0]

            def epi(pp, j, tt0, ntl):
                n = ntl * 128
                c = cnt[0] % 2
                cnt[0] += 1
                gi = 0 if j < 32 else 1
                rms_rstd(P, pp[:, 0:n], ones[:, :], sq[c], pss[c], rs[c], n, [pp])
                P.add("dve", lambda e: e.scalar_tensor_tensor(out=qn[c][:, 0:n], in0=pp[:, 0:n], scalar=gq[:, gi:gi + 1],
                                                              in1=rs[c][:, 0:n], op0=ALU.mult, op1=ALU.mult),
                      reads=[pp, gq, rs[c]], writes=[qn[c]])
                P.add("pe", lambda e: e.matmul(prr[c][:, 0:n], lhsT=rm[:, :], rhs=qn[c][:, 0:n], start=True, stop=True),
                      reads=[rm, qn[c]], writes=[prr[c]])
                P.add("pool", lambda e: e.tensor_tensor(out=t1[c][:, 0:n], in0=qn[c][:, 0:n], in1=cs[:, tt0 * 128:tt0 * 128 + n],
                                                       op=ALU.mult), reads=[qn[c], cs], writes=[t1[c]])
                P.add("dve", lambda e: e.tensor_tensor(out=rs[c][:, 0:n], in0=prr[c][:, 0:n], in1=sn[:, tt0 * 128:tt0 * 128 + n],
                                                      op=ALU.mult), reads=[prr[c], sn, rs[c]], writes=[rs[c]])
                P.add("dve", lambda e: e.tensor_tensor(out=qo[c][:, 0:n], in0=t1[c][:, 0:n], in1=rs[c][:, 0:n], op=ALU.add),
                      reads=[t1[c], rs[c]], writes=[qo[c]])
                tg = (blk * 9 + tt0) * 128
                P.dma(qT.ap()[j, :, tg:tg + n], qo[c][:, 0:n], reads=[qo[c]], writes=[("qT", j)])

            gemm_ws(C, es, act, 9, KC, w_in, 0, 40 * 128, epi, "w", psb=gps)
            vc = [0]

            def epv(pp, ti, c0):
                c = vc[0] % 2
                vc[0] += 1
                P.add("act", lambda e: e.copy(out=vo[c][:, :], in_=pp[:, 0:512]), reads=[pp], writes=[vo[c]])
                tg = (blk * 9 + ti) * 128
                P.dma(Vd.ap()[tg:tg + 128, c0:c0 + 512], vo[c][:, :], reads=[vo[c]], writes=[("Vd", c0)])

            gemm_ts(C, es, act, 9, KC, w_in, 40 * 128, 1024, epv, psb=gps)
        P.barrier()
    for blk_ in range(2):
        do_block(blk_)


def attn_step(P, terms, sc, pt, mask, vap, ones, ov, dn, first, last, rd, n=512):
    for ti, (lt, rh) in enumerate(terms):
        P.add("pe", lambda e, lt=lt, rh=rh, ti=ti: e.matmul(sc[:, 0:n], lhsT=lt, rhs=rh, start=(ti == 0),
                                                            stop=(ti == len(terms) - 1)), reads=rd, writes=[sc])
    P.add("act", lambda e: e.activation(out=pt[:, 0:n], in_=sc[:, 0:n], func=AF.Exp), reads=[sc], writes=[pt])
    if mask is not None:
        P.add("pool", lambda e: e.tensor_tensor(out=pt[:, 0:n], in0=pt[:, 0:n], in1=mask, op=ALU.mult),
              reads=[pt], writes=[pt])
    P.add("pe", lambda e: e.matmul(ov[:, 0:n], lhsT=vap, rhs=pt[:, 0:n], start=first, stop=last), reads=rd + [pt], writes=[ov])
    P.add("pe", lambda e: e.matmul(dn[:, 0:n], lhsT=ones, rhs=pt[:, 0:n], start=first, stop=last), reads=[pt], writes=[dn])


def pass_swa_attn(C, qT, Vd, sinks, maskP, maskN, oT):
    P = C.P
    with ExitStack() as es:
        qs = [C.sb(es, [128, 4, NT], BF16, "qs") for _ in range(2)]
        ks = [C.sb(es, [128, NT], BF16, "ks") for _ in range(2)]
        vs = [C.sb(es, [128, NTT, 128], BF16, "vs") for _ in range(2)]
        mP = C.sb(es, [128, 128], BF16, "mP")
        mN = C.sb(es, [128, 128], BF16, "mN")
        ones = C.sb(es, [128, 128], BF16, "ones")
        esk = C.sb(es, [128, 32], F32, "esk")
        pts = [C.sb(es, [128, 512], BF16, "pt") for _ in range(3)]
        den = [C.sb(es, [128, 512], F32, "den") for _ in range(2)]
        ob = [C.sb(es, [128, 512], BF16, "ob") for _ in range(2)]
        scp = [C.ps(es, [128, 512], F32, "sc") for _ in range(3)]
        ovp = [C.ps(es, [128, 512], F32, "ov") for _ in range(2)]
        dnp = [C.ps(es, [128, 512], F32, "dn") for _ in range(2)]
        P.dma(mP[:, :], maskP.ap(), writes=[mP])
        P.dma(mN[:, :], maskN.ap(), writes=[mN])
        P.add("pool", lambda e: e.memset(ones[:, :], 1.0), writes=[ones])
        P.dma(esk[:, :], sinks.partition_broadcast(128), writes=[esk])
        P.add("act", lambda e: e.activation(out=esk[:, :], in_=esk[:, :], func=AF.Exp), reads=[esk], writes=[esk])
        nstep = 0
        nblk = 0
        for g in range(8):
            q = qs[g % 2]
            k = ks[g % 2]
            v = vs[g % 2]
            P.dma(q[:, :, :], qT.ap()[4 * g:4 * g + 4].rearrange("h d t -> d h t"), reads=[("qT", 4 * g + i) for i in range(4)],
                  writes=[q])
            P.dma(k[:, :], qT.ap()[32 + g], reads=[("qT", 32 + g)], writes=[k])
            P.dma(v[:, :, :], Vd.ap()[:, g * 128:(g + 1) * 128].rearrange("(i p) d -> p i d", p=128),
                  reads=[("Vd", 0), ("Vd", 512)], writes=[v])
            for j in range(NTT):
                if j < 16:
                    chunks = [(c, m) for c, m in ((j - 1, mP), (j, None), (j + 1, mN)) if 0 <= c < 16] + [(16, None), (17, None)]
                else:
                    chunks = [(16, None), (17, None)]
                ov = ovp[nblk % 2]
                dn = dnp[nblk % 2]
                dsb = den[nblk % 2]
                o = ob[nblk % 2]
                nblk += 1
                for ci, (c, m) in enumerate(chunks):
                    sc = scp[nstep % 3]
                    pt = pts[nstep % 3]
                    nstep += 1
                    mask = None if m is None else m[:, None, :].to_broadcast([128, 4, 128])
                    ptv = pt[:, :].rearrange("p (h t) -> p h t", h=4) if m is not None else None
                    terms = [(k[:, c * 128:(c + 1) * 128], q[:, :, j * 128:(j + 1) * 128])]
                    P.add("pe", lambda e, sc=sc, terms=terms: e.matmul(sc[:, :], lhsT=terms[0][0], rhs=terms[0][1],
                                                                       start=True, stop=True), reads=[k, q], writes=[sc])
                    P.add("act", lambda e, sc=sc, pt=pt: e.activation(out=pt[:, :], in_=sc[:, :], func=AF.Exp),
                          reads=[sc], writes=[pt])
                    if m is not None:
                        P.add("pool", lambda e, ptv=ptv, mask=mask: e.tensor_tensor(out=ptv, in0=ptv, in1=mask, op=ALU.mult),
                              reads=[pt, m], writes=[pt])
                    fst = ci == 0
                    lst = ci == len(chunks) - 1
                    P.add("pe", lambda e, ov=ov, v=v, c=c, pt=pt, fst=fst, lst=lst: e.matmul(
                        ov[:, :], lhsT=v[:, c, :], rhs=pt[:, :], start=fst, stop=lst), reads=[v, pt], writes=[ov])
                    P.add("pe", lambda e, dn=dn, pt=pt, fst=fst, lst=lst: e.matmul(
                        dn[:, :], lhsT=ones[:, :], rhs=pt[:, :], start=fst, stop=lst), reads=[ones, pt], writes=[dn])
                P.add("dve", lambda e, dsb=dsb, dn=dn, g=g: e.tensor_tensor(
                    out=dsb[:, :].rearrange("p (h t) -> p h t", h=4), in0=dn[:, :].rearrange("p (h t) -> p h t", h=4),
                    in1=esk[:, 4 * g:4 * g + 4, None].to_broadcast([128, 4, 128]), op=ALU.add), reads=[dn, esk], writes=[dsb])
                P.add("dve", lambda e, dsb=dsb: e.reciprocal(out=dsb[:, :], in_=dsb[:, :]), reads=[dsb], writes=[dsb])
                P.add("dve", lambda e, o=o, ov=ov, dsb=dsb: e.tensor_tensor(out=o[:, :], in0=ov[:, :], in1=dsb[:, :], op=ALU.mult),
                      reads=[ov, dsb], writes=[o])
                P.dma(oT.ap()[j, :, 4 * g:4 * g + 4, :], o[:, :].rearrange("p (h t) -> p h t", h=4), reads=[o],
                      writes=[("oT", j)])
    P.barrier()


def pass_outproj(C, aT, kcx, W, ncols, modv, l, i_gate, xres, nblk_tiles, glu=False, cb=512):
    P = C.P
    def do_block(t0):
        nt = min(nblk_tiles, NTT - t0)
        with ExitStack() as es:
            act = C.sb(es, [128, nt, kcx, 128], BF16, "act")
            G = [C.sb(es, [128, D], F32, "G") for _ in range(2)]
            xs = [C.sb(es, [128, 512], F32, "xs") for _ in range(3)]
            ts = [C.sb(es, [128, 512], F32, "ts") for _ in range(3)]
            load_act(C, act, aT, t0, nt, "aT")
            for s in range(2):
                P.dma(G[s][:, :], modv.ap()[l, s, i_gate * D:(i_gate + 1) * D].partition_broadcast(128),
                      reads=[("modv", l)], writes=[G[s]])
            cnt = [0]
            if not glu:
                def epi(pp, ti, c0):
                    c = cnt[0] % 3
                    cnt[0] += 1
                    ig = t0 + ti
                    s = 0 if ig < 16 else 1
                    P.dma(xs[c][:, 0:cb], xres.ap()[ig * 128:(ig + 1) * 128, c0:c0 + cb], reads=[("xres", ig)], writes=[xs[c]])
                    P.add("dve", lambda e: e.tensor_tensor(out=ts[c][:, 0:cb], in0=pp[:, 0:cb], in1=G[s][:, c0:c0 + cb], op=ALU.mult),
                          reads=[pp, G[s]], writes=[ts[c]])
                    P.add("dve", lambda e: e.tensor_tensor(out=xs[c][:, 0:cb], in0=xs[c][:, 0:cb], in1=ts[c][:, 0:cb], op=ALU.add),
                          reads=[xs[c], ts[c]], writes=[xs[c]])
                    P.dma(xres.ap()[ig * 128:(ig + 1) * 128, c0:c0 + cb], xs[c][:, 0:cb], reads=[xs[c]], writes=[("xres", ig)],
                          eng="act")
                gemm_ts(C, es, act, nt, kcx, W, 0, ncols, epi, cb=cb)
            else:
                wt = [C.sb(es, [128, kcx, 2, cb], BF16, "wglu") for _ in range(2)]
                sg = [C.sb(es, [128, 512], F32, "sg") for _ in range(2)]
                pa = [C.ps(es, [128, 512], F32, "pa") for _ in range(2)]
                pb = [C.ps(es, [128, 512], F32, "pb") for _ in range(2)]
                np_ = [0]

                def do_col(j):
                    w = wt[j % 2]
                    c0 = j * cb
                    for hh in range(2):
                        P.dma(w[:, :, hh, :], W[:, hh * D + c0:hh * D + c0 + cb].rearrange("(k p) c -> p k c", p=128), writes=[w], eng="pool")
                    def do_ti(ti):
                        a_ = pa[np_[0] % 2]
                        b_ = pb[np_[0] % 2]
                        s_ = sg[np_[0] % 2]
                        c = np_[0] % 3
                        np_[0] += 1
                        for (pp, hh) in ((a_, 0), (b_, 1)):
                            for k in range(kcx):
                                P.add("pe", lambda e, pp=pp, hh=hh, k=k, ti=ti: e.matmul(pp[:, 0:cb], lhsT=act[:, ti, k, :], rhs=w[:, k, hh, :],
                                                                                         start=(k == 0), stop=(k == kcx - 1)), reads=[w, act], writes=[pp])
                        ig = t0 + ti
                        s = 0 if ig < 16 else 1
                        P.add("act", lambda e: e.activation(out=s_[:, 0:cb], in_=b_[:, 0:cb], func=AF.Sigmoid), reads=[b_], writes=[s_])
                        P.add("dve", lambda e: e.tensor_tensor(out=s_[:, 0:cb], in0=s_[:, 0:cb], in1=a_[:, 0:cb], op=ALU.mult), reads=[s_, a_], writes=[s_])
                        P.dma(xs[c][:, 0:cb], xres.ap()[ig * 128:(ig + 1) * 128, c0:c0 + cb], reads=[("xres", ig)], writes=[xs[c]])
                        P.add("dve", lambda e: e.tensor_tensor(out=ts[c][:, 0:cb], in0=s_[:, 0:cb], in1=G[s][:, c0:c0 + cb], op=ALU.mult),
                              reads=[s_, G[s]], writes=[ts[c]])
                        P.add("dve", lambda e: e.tensor_tensor(out=xs[c][:, 0:cb], in0=xs[c][:, 0:cb], in1=ts[c][:, 0:cb], op=ALU.add),
                              reads=[xs[c], ts[c]], writes=[xs[c]])
                        P.dma(xres.ap()[ig * 128:(ig + 1) * 128, c0:c0 + cb], xs[c][:, 0:cb], reads=[xs[c]], writes=[("xres", ig)], eng="act")

                    for ti_ in range(nt):
                        do_ti(ti_)

                for j_ in range(ncols // cb):
                    do_col(j_)
        P.barrier()
    for t0_ in range(0, NTT, nblk_tiles):
        do_block(t0_)


def _bf(a):
    import ml_dtypes
    return np.asarray(a, dtype=np.float32).astype(ml_dtypes.bfloat16)


def host_consts():
    c = {}
    eye = np.eye(128, dtype=np.float32)
    c["ident"] = eye
    c["ident_bf"] = _bf(eye)
    kk = np.arange(128)[:, None]
    qq = np.arange(128)[None, :]
    c["maskP"] = _bf((kk >= qq).astype(np.float32))
    c["maskN"] = _bf((kk <= qq).astype(np.float32))

    def rope(rot_dim):
        axis_dim = rot_dim // 2
        half = axis_dim // 2
        inv = (np.float32(10000.0) ** (-np.arange(0, axis_dim, 2, dtype=np.float32) / np.float32(axis_dim))).astype(np.float32)
        t = np.arange(NL)
        pos = [(t // 64).astype(np.float32), (t % 64).astype(np.float32)]
        cs = np.ones((rot_dim, NT), np.float32)
        sn = np.zeros((rot_dim, NT), np.float32)
        rot = np.zeros((rot_dim, rot_dim), np.float32)
        for a in range(2):
            ang = (pos[a][None, :] * inv[:, None]).astype(np.float32)
            for hh in range(2):
                r0 = a * axis_dim + hh * half
                cs[r0:r0 + half, :NL] = np.cos(ang)
                sn[r0:r0 + half, :NL] = np.sin(ang)
            for i in range(half):
                d1 = a * axis_dim + i
                d2 = d1 + half
                rot[d2, d1] = -1.0
                rot[d1, d2] = 1.0
        return cs, sn, rot
    ufb = np.stack([(kk <= qq), (kk >= qq)]).astype(np.float32)
    c["ssd_U"] = ufb
    c["ssd_negm"] = _bf((ufb - 1.0) * 30000.0)
    r = np.arange(128)
    c["s5_rowmask"] = np.stack([(r % 32 < 16), (r % 32 >= 16)], 1).astype(np.float32)
    c["s5_cmask"] = ((r[:, None] % 32 < 16) == (r[None, :] < 64)).astype(np.float32)
    cs, sn, rot = rope(128)
    c["swa_cos"], c["swa_sin"], c["swa_rot"] = cs, sn, _bf(rot)
    cs, sn, rot = rope(64)
    c["mla_cos"], c["mla_sin"], c["mla_rot"] = cs, sn, _bf(rot)
    return c


def pass_moe(C, hT, w_group, b_group, w_expert, b_expert, w1, w3, w2, modv, l, xres, ident, groups=None, stages=(1, 2, 3), nexp=32, dbg_gt=None, dbg2=None):
    P = C.P
    if groups is None:
        groups = [(0, 4), (4, 4), (8, 4), (12, 4), (16, 2)]
    BIG = 1.0e9
    with ExitStack() as es:
        act = C.sb(es, [128, 4, KC, 128], BF16, "act")
        gT = C.sb(es, [128, 32, 2, 512], BF16, "gT")
        w2u = [C.sb(es, [128, 8, 2, 512], BF16, "w2u") for _ in range(2)]
        wch = [C.sb(es, [128, KC, 128], BF16, "wch") for _ in range(5)]
        wr = C.sb(es, [128, KC, 36], BF16, "wr")
        bia = C.sb(es, [128, 36], F32, "bia")
        idt = C.sb(es, [128, 128], F32, "idt")
        Ssb = [C.sb(es, [128, 512], F32, "Ssb") for _ in range(2)]
        Tsb = [C.sb(es, [128, 512], F32, "Tsb") for _ in range(2)]
        GBs = [C.sb(es, [128, 512], F32, "GBs") for _ in range(2)]
        GT = C.sb(es, [32, 512], F32, "GT")
        L = [C.sb(es, [128, 36], F32, "L") for _ in range(2)]
        L2 = [C.sb(es, [128, 32], F32, "L2") for _ in range(2)]
        sm = [C.sb(es, [128, 16], F32, "sm") for _ in range(2)]
        oh = [C.sb(es, [128, 4 + 32 + 32], F32, "oh") for _ in range(2)]
        Gm = [C.sb(es, [128, 32], F32, "Gm") for _ in range(2)]
        xs = [C.sb(es, [128, 512], F32, "xs") for _ in range(2)]
        ts = [C.sb(es, [128, 512], F32, "ts") for _ in range(2)]
        G5 = [C.sb(es, [128, 512], F32, "G5") for _ in range(2)]
        ppr = C.ps(es, [128, 512], F32, "ppr")
        ptr = C.ps(es, [128, 512], F32, "ptr")
        H = [C.ps(es, [128, 512], F32, "H") for _ in range(4)]
        GBp = [C.ps(es, [128, 512], F32, "GBp") for _ in range(2)]

        P.dma(idt[:, :], ident.ap(), writes=[idt])
        P.dma(wr[:, :, 0:4], w_group.rearrange("(k p) c -> p k c", p=128), writes=[wr], eng="pool")
        P.dma(wr[:, :, 4:36], w_expert.rearrange("(k p) c -> p k c", p=128), writes=[wr], eng="pool")
        P.dma(bia[:, 0:4], b_group.partition_broadcast(128), writes=[bia])
        P.dma(bia[:, 4:36], b_expert.partition_broadcast(128), writes=[bia])
        nw = 0
        nu = 0
        nx = 0
        ng = 0
        nr = 0
        def do_group(t0, nt):
            nonlocal nw, nu, nx, ng, nr
            n = nt * 128
            s_idx = 0 if t0 < 16 else 1
            load_act(C, act, hT, t0, nt, "hT")
            for ti in (range(nt) if 1 in stages else []):
                c = nr % 2
                nr += 1
                Lc, L2c, smc, ohc, Gc = L[c], L2[c], sm[c], oh[c], Gm[c]
                for k in range(KC):
                    P.add("pe", lambda e, ti=ti, k=k: e.matmul(ppr[:, 0:36], lhsT=act[:, ti, k, :], rhs=wr[:, k, :],
                                                               start=(k == 0), stop=(k == KC - 1)), reads=[act, wr], writes=[ppr])
                P.add("dve", lambda e, Lc=Lc: e.tensor_tensor(out=Lc[:, :], in0=ppr[:, 0:36], in1=bia[:, :], op=ALU.add),
                      reads=[ppr, bia], writes=[Lc])
                P.add("dve", lambda e, Lc=Lc, smc=smc: e.tensor_reduce(out=smc[:, 0:1], in_=Lc[:, 0:4], axis=AX.X, op=ALU.max),
                      reads=[Lc], writes=[smc])
                P.add("dve", lambda e, Lc=Lc, smc=smc, ohc=ohc: e.tensor_scalar(out=ohc[:, 0:4], in0=Lc[:, 0:4], scalar1=smc[:, 0:1],
                                                                               scalar2=None, op0=ALU.is_equal),
                      reads=[Lc, smc], writes=[ohc])
                P.add("dve", lambda e, smc=smc: e.tensor_scalar(out=smc[:, 1:2], in0=smc[:, 0:1], scalar1=-1.0, scalar2=None,
                                                                op0=ALU.mult), reads=[smc], writes=[smc])
                P.add("act", lambda e, Lc=Lc, smc=smc, ohc=ohc: e.activation(out=ohc[:, 36:40], in_=Lc[:, 0:4], func=AF.Exp,
                                                                            bias=smc[:, 1:2], accum_out=smc[:, 2:3]),
                      reads=[Lc, smc], writes=[ohc, smc])
                P.add("dve", lambda e, smc=smc: e.reciprocal(out=smc[:, 3:4], in_=smc[:, 2:3]), reads=[smc], writes=[smc])
                P.add("dve", lambda e, ohc=ohc: e.tensor_scalar(out=ohc[:, 40:44], in0=ohc[:, 0:4], scalar1=-1.0, scalar2=BIG,
                                                                op0=ALU.add, op1=ALU.mult), reads=[ohc], writes=[ohc])
                P.add("dve", lambda e, Lc=Lc, L2c=L2c, ohc=ohc: e.tensor_tensor(
                    out=L2c[:, :].rearrange("p (g j) -> p g j", g=4), in0=Lc[:, 4:36].rearrange("p (g j) -> p g j", g=4),
                    in1=ohc[:, 40:44, None].to_broadcast([128, 4, 8]), op=ALU.add), reads=[Lc, ohc], writes=[L2c])
                P.add("dve", lambda e, L2c=L2c, smc=smc: e.tensor_reduce(out=smc[:, 4:5], in_=L2c[:, :], axis=AX.X, op=ALU.max),
                      reads=[L2c], writes=[smc])
                P.add("dve", lambda e, L2c=L2c, smc=smc, ohc=ohc: e.tensor_scalar(out=ohc[:, 4:36], in0=L2c[:, :], scalar1=smc[:, 4:5],
                                                                                 scalar2=None, op0=ALU.is_equal),
                      reads=[L2c, smc], writes=[ohc])
                P.add("dve", lambda e, L2c=L2c, ohc=ohc: e.scalar_tensor_tensor(out=L2c[:, :], in0=ohc[:, 4:36], scalar=-BIG,
                                                                               in1=L2c[:, :], op0=ALU.mult, op1=ALU.add),
                      reads=[L2c, ohc], writes=[L2c])
                P.add("dve", lambda e, L2c=L2c, smc=smc: e.tensor_reduce(out=smc[:, 5:6], in_=L2c[:, :], axis=AX.X, op=ALU.max),
                      reads=[L2c], writes=[smc])
                P.add("dve", lambda e, L2c=L2c, smc=smc, Gc=Gc: e.tensor_scalar(out=Gc[:, :], in0=L2c[:, :], scalar1=smc[:, 5:6],
                                                                               scalar2=None, op0=ALU.is_equal),
                      reads=[L2c, smc], writes=[Gc])
                P.add("dve", lambda e, smc=smc: e.tensor_tensor(out=smc[:, 6:7], in0=smc[:, 5:6], in1=smc[:, 4:5], op=ALU.subtract),
                      reads=[smc], writes=[smc])
                P.add("act", lambda e, smc=smc: e.activation(out=smc[:, 7:8], in_=smc[:, 6:7], func=AF.Exp), reads=[smc], writes=[smc])
                P.add("dve", lambda e, smc=smc: e.tensor_scalar(out=smc[:, 8:9], in0=smc[:, 7:8], scalar1=1.0, scalar2=None, op0=ALU.add),
                      reads=[smc], writes=[smc])
                P.add("dve", lambda e, smc=smc: e.reciprocal(out=smc[:, 9:10], in_=smc[:, 8:9]), reads=[smc], writes=[smc])
                P.add("dve", lambda e, smc=smc: e.tensor_tensor(out=smc[:, 10:11], in0=smc[:, 9:10], in1=smc[:, 3:4], op=ALU.mult),
                      reads=[smc], writes=[smc])
                P.add("dve", lambda e, smc=smc: e.tensor_tensor(out=smc[:, 11:12], in0=smc[:, 10:11], in1=smc[:, 7:8], op=ALU.mult),
                      reads=[smc], writes=[smc])
                P.add("dve", lambda e, Gc=Gc, smc=smc: e.tensor_scalar(out=Gc[:, :], in0=Gc[:, :], scalar1=smc[:, 11:12], scalar2=None,
                                                                      op0=ALU.mult), reads=[Gc, smc], writes=[Gc])
                P.add("dve", lambda e, Gc=Gc, smc=smc, ohc=ohc: e.scalar_tensor_tensor(out=Gc[:, :], in0=ohc[:, 4:36], scalar=smc[:, 10:11],
                                                                                      in1=Gc[:, :], op0=ALU.mult, op1=ALU.add),
                      reads=[Gc, smc, ohc], writes=[Gc])
                P.add("pe", lambda e, Gc=Gc: e.transpose(out=ptr[0:32, 0:128], in_=Gc[:, :], identity=idt[:, :]),
                      reads=[Gc, idt], writes=[ptr])
                P.add("act", lambda e, ti=ti: e.copy(out=GT[:, ti * 128:(ti + 1) * 128], in_=ptr[0:32, 0:128]),
                      reads=[ptr], writes=[GT])
            if dbg_gt is not None:
                P.dma(dbg_gt.ap()[:, t0 * 128:t0 * 128 + n], GT[:, 0:n], reads=[GT], writes=[("dbg_gt", t0)], is_out=True)
            for ex in (range(nexp) if 2 in stages else []):
                for (wsrc, c, hi) in ((w1, 0, 0), (w1, 1, 1), (w3, 0, 2), (w3, 1, 3)):
                    w = wch[nw % 5]
                    nw += 1
                    P.dma(w[:, :, :], wsrc[ex, :, c * 128:(c + 1) * 128].rearrange("(k p) c -> p k c", p=128), writes=[w], eng="pool")
                    for k in range(KC):
                        P.add("pe", lambda e, w=w, k=k, hi=hi: e.matmul(H[hi][:, 0:n], lhsT=w[:, k, :], rhs=act[:, 0:nt, k, :],
                                                                        start=(k == 0), stop=(k == KC - 1)),
                              reads=[w, act], writes=[H[hi]])
                gb = GBp[ng % 2]
                gbs = GBs[ng % 2]
                ng += 1
                dbg = int(os.environ.get("MOE_DBG", "9"))
                if dbg < 1:
                    continue
                P.add("pe", lambda e, gb=gb, ex=ex: e.matmul(gb[:, 0:n], lhsT=idt[0:32, ex:ex + 1].to_broadcast([32, 128]),
                                                             rhs=GT[:, 0:n], start=True, stop=True), reads=[idt, GT], writes=[gb])
                P.add("act", lambda e, gb=gb, gbs=gbs: e.copy(out=gbs[:, 0:n], in_=gb[:, 0:n]), reads=[gb], writes=[gbs])
                for c in (range(2) if dbg >= 2 else []):
                    S = Ssb[c]
                    T = Tsb[c]
                    P.add("act", lambda e, S=S, c=c: e.activation(out=S[:, 0:n], in_=H[c][:, 0:n], func=AF.Silu),
                          reads=[H[c]], writes=[S])
                    if dbg < 3:
                        continue
                    P.add("dve", lambda e, S=S, T=T, c=c: e.tensor_tensor(out=T[:, 0:n], in0=S[:, 0:n], in1=H[2 + c][:, 0:n], op=ALU.mult),
                          reads=[S, H[2 + c]], writes=[T])
                    if dbg < 4:
                        continue
                    P.add("dve", lambda e, T=T, gbs=gbs, ex=ex, c=c: e.tensor_tensor(out=gT[:, ex, c, 0:n], in0=T[:, 0:n], in1=gbs[:, 0:n],
                                                                                      op=ALU.mult), reads=[T, gbs], writes=[gT])
            if dbg2 is not None:
                for cc_ in range(2):
                    P.add("dve", lambda e, cc_=cc_: e.tensor_copy(out=Ssb[cc_][:, :], in_=gT[:, 0, cc_, :]), reads=[gT, Tsb[cc_]], writes=[Ssb[cc_]])
                for ii, tt_ in enumerate((Ssb[0], Tsb[0], GBs[0], Ssb[1], Tsb[1], GBs[1])):
                    P.dma(dbg2.ap()[ii], tt_[:, :], reads=[tt_], writes=[("dbg2", ii)], is_out=True)
            for cb in (range(8) if 3 in stages else []):
                g5 = G5[cb % 2]
                P.dma(g5[:, :], modv.ap()[l, s_idx, 5 * D + cb * 512:5 * D + (cb + 1) * 512].partition_broadcast(128),
                      reads=[("modv", l)], writes=[g5])
                for q in range(4):
                    u = w2u[nu % 2]
                    nu += 1
                    P.dma(u[:, :, :, :], w2[8 * q:8 * q + 8, :, cb * 512:(cb + 1) * 512].rearrange("e (c p) n -> p e c n", p=128),
                          writes=[u], eng="pool")
                    for ti in range(nt):
                        for el in range(8):
                            for c in range(2):
                                first = (q == 0 and el == 0 and c == 0)
                                last = (q == 3 and el == 7 and c == 1)
                                P.add("pe", lambda e, ti=ti, el=el, c=c, q=q, u=u, first=first, last=last: e.matmul(
                                    H[ti][:, :], lhsT=gT[:, 8 * q + el, c, ti * 128:(ti + 1) * 128], rhs=u[:, el, c, :],
                                    start=first, stop=last), reads=[gT, u], writes=[H[ti]])
                for ti in range(nt):
                    ig = t0 + ti
                    xc = xs[nx % 2]
                    tc = ts[nx % 2]
                    nx += 1
                    P.dma(xc[:, :], xres.ap()[ig * 128:(ig + 1) * 128, cb * 512:(cb + 1) * 512], reads=[("xres", ig)], writes=[xc])
                    P.add("dve", lambda e, tc=tc, ti=ti, g5=g5: e.tensor_tensor(out=tc[:, :], in0=H[ti][:, :], in1=g5[:, :], op=ALU.mult),
                          reads=[H[ti], g5], writes=[tc])
                    P.add("dve", lambda e, xc=xc, tc=tc: e.tensor_tensor(out=xc[:, :], in0=xc[:, :], in1=tc[:, :], op=ALU.add),
                          reads=[xc, tc], writes=[xc])
                    P.dma(xres.ap()[ig * 128:(ig + 1) * 128, cb * 512:(cb + 1) * 512], xc[:, :], reads=[xc], writes=[("xres", ig)],
                          eng="act")
        for (t0_, nt_) in groups:
            do_group(t0_, nt_)
    P.barrier()


def pass_mla_a(C, hT, w_in, q_a_g, kv_a_g, cqT, ckvT, krT):
    P = C.P
    def do_block(blk):
        with ExitStack() as es:
            act = C.sb(es, [128, 9, KC, 128], BF16, "act")
            raw = C.sb(es, [128, 12, 1152], F32, "raw")
            krs = [C.sb(es, [64, 384], F32, "krs") for _ in range(2)]
            ga = C.sb(es, [128, 12], F32, "ga")
            ones = C.sb(es, [128, 128], BF16, "ones")
            sq = [C.sb(es, [128, 384], BF16, "sq") for _ in range(3)]
            rs = [C.sb(es, [128, 384], F32, "rs") for _ in range(2)]
            stq = [C.sb(es, [128, 3, 8, 128], BF16, "stq") for _ in range(2)]
            stk = [C.sb(es, [128, 3, 4, 128], BF16, "stk") for _ in range(2)]
            pss = [C.ps(es, [128, 512], F32, "pss") for _ in range(2)]
            gps = [C.ps(es, [128, 512], F32, "gps") for _ in range(3)]
            load_act(C, act, hT, blk * 9, 9, "hT")
            P.dma(ga[:, 0:8], q_a_g.rearrange("(c p) -> p c", p=128), writes=[ga], slow=True)
            P.dma(ga[:, 8:12], kv_a_g.rearrange("(c p) -> p c", p=128), writes=[ga], slow=True)
            P.add("pool", lambda e: e.memset(ones[:, :], 1.0), writes=[ones])
            nk = [0]

            def epi(pp, j, tt0, ntl):
                n = ntl * 128
                if j < 12:
                    P.add("act", lambda e: e.copy(out=raw[:, j, tt0 * 128:tt0 * 128 + n], in_=pp[:, 0:n]), reads=[pp], writes=[raw])
                else:
                    kr = krs[nk[0] % 2]
                    nk[0] += 1
                    P.add("act", lambda e: e.copy(out=kr[:, 0:n], in_=pp[0:64, 0:n]), reads=[pp], writes=[kr])
                    tg = (blk * 9 + tt0) * 128
                    P.dma(krT.ap()[:, tg:tg + n], kr[:, 0:n], reads=[kr], writes=[("krT", 0)])

            gemm_ws(C, es, act, 9, KC, w_in, 0, 1600, epi, "w", psb=gps)
            nsq = 0
            def do_sub(sb_):
                nonlocal nsq
                c0 = sb_ * 384
                for (j0, nj, stg, dst, key) in ((0, 8, stq[sb_ % 2], cqT, "cqT"), (8, 4, stk[sb_ % 2], ckvT, "ckvT")):
                    pp = pss[(2 * sb_ + (j0 > 0)) % 2]
                    r = rs[(2 * sb_ + (j0 > 0)) % 2]
                    for jj in range(nj):
                        s_ = sq[nsq % 3]
                        nsq += 1
                        P.add("act", lambda e, s_=s_, jj=jj, j0=j0: e.activation(out=s_[:, :], in_=raw[:, j0 + jj, c0:c0 + 384], func=AF.Square),
                              reads=[raw], writes=[s_])
                        P.add("pe", lambda e, s_=s_, jj=jj, nj=nj, pp=pp: e.matmul(pp[:, 0:384], lhsT=ones[:, :], rhs=s_[:, :],
                                                                                 start=(jj == 0), stop=(jj == nj - 1)),
                              reads=[ones, s_], writes=[pp])
                    P.add("dve", lambda e, pp=pp, r=r, nj=nj: e.tensor_scalar(out=r[:, :], in0=pp[:, 0:384], scalar1=1.0 / (nj * 128),
                                                                              scalar2=EPS, op0=ALU.mult, op1=ALU.add), reads=[pp], writes=[r])
                    P.add("act", lambda e, r=r: e.activation(out=r[:, :], in_=r[:, :], func=AF.Sqrt), reads=[r], writes=[r])
                    P.add("dve", lambda e, r=r: e.reciprocal(out=r[:, :], in_=r[:, :]), reads=[r], writes=[r])
                    for jj in range(nj):
                        eng = "dve"
                        P.add(eng, lambda e, jj=jj, j0=j0, stg=stg, r=r: e.scalar_tensor_tensor(
                            out=stg[:, :, jj, :], in0=raw[:, j0 + jj, c0:c0 + 384].rearrange("p (i t) -> p i t", i=3),
                            scalar=ga[:, j0 + jj:j0 + jj + 1], in1=r[:, :].rearrange("p (i t) -> p i t", i=3),
                            op0=ALU.mult, op1=ALU.mult), reads=[raw, ga, r], writes=[stg])
                    ti0 = blk * 9 + sb_ * 3
                    P.dma(dst.ap()[ti0:ti0 + 3].rearrange("i p k t -> p i k t"), stg[:, :, :, :], reads=[stg], writes=[(key, ti0)])
            for sbi_ in range(3):
                do_sub(sbi_)
        P.barrier()
    for blk_ in range(2):
        do_block(blk_)


def pass_mla_b(C, cqT, ckvT, krT, w_uq, w_ukv, q_g, k_g, cos64, sin64, rot64, QN, QR, KN, KR, Vd):
    P = C.P
    scale = 192 ** -0.5
    with ExitStack() as es:
        actq = C.sb(es, [128, NTT, 8, 128], BF16, "actq")
        actk = C.sb(es, [128, NTT, 4, 128], BF16, "actk")
        kr = C.sb(es, [64, NT], F32, "kr")
        krsq = C.sb(es, [64, NT], BF16, "krsq")
        cs = C.sb(es, [64, NT], F32, "cs")
        sn = C.sb(es, [64, NT], F32, "sn")
        rm = C.sb(es, [64, 64], BF16, "rm")
        ones = C.sb(es, [128, 128], BF16, "ones")
        gq = C.sb(es, [128, 4], F32, "gq")
        wq = [C.sb(es, [128, 8, 192], BF16, "wq") for _ in range(2)]
        wk = [C.sb(es, [128, 4, 256], BF16, "wk") for _ in range(2)]
        sqn = [C.sb(es, [128, 384], BF16, "sqn") for _ in range(2)]
        sqr = [C.sb(es, [64, 384], BF16, "sqr") for _ in range(2)]
        rs = [C.sb(es, [128, 384], F32, "rs") for _ in range(2)]
        on_ = [C.sb(es, [128, 384], BF16, "on") for _ in range(2)]
        rn = [C.sb(es, [64, 384], BF16, "rn") for _ in range(2)]
        t1 = [C.sb(es, [64, 384], F32, "t1") for _ in range(2)]
        t2 = [C.sb(es, [64, 384], F32, "t2") for _ in range(2)]
        orr = [C.sb(es, [64, 384], BF16, "orr") for _ in range(2)]
        vo = [C.sb(es, [128, 3, 128], BF16, "vo") for _ in range(2)]
        pqn = C.ps(es, [128, 512], F32, "pqn")
        pqr = C.ps(es, [128, 512], F32, "pqr")
        pkn = C.ps(es, [128, 512], F32, "pkn")
        pss = [C.ps(es, [128, 512], F32, "pss") for _ in range(2)]
        prt = [C.ps(es, [128, 512], F32, "prt") for _ in range(2)]
        pv = C.ps(es, [128, 512], F32, "pv")
        load_act(C, actq, cqT, 0, NTT, "cqT")
        load_act(C, actk, ckvT, 0, NTT, "ckvT")
        P.dma(kr[:, :], krT.ap(), reads=[("krT", 0)], writes=[kr])
        P.dma(cs[:, :], cos64.ap(), writes=[cs])
        P.dma(sn[:, :], sin64.ap(), writes=[sn])
        P.dma(rm[:, :], rot64.ap(), writes=[rm])
        P.dma(gq[:, 0:1], q_g[0:128].rearrange("(p o) -> p o", o=1), writes=[gq])
        P.dma(gq[0:64, 1:2], q_g[128:192].rearrange("(p o) -> p o", o=1), writes=[gq])
        P.dma(gq[:, 2:3], k_g[0:128].rearrange("(p o) -> p o", o=1), writes=[gq])
        P.dma(gq[0:64, 3:4], k_g[128:192].rearrange("(p o) -> p o", o=1), writes=[gq])
        P.add("dve", lambda e: e.tensor_scalar(out=gq[:, 0:1], in0=gq[:, 0:1], scalar1=scale, scalar2=None, op0=ALU.mult),
              reads=[gq], writes=[gq])
        P.add("dve", lambda e: e.tensor_scalar(out=gq[0:64, 1:2], in0=gq[0:64, 1:2], scalar1=scale, scalar2=None, op0=ALU.mult),
              reads=[gq], writes=[gq])
        P.add("pool", lambda e: e.memset(ones[:, :], 1.0 / 192), writes=[ones])
        P.add("act", lambda e: e.activation(out=krsq[:, :], in_=kr[:, :], func=AF.Square), reads=[kr], writes=[krsq])
        cnt = 0

        def norm_rope(pn, pr_src, pr_reads, gi, dstN, dstR, h, tok0, n, c, extra_sq=None):
            ps_ = pss[c]
            r = rs[c]
            P.add("act", lambda e: e.activation(out=sqn[c][:, 0:n], in_=pn[:, 0:n], func=AF.Square), reads=[pn], writes=[sqn[c]])
            P.add("pe", lambda e: e.matmul(ps_[:, 0:n], lhsT=ones[:, :], rhs=sqn[c][:, 0:n], start=True, stop=False),
                  reads=[ones, sqn[c]], writes=[ps_])
            if extra_sq is None:
                P.add("act", lambda e: e.activation(out=sqr[c][:, 0:n], in_=pr_src, func=AF.Square), reads=pr_reads, writes=[sqr[c]])
                sq2 = sqr[c][:, 0:n]
                rd2 = [sqr[c]]
            else:
                sq2 = extra_sq
                rd2 = [krsq]
            P.add("pe", lambda e: e.matmul(ps_[:, 0:n], lhsT=ones[0:64, :], rhs=sq2, start=False, stop=True),
                  reads=[ones] + rd2, writes=[ps_])
            P.add("dve", lambda e: e.tensor_scalar(out=r[:, 0:n], in0=ps_[:, 0:n], scalar1=EPS, scalar2=None, op0=ALU.add),
                  reads=[ps_], writes=[r])
            P.add("act", lambda e: e.activation(out=r[:, 0:n], in_=r[:, 0:n], func=AF.Sqrt), reads=[r], writes=[r])
            P.add("dve", lambda e: e.reciprocal(out=r[:, 0:n], in_=r[:, 0:n]), reads=[r], writes=[r])
            P.add("dve", lambda e: e.scalar_tensor_tensor(out=on_[c][:, 0:n], in0=pn[:, 0:n], scalar=gq[:, gi:gi + 1], in1=r[:, 0:n],
                                                          op0=ALU.mult, op1=ALU.mult), reads=[pn, gq, r], writes=[on_[c]])
            P.dma(dstN.ap()[h, :, tok0:tok0 + n], on_[c][:, 0:n], reads=[on_[c]], writes=[("N", gi, h)])
            P.add("dve", lambda e: e.scalar_tensor_tensor(out=rn[c][:, 0:n], in0=pr_src, scalar=gq[0:64, gi + 1:gi + 2], in1=r[0:64, 0:n],
                                                          op0=ALU.mult, op1=ALU.mult), reads=pr_reads + [gq, r], writes=[rn[c]])
            pt_ = prt[c]
            P.add("pe", lambda e: e.matmul(pt_[0:64, 0:n], lhsT=rm[:, :], rhs=rn[c][:, 0:n], start=True, stop=True),
                  reads=[rm, rn[c]], writes=[pt_])
            P.add("pool", lambda e: e.tensor_tensor(out=t1[c][:, 0:n], in0=rn[c][:, 0:n], in1=cs[:, tok0:tok0 + n], op=ALU.mult),
                  reads=[rn[c], cs], writes=[t1[c]])
            P.add("dve", lambda e: e.tensor_tensor(out=t2[c][:, 0:n], in0=pt_[0:64, 0:n], in1=sn[:, tok0:tok0 + n], op=ALU.mult),
                  reads=[pt_, sn], writes=[t2[c]])
            P.add("pool", lambda e: e.tensor_tensor(out=orr[c][:, 0:n], in0=t1[c][:, 0:n], in1=t2[c][:, 0:n], op=ALU.add),
                  reads=[t1[c], t2[c]], writes=[orr[c]])
            P.dma(dstR.ap()[h, :, tok0:tok0 + n], orr[c][:, 0:n], reads=[orr[c]], writes=[("R", gi, h)])

        nv = 0
        for h in range(32):
            wqh = wq[h % 2]
            wkh = wk[h % 2]
            P.dma(wqh[:, :, :], w_uq[:, h * 192:(h + 1) * 192].rearrange("(k p) c -> p k c", p=128), writes=[wqh], eng="pool")
            P.dma(wkh[:, :, :], w_ukv[:, h * 256:(h + 1) * 256].rearrange("(k p) c -> p k c", p=128), writes=[wkh], eng="pool")
            for sb_ in range(6):
                ti0 = sb_ * 3
                tok0 = ti0 * 128
                n = 384
                for k in range(8):
                    P.add("pe", lambda e, k=k, wqh=wqh, ti0=ti0: e.matmul(pqn[:, 0:n], lhsT=wqh[:, k, 0:128], rhs=actq[:, ti0:ti0 + 3, k, :],
                                                                          start=(k == 0), stop=(k == 7)), reads=[wqh, actq], writes=[pqn])
                for k in range(8):
                    P.add("pe", lambda e, k=k, wqh=wqh, ti0=ti0: e.matmul(pqr[0:64, 0:n], lhsT=wqh[:, k, 128:192], rhs=actq[:, ti0:ti0 + 3, k, :],
                                                                          start=(k == 0), stop=(k == 7)), reads=[wqh, actq], writes=[pqr])
                for k in range(4):
                    P.add("pe", lambda e, k=k, wkh=wkh, ti0=ti0: e.matmul(pkn[:, 0:n], lhsT=wkh[:, k, 0:128], rhs=actk[:, ti0:ti0 + 3, k, :],
                                                                          start=(k == 0), stop=(k == 3)), reads=[wkh, actk], writes=[pkn])
                norm_rope(pqn, pqr[0:64, 0:n], [pqr], 0, QN, QR, h, tok0, n, 0)
                norm_rope(pkn, kr[:, tok0:tok0 + n], [kr], 2, KN, KR, h, tok0, n, 1, extra_sq=krsq[:, tok0:tok0 + n])
                v_ = vo[nv % 2]
                nv += 1
                for i in range(3):
                    for k in range(4):
                        P.add("pe", lambda e, k=k, i=i, wkh=wkh, ti0=ti0: e.matmul(pv[:, i * 128:(i + 1) * 128], lhsT=actk[:, ti0 + i, k, :],
                                                                                   rhs=wkh[:, k, 128:256], start=(k == 0), stop=(k == 3)),
                              reads=[wkh, actk], writes=[pv])
                P.add("act", lambda e, v_=v_: e.copy(out=v_[:, :, :], in_=pv[:, 0:384].rearrange("p (i d) -> p i d", i=3)),
                      reads=[pv], writes=[v_])
                P.dma(Vd.ap()[tok0:tok0 + 384, h * 128:(h + 1) * 128].rearrange("(i p) d -> p i d", p=128), v_[:, :, :],
                      reads=[v_], writes=[("Vd", h)])
    P.barrier()


def pass_mla_attn(C, QN, QR, KN, KR, Vd, oT, ctx_out=True):
    P = C.P
    with ExitStack() as es:
        qn = [C.sb(es, [128, NT], BF16, "qn") for _ in range(2)]
        qr = [C.sb(es, [64, NT], BF16, "qr") for _ in range(2)]
        kn = [C.sb(es, [128, NT], BF16, "kn") for _ in range(2)]
        kr = [C.sb(es, [64, NT], BF16, "kr") for _ in range(2)]
        vs = [C.sb(es, [128, NTT, 128], BF16, "vs") for _ in range(2)]
        ones = C.sb(es, [128, 128], BF16, "ones")
        pts = [C.sb(es, [128, 512], BF16, "pt") for _ in range(3)]
        den = [C.sb(es, [128, 512], F32, "den") for _ in range(2)]
        ob = [C.sb(es, [128, 512], BF16, "ob") for _ in range(2)]
        scp = [C.ps(es, [128, 512], F32, "sc") for _ in range(3)]
        ovp = [C.ps(es, [128, 512], F32, "ov") for _ in range(2)]
        dnp = [C.ps(es, [128, 512], F32, "dn") for _ in range(2)]
        P.add("pool", lambda e: e.memset(ones[:, :], 1.0), writes=[ones])
        nstep = 0
        nblk = 0
        qgroups = [(0, 4, list(range(18))), (4, 4, list(range(18))), (8, 4, list(range(18))), (12, 4, list(range(18)))]
        if ctx_out:
            qgroups.append((16, 2, [16, 17]))
        for h in range(32):
            a, b, c_, d_, v = qn[h % 2], qr[h % 2], kn[h % 2], kr[h % 2], vs[h % 2]
            P.dma(a[:, :], QN.ap()[h], reads=[("N", 0, h)], writes=[a])
            P.dma(b[:, :], QR.ap()[h], reads=[("R", 0, h)], writes=[b])
            P.dma(c_[:, :], KN.ap()[h], reads=[("N", 2, h)], writes=[c_])
            P.dma(d_[:, :], KR.ap()[h], reads=[("R", 2, h)], writes=[d_])
            P.dma(v[:, :, :], Vd.ap()[:, h * 128:(h + 1) * 128].rearrange("(i p) d -> p i d", p=128), reads=[("Vd", h)], writes=[v])
            def do_qg(t0, nt, chunks, h=h, a=a, b=b, c_=c_, d_=d_, v=v):
                nonlocal nstep, nblk
                n = nt * 128
                q0 = t0 * 128
                ov = ovp[nblk % 2]
                dn = dnp[nblk % 2]
                dsb = den[nblk % 2]
                o = ob[nblk % 2]
                nblk += 1
                for ci, c in enumerate(chunks):
                    sc = scp[nstep % 3]
                    pt = pts[nstep % 3]
                    nstep += 1
                    fst = ci == 0
                    lst = ci == len(chunks) - 1
                    P.add("pe", lambda e, sc=sc, c=c, c_=c_, a=a: e.matmul(sc[:, 0:n], lhsT=c_[:, c * 128:(c + 1) * 128], rhs=a[:, q0:q0 + n],
                                                                         start=True, stop=False), reads=[c_, a], writes=[sc])
                    P.add("pe", lambda e, sc=sc, c=c, d_=d_, b=b: e.matmul(sc[:, 0:n], lhsT=d_[:, c * 128:(c + 1) * 128], rhs=b[:, q0:q0 + n],
                                                                         start=False, stop=True), reads=[d_, b], writes=[sc])
                    P.add("act", lambda e, sc=sc, pt=pt: e.activation(out=pt[:, 0:n], in_=sc[:, 0:n], func=AF.Exp), reads=[sc], writes=[pt])
                    P.add("pe", lambda e, ov=ov, v=v, c=c, pt=pt, fst=fst, lst=lst: e.matmul(ov[:, 0:n], lhsT=v[:, c, :], rhs=pt[:, 0:n],
                                                                                           start=fst, stop=lst), reads=[v, pt], writes=[ov])
                    P.add("pe", lambda e, dn=dn, pt=pt, fst=fst, lst=lst: e.matmul(dn[:, 0:n], lhsT=ones[:, :], rhs=pt[:, 0:n],
                                                                                 start=fst, stop=lst), reads=[ones, pt], writes=[dn])
                P.add("dve", lambda e, dsb=dsb, dn=dn: e.reciprocal(out=dsb[:, 0:n], in_=dn[:, 0:n]), reads=[dn], writes=[dsb])
                P.add("dve", lambda e, o=o, ov=ov, dsb=dsb: e.tensor_tensor(out=o[:, 0:n], in0=ov[:, 0:n], in1=dsb[:, 0:n], op=ALU.mult),
                      reads=[ov, dsb], writes=[o])
                P.dma(oT.ap()[t0:t0 + nt, :, h, :].rearrange("i p t -> p i t"), o[:, 0:n].rearrange("p (i t) -> p i t", i=nt),
                      reads=[o], writes=[("oT", t0)])
            for (t0_, nt_, ch_) in qgroups:
                do_qg(t0_, nt_, ch_)
    P.barrier()


SSD_DI = 8192
SSD_XBC = 10240


def pass_ssd_proj(C, hT, w_in, xbc_raw, szd, dtd):
    P = C.P

    def do_block(blk):
        with ExitStack() as es:
            act = C.sb(es, [128, 9, KC, 128], BF16, "act")
            rw = [C.sb(es, [128, 384], F32, "rw") for _ in range(3)]
            zo = [C.sb(es, [128, 512], BF16, "zo") for _ in range(3)]
            do_ = [C.sb(es, [128, 256], F32, "do") for _ in range(2)]
            gps = [C.ps(es, [128, 512], F32, "gps") for _ in range(4)]
            load_act(C, act, hT, blk * 9, 9, "hT")
            cnt = [0, 0, 0]

            def epi(pp, j, tt0, ntl):
                n = ntl * 128
                r = rw[cnt[0] % 3]
                cnt[0] += 1
                P.add("act", lambda e: e.copy(out=r[:, 0:n], in_=pp[:, 0:n]), reads=[pp], writes=[r])
                tg = (blk * 9 + tt0) * 128
                P.dma(xbc_raw.ap()[j, :, tg:tg + n], r[:, 0:n], reads=[r], writes=[("xbc", j)])

            gemm_ws(C, es, act, 9, KC, w_in, SSD_DI, SSD_XBC, epi, "w", psb=gps)

            def epz(pp, ti, c0):
                o = zo[cnt[1] % 3]
                cnt[1] += 1
                P.add("act", lambda e: e.activation(out=o[:, :], in_=pp[:, 0:512], func=AF.Silu), reads=[pp], writes=[o])
                tg = (blk * 9 + ti) * 128
                P.dma(szd.ap()[tg:tg + 128, c0:c0 + 512], o[:, :], reads=[o], writes=[("sz", c0)])

            gemm_ts(C, es, act, 9, KC, w_in, 0, SSD_DI, epz, psb=gps)

            def epd(pp, ti, c0):
                o = do_[cnt[2] % 2]
                cnt[2] += 1
                P.add("act", lambda e: e.copy(out=o[:, :], in_=pp[:, 0:256]), reads=[pp], writes=[o])
                tg = (blk * 9 + ti) * 128
                P.dma(dtd.ap()[tg:tg + 128, :], o[:, :], reads=[o], writes=[("dtd", 0)])

            gemm_ts(C, es, act, 9, KC, w_in, SSD_DI + SSD_XBC, 256, epd, cb=256, psb=gps)
        P.barrier()

    for b_ in range(2):
        do_block(b_)


def pass_ssd_conv(C, xbc_raw, conv_w, conv_b, ident_bf, xs_tm, B_tm, BCT):
    P = C.P
    with ExitStack() as es:
        idb = C.sb(es, [128, 128], BF16, "idb")
        cw = C.sb(es, [128, 80, 4], F32, "cw")
        xr = [C.sb(es, [128, NT], F32, "xr") for _ in range(2)]
        ya = [C.sb(es, [128, NT], F32, "ya") for _ in range(2)]
        yb = [C.sb(es, [128, NT], BF16, "yb") for _ in range(2)]
        tm = [C.sb(es, [128, NTT, 128], BF16, "tm") for _ in range(2)]
        ptr = [C.ps(es, [128, 1024], BF16, "ptr") for _ in range(3)]
        P.dma(idb[:, :], ident_bf.ap(), writes=[idb])
        for c0_ in range(0, 80, 16):
            for kk in range(3):
                P.dma(cw[:, c0_:c0_ + 16, kk], conv_w[kk, c0_ * 128:(c0_ + 16) * 128].rearrange("(c p) -> p c", p=128), writes=[cw], slow=True)
            P.dma(cw[:, c0_:c0_ + 16, 3], conv_b[c0_ * 128:(c0_ + 16) * 128].rearrange("(c p) -> p c", p=128), writes=[cw], slow=True)
        npt = [0]

        def do_chunk(j):
            x = xr[j % 2]
            y = ya[j % 2]
            yo = yb[j % 2]
            P.dma(x[:, :], xbc_raw.ap()[j], reads=[("xbc", j)], writes=[x])
            for (a, b) in ((0, NL), (NL, NT)):
                P.add("dve", lambda e: e.tensor_scalar(out=y[:, a:b], in0=x[:, a:b], scalar1=cw[:, j, 1:2], scalar2=None, op0=ALU.mult),
                      reads=[x, cw], writes=[y]) if False else None
            P.add("dve", lambda e: e.tensor_scalar(out=y[:, :], in0=x[:, :], scalar1=cw[:, j, 1:2], scalar2=None, op0=ALU.mult),
                  reads=[x, cw], writes=[y])
            for (a, b) in ((0, NL), (NL, NT)):
                P.add("dve", lambda e, a=a, b=b: e.scalar_tensor_tensor(out=y[:, a + 1:b], in0=x[:, a:b - 1], scalar=cw[:, j, 0:1],
                                                                        in1=y[:, a + 1:b], op0=ALU.mult, op1=ALU.add),
                      reads=[x, cw, y], writes=[y])
                P.add("dve", lambda e, a=a, b=b: e.scalar_tensor_tensor(out=y[:, a:b - 1], in0=x[:, a + 1:b], scalar=cw[:, j, 2:3],
                                                                        in1=y[:, a:b - 1], op0=ALU.mult, op1=ALU.add),
                      reads=[x, cw, y], writes=[y])
            P.add("act", lambda e: e.activation(out=yo[:, :], in_=y[:, :], func=AF.Silu, bias=cw[:, j, 3:4]), reads=[y, cw], writes=[yo])
            if j >= 64:
                P.dma(BCT.ap()[j - 64], yo[:, :], reads=[yo], writes=[("BCT", j - 64)])
            if j < 72:
                t = tm[j % 2]
                for q in range(0, NTT, 8):
                    nq = min(8, NTT - q)
                    pt = ptr[npt[0] % 3]
                    npt[0] += 1
                    for i in range(nq):
                        P.add("pe", lambda e, i=i, q=q, pt=pt: e.transpose(out=pt[:, i * 128:(i + 1) * 128], in_=yo[:, (q + i) * 128:(q + i + 1) * 128],
                                                                           identity=idb[:, :]), reads=[yo, idb], writes=[pt])
                    P.add("act" if (q // 8) % 2 == 0 else "dve",
                          (lambda e, q=q, nq=nq, pt=pt: e.copy(out=t[:, q:q + nq, :], in_=pt[:, 0:nq * 128].rearrange("p (i c) -> p i c", i=nq)))
                          if (q // 8) % 2 == 0 else
                          (lambda e, q=q, nq=nq, pt=pt: e.tensor_copy(out=t[:, q:q + nq, :], in_=pt[:, 0:nq * 128].rearrange("p (i c) -> p i c", i=nq))),
                          reads=[pt], writes=[t])
                if j < 64:
                    P.dma(xs_tm.ap()[:, j * 128:(j + 1) * 128].rearrange("(i p) c -> p i c", p=128), t[:, :, :], reads=[t], writes=[("xs_tm", 0)])
                else:
                    P.dma(B_tm.ap()[:, (j - 64) * 128:(j - 63) * 128].rearrange("(i p) c -> p i c", p=128), t[:, :, :], reads=[t],
                          writes=[("B_tm", 0)])

        for j_ in range(80):
            do_chunk(j_)
    P.barrier()


def pass_ssd_scan(C, xs_tm, B_tm, BCT, dtd, dt_bias, a_log, d_skip, ident, ident_bf, Ufb, negm, ydr):
    P = C.P
    with ExitStack() as es:
        idt = C.sb(es, [128, 128], F32, "idt")
        idb = C.sb(es, [128, 128], BF16, "idb")
        U = C.sb(es, [128, 2, 128], F32, "U")
        NM = C.sb(es, [128, 2, 128], BF16, "NM")
        onesf = C.sb(es, [128, 128], F32, "onesf")
        bia = C.sb(es, [128, 2, 128], F32, "bia")
        av = C.sb(es, [128, 2, 128], F32, "av")
        dsk = C.sb(es, [128, 128], F32, "dsk")
        xst = [C.sb(es, [128, SSD_DI], BF16, "xst") for _ in range(2)]
        bt = [C.sb(es, [128, 1024], BF16, "bt") for _ in range(2)]
        bcT = [C.sb(es, [128, 16, 128], BF16, "bcT") for _ in range(2)]
        dtr = [C.sb(es, [128, 256], F32, "dtr") for _ in range(2)]
        dt = C.sb(es, [128, 128], F32, "dt")
        dta = C.sb(es, [128, 128], F32, "dta")
        ncum = C.sb(es, [128, 128], F32, "ncum")
        ecum = C.sb(es, [128, 128], F32, "ecum")
        clast = C.sb(es, [128, 128], F32, "clast")
        eclast = C.sb(es, [128, 128], F32, "eclast")
        te = C.sb(es, [128, 128], F32, "te")
        cumF = C.sb(es, [128, 128], F32, "cumF")
        stF = C.sb(es, [128, 8, 1024], F32, "stF")
        stB = C.sb(es, [128, 8, 1024], BF16, "stB")
        xdt = [C.sb(es, [128, 1024], BF16, "xdt") for _ in range(2)]
        xte = [C.sb(es, [128, 1024], BF16, "xte") for _ in range(2)]
        cb = [C.sb(es, [128, 128], F32, "cb") for _ in range(2)]
        E = [C.sb(es, [128, 128], F32, "E") for _ in range(3)]
        M = [C.sb(es, [128, 128], BF16, "M") for _ in range(3)]
        tmp = [C.sb(es, [128, 1024], F32, "tmp") for _ in range(2)]
        yo = [C.sb(es, [128, 1024], F32, "yo") for _ in range(2)]
        yp = [C.sb(es, [128, 1024], F32, "yp") for _ in range(2)]
        pc = C.ps(es, [128, 512], F32, "pc")
        pR = [C.ps(es, [128, 512], F32, "pR") for _ in range(2)]
        pY = C.ps(es, [128, 1024], F32, "pY")
        pI = C.ps(es, [128, 1024], F32, "pI")
        P.dma(idt[:, :], ident.ap(), writes=[idt])
        P.dma(idb[:, :], ident_bf.ap(), writes=[idb])
        P.dma(U[:, :, :], Ufb.ap().rearrange("d s t -> s d t"), writes=[U])
        P.dma(NM[:, :, :], negm.ap().rearrange("d s t -> s d t"), writes=[NM])
        P.add("pool", lambda e: e.memset(onesf[:, :], 1.0), writes=[onesf])
        for d in range(2):
            P.dma(bia[:, d, :], dt_bias[d].partition_broadcast(128), writes=[bia])
            P.dma(av[:, d, :], a_log[d].partition_broadcast(128), writes=[av])
        P.dma(dsk[:, :], d_skip.partition_broadcast(128), writes=[dsk])
        P.add("act", lambda e: e.activation(out=av[:, :, :], in_=av[:, :, :], func=AF.Exp), reads=[av], writes=[av])
        P.add("dve", lambda e: e.tensor_scalar(out=av[:, :, :], in0=av[:, :, :], scalar1=-1.0, scalar2=None, op0=ALU.mult),
              reads=[av], writes=[av])
        cnt = {"c": 0, "e": 0, "g": 0}

        def do_chunk(d, i, first):
            c = cnt["c"] % 2
            cnt["c"] += 1
            xs, b_, bc, dr = xst[c], bt[c], bcT[c], dtr[c]
            r0 = i * 128
            P.dma(xs[:, :], xs_tm.ap()[r0:r0 + 128, :], reads=[("xs_tm", 0)], writes=[xs])
            P.dma(b_[:, :], B_tm.ap()[r0:r0 + 128, :], reads=[("B_tm", 0)], writes=[b_])
            P.dma(bc[:, :, :], BCT.ap()[:, :, r0:r0 + 128].rearrange("g n t -> n g t"), reads=[("BCT", q) for q in range(16)], writes=[bc])
            P.dma(dr[:, :], dtd.ap()[r0:r0 + 128, :], reads=[("dtd", 0)], writes=[dr])
            P.add("dve", lambda e: e.tensor_tensor(out=dt[:, :], in0=dr[:, d * 128:(d + 1) * 128], in1=bia[:, d, :], op=ALU.add),
                  reads=[dr, bia], writes=[dt])
            P.add("act", lambda e: e.activation(out=dt[:, :], in_=dt[:, :], func=AF.Exp), reads=[dt], writes=[dt])
            P.add("dve", lambda e: e.tensor_scalar(out=dt[:, :], in0=dt[:, :], scalar1=1.0, scalar2=None, op0=ALU.add), reads=[dt], writes=[dt])
            P.add("act", lambda e: e.activation(out=dt[:, :], in_=dt[:, :], func=AF.Ln), reads=[dt], writes=[dt])
            P.add("dve", lambda e: e.tensor_tensor(out=dta[:, :], in0=dt[:, :], in1=av[:, d, :], op=ALU.mult), reads=[dt, av], writes=[dta])
            P.add("pe", lambda e: e.matmul(pc[:, 0:128], lhsT=U[:, d, :], rhs=dta[:, :], start=True, stop=True), reads=[U, dta], writes=[pc])
            P.add("pe", lambda e: e.matmul(pc[:, 128:256], lhsT=onesf[:, :], rhs=dta[:, :], start=True, stop=True), reads=[onesf, dta],
                  writes=[pc])
            P.add("pe", lambda e: e.matmul(pc[:, 256:384], lhsT=dta[:, :], rhs=U[:, d, :], start=True, stop=True), reads=[U, dta],
                  writes=[pc])
            P.add("dve", lambda e: e.tensor_scalar(out=ncum[:, :], in0=pc[:, 0:128], scalar1=-1.0, scalar2=None, op0=ALU.mult),
                  reads=[pc], writes=[ncum])
            P.add("act", lambda e: e.activation(out=ecum[:, :], in_=pc[:, 0:128], func=AF.Exp), reads=[pc], writes=[ecum])
            P.add("act", lambda e: e.copy(out=clast[:, :], in_=pc[:, 128:256]), reads=[pc], writes=[clast])
            P.add("act", lambda e: e.activation(out=eclast[:, :], in_=pc[:, 128:256], func=AF.Exp), reads=[pc], writes=[eclast])
            P.add("dve", lambda e: e.tensor_copy(out=cumF[:, :], in_=pc[:, 256:384]), reads=[pc], writes=[cumF])
            P.add("dve", lambda e: e.tensor_tensor(out=te[:, :], in0=clast[:, :], in1=ncum[:, :], op=ALU.add), reads=[clast, ncum], writes=[te])
            P.add("act", lambda e: e.activation(out=te[:, :], in_=te[:, :], func=AF.Exp), reads=[te], writes=[te])

            STOP = int(os.environ.get("SSD_STOP", "9"))
            if STOP <= 1:
                return

            def do_group(g):
                gc = cnt["g"] % 2
                cnt["g"] += 1
                xd, xt, cbs, tp, yy, ypv = xdt[gc], xte[gc], cb[gc], tmp[gc], yo[gc], yp[gc]
                hs = slice(g * 16, (g + 1) * 16)
                xg = xs[:, g * 1024:(g + 1) * 1024].rearrange("p (j q) -> p j q", j=16)
                P.add("dve", lambda e: e.tensor_tensor(out=xd[:, :].rearrange("p (j q) -> p j q", j=16), in0=xg,
                                                       in1=dt[:, hs, None].to_broadcast([128, 16, 64]), op=ALU.mult), reads=[xs, dt], writes=[xd])
                P.add("dve", lambda e: e.tensor_tensor(out=xt[:, :].rearrange("p (j q) -> p j q", j=16),
                                                        in0=xd[:, :].rearrange("p (j q) -> p j q", j=16),
                                                        in1=te[:, hs, None].to_broadcast([128, 16, 64]), op=ALU.mult), reads=[xd, te], writes=[xt])
                P.add("pe", lambda e: e.matmul(pc[:, 384:512], lhsT=bc[:, g, :], rhs=bc[:, 8 + g, :], start=True, stop=True), reads=[bc],
                      writes=[pc])
                P.add("act", lambda e: e.copy(out=cbs[:, :], in_=pc[:, 384:512]), reads=[pc], writes=[cbs])

                if STOP <= 2:
                    return

                def do_head(jj):
                    j = g * 16 + jj
                    ec = cnt["e"]
                    cnt["e"] += 1
                    pr = pR[ec % 2]
                    Ej = E[ec % 3]
                    Mj = M[ec % 3]
                    P.add("pe", lambda e: e.matmul(pr[:, 0:128], lhsT=idt[:, j:j + 1].to_broadcast([128, 128]), rhs=cumF[:, :], start=True, stop=False),
                          reads=[idt, cumF], writes=[pr])
                    P.add("pe", lambda e: e.matmul(pr[:, 0:128], lhsT=idb[:, :], rhs=NM[:, d, :], start=False, stop=True), reads=[idb, NM], writes=[pr])
                    P.add("act", lambda e: e.activation(out=Ej[:, :], in_=pr[:, 0:128], func=AF.Exp, bias=ncum[:, j:j + 1]),
                          reads=[pr, ncum], writes=[Ej])
                    P.add("dve", lambda e: e.tensor_tensor(out=Mj[:, :], in0=Ej[:, :], in1=cbs[:, :], op=ALU.mult), reads=[Ej, cbs], writes=[Mj])
                    P.add("pe", lambda e: e.matmul(pY[:, jj * 64:(jj + 1) * 64], lhsT=Mj[:, :], rhs=xd[:, jj * 64:(jj + 1) * 64], start=True, stop=True),
                          reads=[Mj, xd], writes=[pY])

                for jj_ in range(16):
                    do_head(jj_)
                if STOP <= 3:
                    return
                if d == 0:
                    P.add("dve", lambda e: e.tensor_tensor(out=ypv[:, :].rearrange("p (j q) -> p j q", j=16), in0=xg,
                                                            in1=dsk[:, hs, None].to_broadcast([128, 16, 64]), op=ALU.mult), reads=[xs, dsk], writes=[ypv])
                else:
                    P.dma(ypv[:, :], ydr.ap()[r0:r0 + 128, g * 1024:(g + 1) * 1024], reads=[("ydr", i, g)], writes=[ypv])
                if not first:
                    for hh in range(2):
                        P.add("pe", lambda e, hh=hh: e.matmul(pI[:, hh * 512:(hh + 1) * 512], lhsT=bc[:, 8 + g, :], rhs=stB[:, g, hh * 512:(hh + 1) * 512],
                                                              start=True, stop=True), reads=[bc, (stB.name, g)], writes=[pI])
                    P.add("dve", lambda e: e.tensor_tensor(out=tp[:, :].rearrange("p (j q) -> p j q", j=16),
                                                           in0=pI[:, :].rearrange("p (j q) -> p j q", j=16),
                                                           in1=ecum[:, hs, None].to_broadcast([128, 16, 64]), op=ALU.mult), reads=[pI, ecum], writes=[tp])
                    P.add("pool", lambda e: e.tensor_tensor(out=ypv[:, :], in0=ypv[:, :], in1=tp[:, :], op=ALU.add), reads=[ypv, tp], writes=[ypv])
                P.add("dve", lambda e: e.tensor_tensor(out=yy[:, :], in0=pY[:, :], in1=ypv[:, :], op=ALU.add), reads=[pY, ypv], writes=[yy])
                P.dma(ydr.ap()[r0:r0 + 128, g * 1024:(g + 1) * 1024], yy[:, :], reads=[yy], writes=[("ydr", i, g)], eng="act")
                for hh in range(2):
                    P.add("pe", lambda e, hh=hh: e.matmul(pI[:, hh * 512:(hh + 1) * 512], lhsT=b_[:, g * 128:(g + 1) * 128], rhs=xt[:, hh * 512:(hh + 1) * 512],
                                                          start=True, stop=True), reads=[b_, xt], writes=[pI])
                if first:
                    P.add("dve", lambda e: e.tensor_copy(out=stF[:, g, :], in_=pI[:, :]), reads=[pI], writes=[(stF.name, g)])
                else:
                    P.add("dve", lambda e: e.tensor_tensor(out=stF[:, g, :].rearrange("p (j q) -> p j q", j=16),
                                                            in0=stF[:, g, :].rearrange("p (j q) -> p j q", j=16),
                                                            in1=eclast[:, hs, None].to_broadcast([128, 16, 64]), op=ALU.mult),
                          reads=[(stF.name, g), eclast], writes=[(stF.name, g)])
                    P.add("dve", lambda e: e.tensor_tensor(out=stF[:, g, :], in0=stF[:, g, :], in1=pI[:, :], op=ALU.add),
                          reads=[(stF.name, g), pI], writes=[(stF.name, g)])
                P.add("act", lambda e: e.copy(out=stB[:, g, :], in_=stF[:, g, :]), reads=[(stF.name, g)], writes=[(stB.name, g)])

            for g_ in range(8):
                do_group(g_)

        for d_ in range(int(os.environ.get("SSD_NDIR", "2"))):
            order = [16, 17] + list(range(16)) if d_ == 0 else [17, 16] + list(range(15, -1, -1))
            for n_, i_ in enumerate(order[:int(os.environ.get("SSD_NCH", "18"))]):
                do_chunk(d_, i_, n_ == 0)
    P.barrier()


def pass_ssd_finish(C, ydr, szd, norm_g, ident_bf, gT):
    P = C.P
    with ExitStack() as es:
        idb = C.sb(es, [128, 128], BF16, "idb")
        ng = C.sb(es, [128, 64], F32, "ng")
        yt = [C.sb(es, [128, SSD_DI], F32, "yt") for _ in range(2)]
        zt = [C.sb(es, [128, SSD_DI], BF16, "zt") for _ in range(2)]
        junk = C.sb(es, [128, 1024], BF16, "junk")
        gb = [C.sb(es, [128, SSD_DI], BF16, "gb") for _ in range(2)]
        st = [C.sb(es, [128, 32], F32, "st") for _ in range(2)]
        hst = [C.sb(es, [128, 64, 128], BF16, "hst") for _ in range(2)]
        ptr = [C.ps(es, [128, 1024], BF16, "ptr") for _ in range(3)]
        P.dma(idb[:, :], ident_bf.ap(), writes=[idb])
        for c0_ in range(0, 64, 16):
            P.dma(ng[:, c0_:c0_ + 16], norm_g[c0_ * 128:(c0_ + 16) * 128].rearrange("(c p) -> p c", p=128), writes=[ng], slow=True)
        npt = [0]

        def do_tile(i):
            y, z_, g_, s_, hs = yt[i % 2], zt[i % 2], gb[i % 2], st[i % 2], hst[i % 2]
            P.dma(y[:, :], ydr.ap()[i * 128:(i + 1) * 128, :], reads=[("ydr", i, q) for q in range(8)], writes=[y])
            P.dma(z_[:, :], szd.ap()[i * 128:(i + 1) * 128, :], reads=[("sz", c0) for c0 in range(0, SSD_DI, 512)], writes=[z_])
            P.add("dve", lambda e: e.tensor_tensor(out=y[:, :], in0=y[:, :], in1=z_[:, :], op=ALU.mult), reads=[y, z_], writes=[y])
            for q in range(8):
                P.add("act", lambda e, q=q: e.activation(out=junk[:, :], in_=y[:, q * 1024:(q + 1) * 1024], func=AF.Square, accum_out=s_[:, q:q + 1]),
                      reads=[y], writes=[junk, s_])
            P.add("dve", lambda e: e.tensor_scalar(out=s_[:, 8:16], in0=s_[:, 0:8], scalar1=1.0 / 1024, scalar2=EPS, op0=ALU.mult, op1=ALU.add),
                  reads=[s_], writes=[s_])
            P.add("act", lambda e: e.activation(out=s_[:, 8:16], in_=s_[:, 8:16], func=AF.Sqrt), reads=[s_], writes=[s_])
            P.add("dve", lambda e: e.reciprocal(out=s_[:, 16:24], in_=s_[:, 8:16]), reads=[s_], writes=[s_])
            for q in range(8):
                P.add("dve" if q % 2 == 0 else "pool", lambda e, q=q: e.tensor_scalar(out=g_[:, q * 1024:(q + 1) * 1024], in0=y[:, q * 1024:(q + 1) * 1024],
                                                                                      scalar1=s_[:, 16 + q:17 + q], scalar2=None, op0=ALU.mult),
                      reads=[y, s_], writes=[g_])
            for q in range(8):
                pt = ptr[npt[0] % 3]
                npt[0] += 1
                for kk in range(8):
                    k = q * 8 + kk
                    P.add("pe", lambda e, kk=kk, k=k, pt=pt: e.transpose(out=pt[:, kk * 128:(kk + 1) * 128], in_=g_[:, k * 128:(k + 1) * 128],
                                                                         identity=idb[:, :]), reads=[g_, idb], writes=[pt])
                for kk in range(8):
                    k = q * 8 + kk
                    P.add("act", lambda e, kk=kk, k=k, pt=pt: e.activation(out=hs[:, k, :], in_=pt[:, kk * 128:(kk + 1) * 128], func=AF.Identity,
                                                                           scale=ng[:, k:k + 1]), reads=[pt, ng], writes=[hs])
            P.dma(gT.ap()[i], hs[:, :, :], reads=[hs], writes=[("aT", i)])

        for i_ in range(NTT):
            do_tile(i_)
    P.barrier()


TWO_PI = 2.0 * np.pi


def pass_s5_proj(C, hT, w_in, uT_d, u_tm):
    P = C.P

    def do_block(blk):
        with ExitStack() as es:
            act = C.sb(es, [128, 9, KC, 128], BF16, "act")
            rw = [C.sb(es, [128, 384], BF16, "rw") for _ in range(3)]
            uo = [C.sb(es, [128, 512], F32, "uo") for _ in range(3)]
            gps = [C.ps(es, [128, 512], F32, "gps") for _ in range(4)]
            load_act(C, act, hT, blk * 9, 9, "hT")
            cnt = [0, 0]

            def epi(pp, j, tt0, ntl):
                n = ntl * 128
                r = rw[cnt[0] % 3]
                cnt[0] += 1
                P.add("act", lambda e: e.copy(out=r[:, 0:n], in_=pp[:, 0:n]), reads=[pp], writes=[r])
                tg = (blk * 9 + tt0) * 128
                P.dma(uT_d.ap()[j, :, tg:tg + n], r[:, 0:n], reads=[r], writes=[("uT", 0)])

            gemm_ws(C, es, act, 9, KC, w_in, 0, D, epi, "w", psb=gps)

            def epu(pp, ti, c0):
                o = uo[cnt[1] % 3]
                cnt[1] += 1
                P.add("dve", lambda e: e.tensor_copy(out=o[:, :], in_=pp[:, 0:512]), reads=[pp], writes=[o])
                tg = (blk * 9 + ti) * 128
                P.dma(u_tm.ap()[tg:tg + 128, c0:c0 + 512], o[:, :], reads=[o], writes=[("u_tm", 0)])

            gemm_ts(C, es, act, 9, KC, w_in, 0, D, epu, psb=gps)
        P.barrier()

    for b_ in range(2):
        do_block(b_)


def s5_lambda_bar(P, lr, li, stp, ar, ai, t1, t2, ti):
    def K(a):
        return a.tensor
    P.add("act", lambda e: e.activation(out=stp, in_=stp, func=AF.Exp), reads=[K(stp)], writes=[K(stp)])
    P.add("dve", lambda e: e.tensor_tensor(out=t1, in0=lr, in1=stp, op=ALU.mult), reads=[K(lr), K(stp)], writes=[K(t1)])
    P.add("act", lambda e: e.activation(out=t1, in_=t1, func=AF.Exp), reads=[K(t1)], writes=[K(t1)])
    P.add("dve", lambda e: e.tensor_tensor(out=t2, in0=li, in1=stp, op=ALU.mult), reads=[K(li), K(stp)], writes=[K(t2)])

    def reduced_sin(dst, shift):
        P.add("dve", lambda e: e.tensor_scalar(out=dst, in0=t2, scalar1=shift, scalar2=1.0 / TWO_PI, op0=ALU.add, op1=ALU.mult),
              reads=[K(t2)], writes=[K(dst)])
        P.add("dve", lambda e: e.tensor_copy(out=ti, in_=dst), reads=[K(dst)], writes=[K(ti)])
        P.add("dve", lambda e: e.tensor_copy(out=dst, in_=ti), reads=[K(ti)], writes=[K(dst)])
        P.add("dve", lambda e: e.tensor_scalar(out=dst, in0=dst, scalar1=-TWO_PI, scalar2=shift, op0=ALU.mult, op1=ALU.add),
              reads=[K(dst)], writes=[K(dst)])
        P.add("dve", lambda e: e.tensor_tensor(out=dst, in0=dst, in1=t2, op=ALU.add), reads=[K(dst), K(t2)], writes=[K(dst)])
        for (thr, cmp_, adj) in ((np.pi, ALU.is_gt, -TWO_PI), (-np.pi, ALU.is_lt, TWO_PI)):
            P.add("dve", lambda e, thr=thr, cmp_=cmp_, adj=adj: e.tensor_scalar(out=stp, in0=dst, scalar1=thr, scalar2=adj, op0=cmp_, op1=ALU.mult),
                  reads=[K(dst)], writes=[K(stp)])
            P.add("dve", lambda e: e.tensor_tensor(out=dst, in0=dst, in1=stp, op=ALU.add), reads=[K(dst), K(stp)], writes=[K(dst)])
        P.add("act", lambda e: e.activation(out=dst, in_=dst, func=AF.Sin), reads=[K(dst)], writes=[K(dst)])

    reduced_sin(ai, 0.0)
    reduced_sin(ar, 0.5 * np.pi)
    P.add("dve", lambda e: e.tensor_tensor(out=ar, in0=ar, in1=t1, op=ALU.mult), reads=[K(ar), K(t1)], writes=[K(ar)])
    P.add("dve", lambda e: e.tensor_tensor(out=ai, in0=ai, in1=t1, op=ALU.mult), reads=[K(ai), K(t1)], writes=[K(ai)])


def pass_s5_scan(C, uT_d, lam_re, lam_im, log_dt, b_re, b_im, c_re, c_im, ident, rowmask, cmask, y5, dbg=None):
    P = C.P
    with ExitStack() as es:
        idt = C.sb(es, [128, 128], F32, "idt")
        rmk = C.sb(es, [128, 2], F32, "rmk")
        cmk = C.sb(es, [128, 128], F32, "cmk")
        CP = C.sb(es, [128, 128, 2, 32], BF16, "CP")
        BP = C.sb(es, [128, 2, KC, 2, 128], BF16, "BP")
        arS = C.sb(es, [128, 2, 128], F32, "arS")
        aiS = C.sb(es, [128, 2, 128], F32, "aiS")
        tt = [C.sb(es, [128, 128], F32, "tt") for _ in range(4)]
        px = [C.ps(es, [128, 512], F32, "px") for _ in range(2)]
        py = C.ps(es, [128, 2048], F32, "py")
        P.dma(idt[:, :], ident.ap(), writes=[idt])
        P.dma(rmk[:, :], rowmask.ap(), writes=[rmk])
        P.dma(cmk[:, :], cmask.ap(), writes=[cmk])
        with ExitStack() as es2:
            cin_ = C.sb(es2, [128, KC, 2, 64], F32, "cin")
            for ri, csrc in enumerate((c_re, c_im)):
                for dup in range(2):
                    for k0_ in range(0, KC, 8):
                        P.dma(cin_[:, k0_:k0_ + 8, dup, :],
                              csrc.rearrange("g c p -> (g c) p")[k0_ * 128:(k0_ + 8) * 128, :].rearrange("(k r) p -> r k p", r=128), writes=[cin_])
                P.add("dve", lambda e: e.tensor_tensor(out=cin_[:, :, :, :].rearrange("r k d p -> r k (d p)"),
                                                       in0=cin_[:, :, :, :].rearrange("r k d p -> r k (d p)"),
                                                       in1=cmk[:, None, :].to_broadcast([128, KC, 128]), op=ALU.mult), reads=[cin_, cmk], writes=[cin_])
                for kc in range(KC):
                    pp = px[kc % 2]
                    P.add("pe", lambda e, kc=kc, pp=pp: e.transpose(out=pp[:, 0:128], in_=cin_[:, kc, :, :].rearrange("r d p -> r (d p)"),
                                                                    identity=idt[:, :]), reads=[cin_, idt], writes=[pp])
                    P.add("act", lambda e, kc=kc, pp=pp, ri=ri: e.activation(out=CP[:, 4 * kc:4 * kc + 4, ri, :],
                                                                             in_=pp[:, 0:128].rearrange("p (q c) -> p q c", q=4),
                                                                             func=AF.Copy if ri == 0 else AF.Identity,
                                                                             scale=1.0 if ri == 0 else -1.0), reads=[pp], writes=[CP])
        P.barrier()
        chunks64 = {0: [2048 + 64 * i for i in range(4)] + [64 * i for i in range(32)],
                    1: [2048 + 64 * i for i in range(3, -1, -1)] + [64 * i for i in range(31, -1, -1)]}
        nst = [0]

        def prep_dir(d):
            with ExitStack() as es2:
                lr = C.sb(es2, [64, 256], F32, "lr")
                li = C.sb(es2, [64, 256], F32, "li")
                sp_ = C.sb(es2, [64, 256], F32, "sp")
                ar = C.sb(es2, [64, 256], F32, "ar")
                ai = C.sb(es2, [64, 256], F32, "ai")
                w1_ = C.sb(es2, [64, 256], F32, "w1")
                w2_ = C.sb(es2, [64, 256], F32, "w2")
                fr = C.sb(es2, [64, 256], F32, "fr")
                fi = C.sb(es2, [64, 256], F32, "fi")
                br = C.sb(es2, [64, 256, 16], F32, "br")
                bi = C.sb(es2, [64, 256, 16], F32, "bi")
                bbr = C.sb(es2, [64, 256, 16], F32, "bbr")
                bbi = C.sb(es2, [64, 256, 16], F32, "bbi")
                lrS = C.sb(es2, [128, 128], F32, "lrS")
                liS = C.sb(es2, [128, 128], F32, "liS")
                spS = C.sb(es2, [128, 128], F32, "spS")
                tiS = C.sb(es2, [128, 128], I32, "tiS")
                tiL = C.sb(es2, [64, 256], I32, "tiL")
                for q0 in range(0, 256, 64):
                    P.dma(lr[:, q0:q0 + 64], lam_re[d, q0:q0 + 64, :].rearrange("g p -> p g"), writes=[lr], slow=True)
                    P.dma(li[:, q0:q0 + 64], lam_im[d, q0:q0 + 64, :].rearrange("g p -> p g"), writes=[li], slow=True)
                P.dma(sp_[:, :], log_dt[d].partition_broadcast(64), writes=[sp_])
                for g0_ in range(0, 256, 32):
                    P.dma(br[:, g0_:g0_ + 32, :], b_re[g0_:g0_ + 32].rearrange("g p c -> p g c"), writes=[br])
                    P.dma(bi[:, g0_:g0_ + 32, :], b_im[g0_:g0_ + 32].rearrange("g p c -> p g c"), writes=[bi])
                for g2 in range(2):
                    for q0 in range(0, 128, 64):
                        P.dma(lrS[g2 * 64:(g2 + 1) * 64, q0:q0 + 64],
                              lam_re[d].rearrange("(q t) p -> t p q", t=2)[g2, :, q0:q0 + 64], writes=[lrS], slow=True)
                        P.dma(liS[g2 * 64:(g2 + 1) * 64, q0:q0 + 64],
                              lam_im[d].rearrange("(q t) p -> t p q", t=2)[g2, :, q0:q0 + 64], writes=[liS], slow=True)
                    P.dma(spS[g2 * 64:(g2 + 1) * 64, :], log_dt[d].rearrange("(q t) -> t q", t=2)[g2].partition_broadcast(64),
                          writes=[spS], slow=True)
                s5_lambda_bar(P, lrS[:, :], liS[:, :], spS[:, :], arS[:, d, :], aiS[:, d, :], tt[0][:, :], tt[1][:, :], tiS[:, :])
                s5_lambda_bar(P, lr[:, :], li[:, :], sp_[:, :], ar[:, :], ai[:, :], w1_[:, :], w2_[:, :], tiL[:, :])
                P.add("dve", lambda e: e.tensor_tensor(out=w1_[:, :], in0=lr[:, :], in1=lr[:, :], op=ALU.mult), reads=[lr], writes=[w1_])
                P.add("dve", lambda e: e.tensor_tensor(out=w2_[:, :], in0=li[:, :], in1=li[:, :], op=ALU.mult), reads=[li], writes=[w2_])
                P.add("dve", lambda e: e.tensor_tensor(out=w1_[:, :], in0=w1_[:, :], in1=w2_[:, :], op=ALU.add), reads=[w1_, w2_], writes=[w1_])
                P.add("dve", lambda e: e.reciprocal(out=w1_[:, :], in_=w1_[:, :]), reads=[w1_], writes=[w1_])
                P.add("dve", lambda e: e.tensor_scalar(out=ar[:, :], in0=ar[:, :], scalar1=-1.0, scalar2=None, op0=ALU.add), reads=[ar], writes=[ar])
                P.add("dve", lambda e: e.tensor_tensor(out=fr[:, :], in0=ar[:, :], in1=lr[:, :], op=ALU.mult), reads=[ar, lr], writes=[fr])
                P.add("dve", lambda e: e.tensor_tensor(out=w2_[:, :], in0=ai[:, :], in1=li[:, :], op=ALU.mult), reads=[ai, li], writes=[w2_])
                P.add("dve", lambda e: e.tensor_tensor(out=fr[:, :], in0=fr[:, :], in1=w2_[:, :], op=ALU.add), reads=[fr, w2_], writes=[fr])
                P.add("dve", lambda e: e.tensor_tensor(out=fr[:, :], in0=fr[:, :], in1=w1_[:, :], op=ALU.mult), reads=[fr, w1_], writes=[fr])
                P.add("dve", lambda e: e.tensor_tensor(out=fi[:, :], in0=ai[:, :], in1=lr[:, :], op=ALU.mult), reads=[ai, lr], writes=[fi])
                P.add("dve", lambda e: e.tensor_tensor(out=w2_[:, :], in0=ar[:, :], in1=li[:, :], op=ALU.mult), reads=[ar, li], writes=[w2_])
                P.add("dve", lambda e: e.tensor_tensor(out=fi[:, :], in0=fi[:, :], in1=w2_[:, :], op=ALU.subtract), reads=[fi, w2_], writes=[fi])
                P.add("dve", lambda e: e.tensor_tensor(out=fi[:, :], in0=fi[:, :], in1=w1_[:, :], op=ALU.mult), reads=[fi, w1_], writes=[fi])
                frb = fr[:, :, None].to_broadcast([64, 256, 16])
                fib = fi[:, :, None].to_broadcast([64, 256, 16])
                P.add("dve", lambda e: e.tensor_tensor(out=bbr[:, :, :], in0=br[:, :, :], in1=frb, op=ALU.mult), reads=[br, fr], writes=[bbr])
                P.add("dve", lambda e: e.tensor_tensor(out=bbi[:, :, :], in0=bi[:, :, :], in1=frb, op=ALU.mult), reads=[bi, fr], writes=[bbi])
                P.add("dve", lambda e: e.tensor_tensor(out=bi[:, :, :], in0=bi[:, :, :], in1=fib, op=ALU.mult), reads=[bi, fi, bbi], writes=[bi])
                P.add("dve", lambda e: e.tensor_tensor(out=br[:, :, :], in0=br[:, :, :], in1=fib, op=ALU.mult), reads=[br, fi, bbr], writes=[br])
                P.add("dve", lambda e: e.tensor_tensor(out=bbr[:, :, :], in0=bbr[:, :, :], in1=bi[:, :, :], op=ALU.subtract), reads=[bbr, bi], writes=[bbr])
                P.add("pool", lambda e: e.tensor_tensor(out=bbi[:, :, :], in0=bbi[:, :, :], in1=br[:, :, :], op=ALU.add), reads=[bbi, br], writes=[bbi])
                for ri, bsrc in enumerate((bbr, bbi)):
                    for kc in range(KC):
                        pp = px[kc % 2]
                        P.add("pe", lambda e, kc=kc, pp=pp, bsrc=bsrc: e.transpose(out=pp[:, 0:64], in_=bsrc[:, 8 * kc:8 * kc + 8, :].rearrange("p g c -> p (g c)"),
                                                                                   identity=idt[0:64, 0:64]), reads=[bsrc, idt], writes=[pp])
                        P.add("dve", lambda e, kc=kc, pp=pp, ri=ri: e.tensor_scalar(out=BP[:, d, kc, ri, 0:64], in0=pp[:, 0:64], scalar1=rmk[:, 0:1], scalar2=None,
                                                                                    op0=ALU.mult), reads=[pp, rmk], writes=[BP])
                        P.add("dve", lambda e, kc=kc, pp=pp, ri=ri: e.tensor_scalar(out=BP[:, d, kc, ri, 64:128], in0=pp[:, 0:64], scalar1=rmk[:, 1:2], scalar2=None,
                                                                                    op0=ALU.mult), reads=[pp, rmk], writes=[BP])
        for d_ in range(2):
            prep_dir(d_)
            P.barrier()
        if dbg is not None:
            P.dma(dbg["arS"].ap(), arS[:, :, :], reads=[arS], writes=["dbg_arS"], is_out=True)
            P.dma(dbg["aiS"].ap(), aiS[:, :, :], reads=[aiS], writes=["dbg_aiS"], is_out=True)
            P.dma(dbg["CP"].ap(), CP[:, :, :, :], reads=[CP], writes=["dbg_CP"], is_out=True)
            P.dma(dbg["BP"].ap(), BP[:, :, :, :, :], reads=[BP], writes=["dbg_BP"], is_out=True)
            P.barrier()
        Sr = C.sb(es, [128, 128], F32, "Sr")
        Si = C.sb(es, [128, 128], F32, "Si")
        X = C.sb(es, [128, 2, 128, 64], BF16, "X")
        Hr = C.sb(es, [128, 128, 64], F32, "Hr")
        Hi = C.sb(es, [128, 128, 64], F32, "Hi")
        uq = [C.sb(es, [128, KC, 64], BF16, "uq") for _ in range(4)]
        yo = [C.sb(es, [64, 2048], F32, "yo") for _ in range(2)]
        for uq_ in uq:
            P.add("pool", lambda e, uq_=uq_: e.memset(uq_[:, :, :], 0.0), writes=[uq_])

        def do_dir(d):
            P.add("dve", lambda e: e.memset(Sr[:, :], 0.0), writes=[Sr])
            P.add("pool", lambda e: e.memset(Si[:, :], 0.0), writes=[Si])

            def do_chunk(ci, t0):
                for qq in range(4):
                    P.dma(uq[qq][qq * 32:(qq + 1) * 32, :, :], uT_d.ap()[:, qq * 32:(qq + 1) * 32, t0:t0 + 64].rearrange("k r t -> r k t"),
                          reads=[("uT", 0)], writes=[uq[qq]])
                for b4 in range(32):
                    pp = px[b4 % 2]
                    for qq in range(4):
                        for ri in range(2):
                            sl = (qq * 2 + ri) * 64
                            P.add("pe", lambda e, pp=pp, qq=qq, ri=ri, sl=sl, b4=b4: e.matmul(pp[:, sl:sl + 64], lhsT=BP[:, d, b4, ri, :],
                                                                                              rhs=uq[qq][:, b4, :], start=True, stop=True),
                                  reads=[BP, uq[qq]], writes=[pp])
                    P.add("act", lambda e, pp=pp, b4=b4: e.copy(out=X[:, :, 4 * b4:4 * b4 + 4, :].rearrange("p r q t -> p q r t"),
                                                                in_=pp[:, :].rearrange("p (q r t) -> p q r t", q=4, r=2)), reads=[pp], writes=[X])
                if int(os.environ.get("S5_STOP", "9")) <= 1:
                    return
                order = range(64) if d == 0 else range(63, -1, -1)
                prev = None
                for t in order:
                    sr_p = Sr[:, :] if prev is None else Hr[:, :, prev]
                    si_p = Si[:, :] if prev is None else Hi[:, :, prev]
                    a_, b_, c_, d__ = tt
                    rd = [Sr, Si] if prev is None else [Hr, Hi]
                    P.add("dve", lambda e, sr_p=sr_p: e.tensor_tensor(out=a_[:, :], in0=arS[:, d, :], in1=sr_p, op=ALU.mult), reads=[arS] + rd, writes=[a_])
                    P.add("pool", lambda e, si_p=si_p: e.tensor_tensor(out=b_[:, :], in0=aiS[:, d, :], in1=si_p, op=ALU.mult), reads=[aiS] + rd, writes=[b_])
                    P.add("pool", lambda e, si_p=si_p: e.tensor_tensor(out=c_[:, :], in0=arS[:, d, :], in1=si_p, op=ALU.mult), reads=[arS] + rd, writes=[c_])
                    P.add("dve", lambda e, sr_p=sr_p: e.tensor_tensor(out=d__[:, :], in0=aiS[:, d, :], in1=sr_p, op=ALU.mult), reads=[aiS] + rd, writes=[d__])
                    P.add("dve", lambda e: e.tensor_tensor(out=a_[:, :], in0=a_[:, :], in1=b_[:, :], op=ALU.subtract), reads=[a_, b_], writes=[a_])
                    P.add("pool", lambda e: e.tensor_tensor(out=c_[:, :], in0=c_[:, :], in1=d__[:, :], op=ALU.add), reads=[c_, d__], writes=[c_])
                    P.add("dve", lambda e, t=t: e.tensor_tensor(out=Hr[:, :, t], in0=a_[:, :], in1=X[:, 0, :, t], op=ALU.add), reads=[a_, X], writes=[Hr])
                    P.add("pool", lambda e, t=t: e.tensor_tensor(out=Hi[:, :, t], in0=c_[:, :], in1=X[:, 1, :, t], op=ALU.add), reads=[c_, X], writes=[Hi])
                    prev = t
                P.add("dve", lambda e, prev=prev: e.tensor_copy(out=Sr[:, :], in_=Hr[:, :, prev]), reads=[Hr], writes=[Sr])
                P.add("pool", lambda e, prev=prev: e.tensor_copy(out=Si[:, :], in_=Hi[:, :, prev]), reads=[Hi], writes=[Si])
                if int(os.environ.get("S5_STOP", "9")) <= 2:
                    return
                P.add("dve", lambda e: e.tensor_copy(out=X[:, 0, :, :], in_=Hr[:, :, :]), reads=[Hr, X], writes=[X])
                P.add("pool", lambda e: e.tensor_copy(out=X[:, 1, :, :], in_=Hi[:, :, :]), reads=[Hi, X], writes=[X])
                for half in range(2):
                    o = yo[nst[0] % 2]
                    nst[0] += 1
                    for qh in range(64):
                        q = half * 64 + qh
                        P.add("pe", lambda e, q=q, qh=qh: e.matmul(py[0:64, qh * 32:(qh + 1) * 32], lhsT=X[:, 0, q, :], rhs=CP[:, q, 0, :], start=True, stop=False),
                              reads=[X, CP], writes=[py])
                        P.add("pe", lambda e, q=q, qh=qh: e.matmul(py[0:64, qh * 32:(qh + 1) * 32], lhsT=X[:, 1, q, :], rhs=CP[:, q, 1, :], start=False, stop=True),
                              reads=[X, CP], writes=[py])
                    dst = y5.ap()[t0:t0 + 64, half * 2048:(half + 1) * 2048]
                    if d == 0:
                        P.add("act", lambda e, o=o: e.copy(out=o[:, :], in_=py[0:64, :]), reads=[py], writes=[o])
                    else:
                        P.dma(o[:, :], dst, reads=[("y5", t0, half)], writes=[o])
                        P.add("dve", lambda e, o=o: e.tensor_tensor(out=o[:, :], in0=o[:, :], in1=py[0:64, :], op=ALU.add), reads=[o, py], writes=[o])
                    P.dma(dst, o[:, :], reads=[o], writes=[("y5", t0, half)], eng="act")

            for ci_, t0_ in enumerate(chunks64[d][:int(os.environ.get("S5_NCH", "36"))]):
                do_chunk(ci_, t0_)

        for d_ in range(int(os.environ.get("S5_NDIR", "2"))):
            do_dir(d_)
    P.barrier()


def pass_s5_glu_in(C, y5, u_tm, d_skip, ident_bf, gT5):
    P = C.P
    with ExitStack() as es:
        idb = C.sb(es, [128, 128], BF16, "idb")
        dsk = C.sb(es, [128, D], F32, "dsk")
        yt = [C.sb(es, [128, D], F32, "yt") for _ in range(2)]
        ut = [C.sb(es, [128, D], F32, "ut") for _ in range(2)]
        w_ = [C.sb(es, [128, D], F32, "w") for _ in range(2)]
        gb = [C.sb(es, [128, D], BF16, "gb") for _ in range(2)]
        hst = [C.sb(es, [128, KC, 128], BF16, "hst") for _ in range(2)]
        ptr = [C.ps(es, [128, 1024], BF16, "ptr") for _ in range(2)]
        P.dma(idb[:, :], ident_bf.ap(), writes=[idb])
        P.dma(dsk[:, :], d_skip.partition_broadcast(128), writes=[dsk])
        npt = [0]

        def do_tile(i):
            y, u, w, g, hs = yt[i % 2], ut[i % 2], w_[i % 2], gb[i % 2], hst[i % 2]
            rows = slice(i * 128, (i + 1) * 128)
            t64 = [(i * 128 + 64 * a_, h_) for a_ in range(2) for h_ in range(2)]
            P.dma(y[:, :], y5.ap()[rows, :], reads=[("y5", t, h) for (t, h) in t64], writes=[y])
            P.dma(u[:, :], u_tm.ap()[rows, :], reads=[("u_tm", 0)], writes=[u])
            P.add("dve", lambda e: e.tensor_tensor(out=u[:, :], in0=u[:, :], in1=dsk[:, :], op=ALU.mult), reads=[u, dsk], writes=[u])
            P.add("pool", lambda e: e.tensor_tensor(out=y[:, :], in0=y[:, :], in1=u[:, :], op=ALU.add), reads=[y, u], writes=[y])
            P.add("act", lambda e: e.activation(out=w[:, :], in_=y[:, :], func=AF.Square), reads=[y], writes=[w])
            P.add("dve", lambda e: e.tensor_scalar(out=w[:, :], in0=w[:, :], scalar1=0.044715, scalar2=1.0, op0=ALU.mult, op1=ALU.add), reads=[w], writes=[w])
            P.add("pool", lambda e: e.tensor_tensor(out=w[:, :], in0=w[:, :], in1=y[:, :], op=ALU.mult), reads=[w, y], writes=[w])
            P.add("act", lambda e: e.activation(out=w[:, :], in_=w[:, :], func=AF.Sigmoid, scale=1.5957691216057308), reads=[w], writes=[w])
            P.add("dve", lambda e: e.tensor_tensor(out=g[:, :], in0=w[:, :], in1=y[:, :], op=ALU.mult), reads=[w, y], writes=[g])
            for q in range(4):
                pt = ptr[npt[0] % 2]
                npt[0] += 1
                for kk in range(8):
                    k = q * 8 + kk
                    P.add("pe", lambda e, kk=kk, k=k, pt=pt: e.transpose(out=pt[:, kk * 128:(kk + 1) * 128], in_=g[:, k * 128:(k + 1) * 128],
                                                                         identity=idb[:, :]), reads=[g, idb], writes=[pt])
                P.add("act", lambda e, q=q, pt=pt: e.copy(out=hs[:, q * 8:(q + 1) * 8, :], in_=pt[:, :].rearrange("p (k t) -> p k t", k=8)),
                      reads=[pt], writes=[hs])
            P.dma(gT5.ap()[i], hs[:, :, :], reads=[hs], writes=[("aT", i)])

        for i_ in range(NTT):
            do_tile(i_)
    P.barrier()


_W_SHAPES = {
    "mod_down": [4, D, 512], "mod_up": [4, 512, 6 * D], "mod_b": [4, 6 * D], "norm1_g": [4, D], "norm2_g": [4, D], "c_ctx": [D],
    "swa_w_in": [1, D, 6144], "swa_q_g": [1, 128], "swa_k_g": [1, 128], "swa_sinks": [1, 32], "swa_w_out": [1, D, D],
    "ssd_w_in": [1, D, 18688], "ssd_conv_w": [1, 3, 10240], "ssd_conv_b": [1, 10240], "ssd_dt_bias": [1, 2, 128], "ssd_a_log": [1, 2, 128],
    "ssd_d": [1, 128], "ssd_norm_g": [1, 8192], "ssd_w_out": [1, 8192, D],
    "s5_w_in": [1, D, D], "s5_lam_re": [1, 2, 256, 64], "s5_lam_im": [1, 2, 256, 64], "s5_log_dt": [1, 2, 256],
    "s5_b_re": [1, 256, 64, 16], "s5_b_im": [1, 256, 64, 16], "s5_c_re": [1, 256, 16, 64], "s5_c_im": [1, 256, 16, 64], "s5_d": [1, D],
    "s5_w_glu": [1, D, 2 * D],
    "mla_w_in": [1, D, 1600], "mla_q_a_g": [1, 1024], "mla_kv_a_g": [1, 512], "mla_w_uq": [1, 1024, 6144], "mla_w_ukv": [1, 512, 8192],
    "mla_q_g": [1, 192], "mla_k_g": [1, 192], "mla_w_out": [1, D, D],
    "moe_w_group": [4, D, 4], "moe_b_group": [4, 4], "moe_w_expert": [4, D, 32], "moe_b_expert": [4, 32],
    "moe_w1": [4, 32, D, 256], "moe_w3": [4, 32, D, 256], "moe_w2": [4, 32, 256, D],
}


def build_program(layers=(0, 1, 2, 3)):
    nc = bass.Bass("TRN2", target_bir_lowering=False)
    C = Ctx(nc)
    P = C.P
    hc = host_consts()

    def inp(name, shape, dt=F32):
        return nc.dram_tensor(name, list(shape), dt, kind="ExternalInput")

    x = inp("x", [NL, D])
    ctx = inp("ctx", [NCX, D])
    c_b = inp("c_b", [D])
    W = {k: inp(k, v) for k, v in _W_SHAPES.items()}
    cin = {k: inp(k, v.shape, F32 if v.dtype == np.float32 else BF16) for k, v in hc.items()}
    out = nc.dram_tensor("out", [NL, D], F32, kind="ExternalOutput")
    xres = nc.dram_tensor("xres", [NT, D], F32)
    modv = nc.dram_tensor("modv", [4, 2, 6 * D], F32)
    hT = nc.dram_tensor("hT", [NTT, 128, KC, 128], BF16)
    aT = nc.dram_tensor("aT", [NTT, 128, 64, 128], BF16)
    aT32 = nc.dram_tensor("aT32", [NTT, 128, KC, 128], BF16)
    for i in range(NTT):
        src = x.ap()[i * 128:(i + 1) * 128, :] if i < 16 else ctx.ap()[(i - 16) * 128:(i - 15) * 128, :]
        P.dma(xres.ap()[i * 128:(i + 1) * 128, :], src, writes=[("xres", i)])
    pass_adaln(C, c_b, W["c_ctx"], W["mod_down"], W["mod_up"], W["mod_b"], modv, cin["ident"], layers=layers)
    for l in layers:
        last = l == 3
        pass_norm(C, xres, modv, l, W["norm1_g"].ap()[l], 0, 1, hT, cin["ident_bf"])
        if l == 0:
            qT = nc.dram_tensor("qT", [40, 128, NT], BF16)
            Vd = nc.dram_tensor("Vd_swa", [NT, 1024], BF16)
            pass_swa_proj(C, hT, W["swa_w_in"].ap()[0], W["swa_q_g"].ap()[0], W["swa_k_g"].ap()[0], cin["swa_cos"], cin["swa_sin"],
                          cin["swa_rot"], qT, Vd)
            pass_swa_attn(C, qT, Vd, W["swa_sinks"].ap()[0], cin["maskP"], cin["maskN"], aT32)
            pass_outproj(C, aT32, KC, W["swa_w_out"].ap()[0], D, modv, l, 2, xres, 9)
        elif l == 1:
            xbc_raw = nc.dram_tensor("xbc_raw", [80, 128, NT], F32)
            szd = nc.dram_tensor("szd", [NT, 8192], BF16)
            dtd = nc.dram_tensor("dtd", [NT, 256], F32)
            xs_tm = nc.dram_tensor("xs_tm", [NT, 8192], BF16)
            B_tm = nc.dram_tensor("B_tm", [NT, 1024], BF16)
            BCT = nc.dram_tensor("BCT", [16, 128, NT], BF16)
            ydr = nc.dram_tensor("ydr", [NT, 8192], F32)
            pass_ssd_proj(C, hT, W["ssd_w_in"].ap()[0], xbc_raw, szd, dtd)
            pass_ssd_conv(C, xbc_raw, W["ssd_conv_w"].ap()[0], W["ssd_conv_b"].ap()[0], cin["ident_bf"], xs_tm, B_tm, BCT)
            pass_ssd_scan(C, xs_tm, B_tm, BCT, dtd, W["ssd_dt_bias"].ap()[0], W["ssd_a_log"].ap()[0], W["ssd_d"].ap()[0],
                          cin["ident"], cin["ident_bf"], cin["ssd_U"], cin["ssd_negm"], ydr)
            pass_ssd_finish(C, ydr, szd, W["ssd_norm_g"].ap()[0], cin["ident_bf"], aT)
            pass_outproj(C, aT, 64, W["ssd_w_out"].ap()[0], D, modv, l, 2, xres, 4, cb=256)
        elif l == 2:
            uT_d = nc.dram_tensor("uT_d", [32, 128, NT], BF16)
            u_tm = nc.dram_tensor("u_tm", [NT, D], F32)
            y5 = nc.dram_tensor("y5", [NT, D], F32)
            pass_s5_proj(C, hT, W["s5_w_in"].ap()[0], uT_d, u_tm)
            pass_s5_scan(C, uT_d, W["s5_lam_re"].ap()[0], W["s5_lam_im"].ap()[0], W["s5_log_dt"].ap()[0], W["s5_b_re"].ap()[0],
                         W["s5_b_im"].ap()[0], W["s5_c_re"].ap()[0], W["s5_c_im"].ap()[0], cin["ident"], cin["s5_rowmask"], cin["s5_cmask"], y5)
            pass_s5_glu_in(C, y5, u_tm, W["s5_d"].ap()[0], cin["ident_bf"], aT32)
            pass_outproj(C, aT32, KC, W["s5_w_glu"].ap()[0], D, modv, l, 2, xres, 9, glu=True, cb=256)
        else:
            cqT = nc.dram_tensor("cqT", [NTT, 128, 8, 128], BF16)
            ckvT = nc.dram_tensor("ckvT", [NTT, 128, 4, 128], BF16)
            krT = nc.dram_tensor("krT", [64, NT], F32)
            QN = nc.dram_tensor("QN", [32, 128, NT], BF16)
            QR = nc.dram_tensor("QR", [32, 64, NT], BF16)
            KN = nc.dram_tensor("KN", [32, 128, NT], BF16)
            KR = nc.dram_tensor("KR", [32, 64, NT], BF16)
            Vm = nc.dram_tensor("Vd_mla", [NT, 4096], BF16)
            pass_mla_a(C, hT, W["mla_w_in"].ap()[0], W["mla_q_a_g"].ap()[0], W["mla_kv_a_g"].ap()[0], cqT, ckvT, krT)
            pass_mla_b(C, cqT, ckvT, krT, W["mla_w_uq"].ap()[0], W["mla_w_ukv"].ap()[0], W["mla_q_g"].ap()[0], W["mla_k_g"].ap()[0],
                       cin["mla_cos"], cin["mla_sin"], cin["mla_rot"], QN, QR, KN, KR, Vm)
            pass_mla_attn(C, QN, QR, KN, KR, Vm, aT32, ctx_out=False)
            pass_outproj(C, aT32, KC, W["mla_w_out"].ap()[0], D, modv, l, 2, xres, 9)
        pass_norm(C, xres, modv, l, W["norm2_g"].ap()[l], 3, 4, hT, cin["ident_bf"])
        pass_moe(C, hT, W["moe_w_group"].ap()[l], W["moe_b_group"].ap()[l], W["moe_w_expert"].ap()[l], W["moe_b_expert"].ap()[l],
                 W["moe_w1"].ap()[l], W["moe_w3"].ap()[l], W["moe_w2"].ap()[l], modv, l, xres, cin["ident"],
                 groups=[(0, 4), (4, 4), (8, 4), (12, 4)] if last else None)
    for i in range(16):
        P.dma(out.ap()[i * 128:(i + 1) * 128, :], xres.ap()[i * 128:(i + 1) * 128, :], reads=[("xres", i)], writes=[("out", i)], is_out=True)
    P.emit()
    return nc, hc


def kernel(**inputs):
    nc, hc = build_program()
    shared = {k: np.ascontiguousarray(np.asarray(inputs[k], dtype=np.float32)) for k in _W_SHAPES}
    shared.update(hc)
    x = np.asarray(inputs["x"], dtype=np.float32)
    c = np.asarray(inputs["c"], dtype=np.float32)
    ctx = np.asarray(inputs["ctx"], dtype=np.float32)
    in_maps = []
    for b in range(8):
        m = dict(shared)
        m["x"] = np.ascontiguousarray(x[b])
        m["ctx"] = np.ascontiguousarray(ctx[b])
        m["c_b"] = np.ascontiguousarray(c[b])
        in_maps.append(m)
    res = run_bass_kernel_spmd(nc, in_maps, core_ids=list(range(8)))
    return np.stack([np.asarray(r["out"], dtype=np.float32) for r in res.results], 0)
```
